# Optimizing a Trainium2 kernel written in Bass

```python
import math
import jax
import jax.numpy as jnp
from jax import lax
import numpy as np

D_MODEL = 1024
BATCH = 4
SEQ = 8192
DEPTH = 4

HEAD_DIM = 64
MIX_HALF = D_MODEL // 2
A_Q_HEADS = MIX_HALF // HEAD_DIM
A_KV_HEADS = A_Q_HEADS // 4
A_WINDOW = 128
B_KEY_DIM = 128
B_HEADS = MIX_HALF // B_KEY_DIM
B_VAL_DIM = MIX_HALF // B_HEADS
B_CHUNK = 64
B_MIN_F = 1e-30
C_VAL_DIM = 128
C_HEADS = MIX_HALF // C_VAL_DIM
C_KEY_DIM = C_VAL_DIM // 2
C_CHUNK = 128
RET_THETA = 10000.0
D_Q_HEADS = MIX_HALF // HEAD_DIM
D_KV_HEADS = D_Q_HEADS // 4
D_PATTERNS = ((128, 1), (512, 4), (2048, 16))
ATTN_BLOCK = 128
ROPE_THETA = 500000.0
ROPE_DIM = HEAD_DIM // 4
N_EXPERTS = 32
TOP_K = 4
D_EXPERT = D_MODEL
SWIGLU_LIMIT = 7.0
SWIGLU_ALPHA = 1.702
MOE_BLOCK = 512
DN_ALPHA = (2 * DEPTH) ** 0.25
DN_BETA = (8 * DEPTH) ** -0.25
LN_EPS = 1e-5
NORM_EPS = 1e-6
N_EVEN = (DEPTH + 1) // 2
N_ODD = DEPTH // 2

EVEN_WIDTHS = (A_Q_HEADS * HEAD_DIM, A_KV_HEADS * HEAD_DIM, A_KV_HEADS * HEAD_DIM,
               B_HEADS * B_KEY_DIM, B_HEADS * B_KEY_DIM, B_HEADS * B_VAL_DIM, B_HEADS * B_VAL_DIM)
ODD_WIDTHS = (C_HEADS * C_KEY_DIM, C_HEADS * C_KEY_DIM, C_HEADS * C_VAL_DIM, C_HEADS * C_VAL_DIM) + \
    (D_Q_HEADS * HEAD_DIM, D_KV_HEADS * HEAD_DIM, D_KV_HEADS * HEAD_DIM) * len(D_PATTERNS)
EVEN_COLS = sum(EVEN_WIDTHS)
ODD_COLS = sum(ODD_WIDTHS)
EVEN_MIX = A_Q_HEADS * HEAD_DIM + B_HEADS * B_VAL_DIM
ODD_MIX = C_HEADS * C_VAL_DIM + D_Q_HEADS * HEAD_DIM

kernel_name = 'hybrid_swa_hgrn2_retention_dilated_moe_deepnorm'


def split_cols(h, widths):
    offs, acc = [], 0
    for w in widths[:-1]:
        acc += w
        offs.append(acc)
    return jnp.split(h, offs, axis=-1)


def layer_norm(x, g, b):
    xf = x.astype(jnp.float32)
    mu = jnp.mean(xf, -1, keepdims=True)
    var = jnp.mean(jnp.square(xf - mu), -1, keepdims=True)
    return ((xf - mu) * lax.rsqrt(var + LN_EPS) * g + b).astype(x.dtype)


def head_norm(o, gain, center):
    B, T = o.shape[:2]
    if center:
        o = o - jnp.mean(o, -1, keepdims=True)
    o = o * lax.rsqrt(jnp.mean(o * o, -1, keepdims=True) + NORM_EPS)
    return o.reshape(B, T, -1) * gain.astype(jnp.float32)


def rotary_tables(pos, inv_freq):
    ang = pos[:, None] * inv_freq[None, :]
    return jnp.cos(ang), jnp.sin(ang)


def apply_rotary(x, rope):
    cos, sin = rope
    half = cos.shape[-1]
    c = cos[None, :, None, :].astype(x.dtype)
    s = sin[None, :, None, :].astype(x.dtype)
    x1, x2, rest = x[..., :half], x[..., half:2 * half], x[..., 2 * half:]
    return jnp.concatenate([x1 * c - x2 * s, x2 * c + x1 * s, rest], axis=-1)


def banded_attention(q, k, v, max_dist, sink=None):
    N, Hkv, G, L, hd = q.shape
    nb = L // ATTN_BLOCK
    qb = q.reshape(N, Hkv, G, nb, ATTN_BLOCK, hd).astype(jnp.float32)

    def with_prev(t):
        tb = t.reshape(N, Hkv, nb, ATTN_BLOCK, hd).astype(jnp.float32)
        prev = jnp.concatenate([jnp.zeros_like(tb[:, :, :1]), tb[:, :, :-1]], axis=2)
        return jnp.concatenate([prev, tb], axis=3)

    kb, vb = with_prev(k), with_prev(v)
    s = jnp.einsum('nhgbqd,nhbkd->nhgbqk', qb, kb) * (hd ** -0.5)
    qi = jnp.arange(ATTN_BLOCK)[:, None]
    kj = jnp.arange(2 * ATTN_BLOCK)[None, :]
    dist = qi - kj + ATTN_BLOCK
    blk = jnp.arange(nb)[:, None, None]
    valid = (dist >= 0) & (dist <= max_dist) & ((blk > 0) | (kj >= ATTN_BLOCK))
    s = jnp.where(valid, s, -jnp.inf)
    if sink is not None:
        sk = jnp.broadcast_to(sink.astype(jnp.float32)[None, :, :, None, None, None], s.shape[:-1] + (1,))
        s = jnp.concatenate([s, sk], axis=-1)
    lse = jax.nn.logsumexp(s, axis=-1)
    p = jnp.exp(s - lse[..., None])
    if sink is not None:
        p = p[..., :-1]
    o = jnp.einsum('nhgbqk,nhbkd->nhgbqd', p, vb)
    return o.reshape(N, Hkv, G, L, hd).astype(v.dtype), lse.reshape(N, Hkv, G, L)


def swa_sink_attention(q, k, v, sinks, rope):
    B, T, Hq, hd = q.shape
    Hkv = k.shape[2]
    G = Hq // Hkv
    q = apply_rotary(q, rope)
    k = apply_rotary(k, rope)
    qh = q.reshape(B, T, Hkv, G, hd).transpose(0, 2, 3, 1, 4)
    o, _ = banded_attention(qh, k.transpose(0, 2, 1, 3), v.transpose(0, 2, 1, 3),
                            A_WINDOW - 1, sinks.reshape(Hkv, G))
    return o.transpose(0, 3, 1, 2, 4).reshape(B, T, Hq * hd)


def hgrn2(q, f_logit, i_in, g, lb, norm_g):
    B, T, H, dk = q.shape
    dv = i_in.shape[-1]
    lb = jnp.clip(lb.reshape(H, dk).astype(jnp.float32), 0.0, 1.0)
    f = lb + (1.0 - lb) * jax.nn.sigmoid(f_logit.astype(jnp.float32))
    log_f = jnp.log(jnp.maximum(f, B_MIN_F))
    key = 1.0 - f
    C = B_CHUNK
    nc = T // C

    def chunks(t):
        return t.astype(jnp.float32).reshape(B, nc, C, H, t.shape[-1]).transpose(1, 0, 3, 2, 4)

    causal = jnp.tril(jnp.ones((C, C), dtype=bool))[:, :, None]

    def step(S, xs):
        qc, kc, lfc, vc = xs
        b = jnp.cumsum(lfc, axis=2)
        pair = jnp.exp(jnp.where(causal, b[:, :, :, None, :] - b[:, :, None, :, :], -jnp.inf))
        att = jnp.einsum('bhtc,bhtsc,bhsc->bhts', qc, pair, kc)
        o = jnp.einsum('bhts,bhsv->bhtv', att, vc) + jnp.einsum('bhtc,bhcv->bhtv', qc * jnp.exp(b), S)
        b_last = b[:, :, -1:, :]
        S = jnp.exp(b_last[:, :, 0, :])[..., None] * S + \
            jnp.einsum('bhsc,bhsv->bhcv', kc * jnp.exp(b_last - b), vc)
        return S, o

    S0 = jnp.zeros((B, H, dk, dv), jnp.float32)
    _, o = lax.scan(step, S0, (chunks(q), chunks(key), chunks(log_f), chunks(i_in)))
    o = o.transpose(1, 0, 3, 2, 4).reshape(B, T, H, dv)
    y = head_norm(o, norm_g, center=False) * jax.nn.silu(g.astype(jnp.float32))
    return y.astype(i_in.dtype)


def retention(q, k, v, g, norm_g, rope):
    B, T, H, dk = q.shape
    dv = v.shape[-1]
    out_dtype = v.dtype
    q = apply_rotary(q, rope).astype(jnp.float32)
    k = apply_rotary(k, rope).astype(jnp.float32) * (dk ** -0.5)
    v = v.astype(jnp.float32)
    C = C_CHUNK
    nc = T // C
    log_gamma = jnp.log1p(-jnp.exp2(-5.0 - jnp.arange(H, dtype=jnp.float32)))

    def chunks(t):
        return t.reshape(B, nc, C, H, t.shape[-1]).transpose(0, 3, 1, 2, 4)

    qc, kc, vc = chunks(q), chunks(k), chunks(v)
    i = jnp.arange(C, dtype=jnp.float32)
    rel = i[:, None] - i[None, :]
    decay = jnp.where(rel >= 0, jnp.exp(log_gamma[:, None, None] * jnp.maximum(rel, 0.0)), 0.0)
    scores = jnp.einsum('bhnid,bhnjd->bhnij', qc, kc) * decay[None, :, None]
    o_intra = jnp.einsum('bhnij,bhnjv->bhniv', scores, vc)
    k_tail = kc * jnp.exp(log_gamma[:, None] * (C - 1 - i)[None, :])[None, :, None, :, None]
    R = jnp.einsum('bhnjd,bhnjv->bhndv', k_tail, vc)
    chunk_decay = jnp.exp(log_gamma * C)[None, :, None, None]

    def step(S, Rn):
        return chunk_decay * S + Rn, S

    _, S_prev = lax.scan(step, jnp.zeros((B, H, dk, dv), jnp.float32), R.transpose(2, 0, 1, 3, 4))
    S_prev = S_prev.transpose(1, 2, 0, 3, 4)
    q_head = qc * jnp.exp(log_gamma[:, None] * (i + 1.0)[None, :])[None, :, None, :, None]
    o_inter = jnp.einsum('bhnid,bhndv->bhniv', q_head, S_prev)
    o = (o_intra + o_inter).transpose(0, 2, 3, 1, 4).reshape(B, T, H, dv)
    y = head_norm(o, norm_g, center=True) * jax.nn.silu(g.astype(jnp.float32))
    return y.astype(out_dtype)


def dilated_attention(qs, ks, vs, rope):
    outs, lses = [], []
    for (window, dil), q, k, v in zip(D_PATTERNS, qs, ks, vs):
        B, T, Hq, hd = q.shape
        Hkv = k.shape[2]
        G = Hq // Hkv
        q = apply_rotary(q, rope)
        k = apply_rotary(k, rope)
        span = dil * ATTN_BLOCK
        Tp = -(-T // span) * span
        L = Tp // dil
        pad = ((0, 0), (0, Tp - T), (0, 0), (0, 0))
        qg = jnp.pad(q, pad).reshape(B, L, dil, Hkv, G, hd).transpose(0, 2, 3, 4, 1, 5).reshape(B * dil, Hkv, G, L, hd)
        kg = jnp.pad(k, pad).reshape(B, L, dil, Hkv, hd).transpose(0, 2, 3, 1, 4).reshape(B * dil, Hkv, L, hd)
        vg = jnp.pad(v, pad).reshape(B, L, dil, Hkv, hd).transpose(0, 2, 3, 1, 4).reshape(B * dil, Hkv, L, hd)
        o, lse = banded_attention(qg, kg, vg, window // dil)
        o = o.reshape(B, dil, Hkv, G, L, hd).transpose(0, 4, 1, 2, 3, 5).reshape(B, Tp, Hq, hd)[:, :T]
        lse = lse.reshape(B, dil, Hkv, G, L).transpose(0, 4, 1, 2, 3).reshape(B, Tp, Hq)[:, :T]
        outs.append(o.astype(jnp.float32))
        lses.append(lse)
    w = jax.nn.softmax(jnp.stack(lses, axis=0), axis=0)
    o = jnp.sum(w[..., None] * jnp.stack(outs, axis=0), axis=0)
    B, T, Hq, hd = o.shape
    return o.reshape(B, T, Hq * hd).astype(vs[0].dtype)


def even_mixer(x, w_in, b_in, sinks, lb, norm_g, w_out, rope):
    B, T, _ = x.shape
    h = x @ w_in + b_in
    aq, ak, av, bq, bf, bi, bg = split_cols(h, EVEN_WIDTHS)
    ya = swa_sink_attention(aq.reshape(B, T, A_Q_HEADS, HEAD_DIM),
                            ak.reshape(B, T, A_KV_HEADS, HEAD_DIM),
                            av.reshape(B, T, A_KV_HEADS, HEAD_DIM), sinks, rope)
    yb = hgrn2(bq.reshape(B, T, B_HEADS, B_KEY_DIM), bf.reshape(B, T, B_HEADS, B_KEY_DIM),
               bi.reshape(B, T, B_HEADS, B_VAL_DIM), bg, lb, norm_g)
    return jnp.concatenate([ya, yb], axis=-1) @ w_out


def odd_mixer(x, w_in, b_in, norm_g, w_out, rope, ret_rope):
    B, T, _ = x.shape
    h = x @ w_in + b_in
    parts = split_cols(h, ODD_WIDTHS)
    cq, ck, cv, cg = parts[:4]
    yc = retention(cq.reshape(B, T, C_HEADS, C_KEY_DIM), ck.reshape(B, T, C_HEADS, C_KEY_DIM),
                   cv.reshape(B, T, C_HEADS, C_VAL_DIM), cg, norm_g, ret_rope)
    dparts = parts[4:]
    qs = [dparts[3 * p].reshape(B, T, D_Q_HEADS, HEAD_DIM) for p in range(len(D_PATTERNS))]
    ks = [dparts[3 * p + 1].reshape(B, T, D_KV_HEADS, HEAD_DIM) for p in range(len(D_PATTERNS))]
    vs = [dparts[3 * p + 2].reshape(B, T, D_KV_HEADS, HEAD_DIM) for p in range(len(D_PATTERNS))]
    yd = dilated_attention(qs, ks, vs, rope)
    return jnp.concatenate([yc, yd], axis=-1) @ w_out


def moe_ffn(h, w_router, b_router, w_gu, b_gu, w_dn, b_dn):
    B, T, D = h.shape
    N = B * T
    NK = N * TOP_K
    hf = h.reshape(N, D)
    logits = (hf @ w_router).astype(jnp.float32) + b_router.astype(jnp.float32)
    top_val, top_idx = lax.top_k(logits, TOP_K)
    gates = jax.nn.softmax(top_val, axis=-1)
    flat_e = top_idx.reshape(NK)
    flat_tok = jnp.arange(NK, dtype=jnp.int32) // TOP_K
    order = jnp.argsort(flat_e)
    sorted_e = flat_e[order]
    counts = jnp.bincount(flat_e, length=N_EXPERTS)
    padded = (counts + MOE_BLOCK - 1) // MOE_BLOCK * MOE_BLOCK
    pad_end = jnp.cumsum(padded)
    pad_start = pad_end - padded
    start = jnp.cumsum(counts) - counts
    dest = pad_start[sorted_e] + jnp.arange(NK, dtype=jnp.int32) - start[sorted_e]
    n_blocks = NK // MOE_BLOCK + N_EXPERTS + 1
    cap = n_blocks * MOE_BLOCK
    rows = jnp.full((cap,), N, dtype=jnp.int32).at[dest].set(flat_tok[order])
    gate_rows = jnp.zeros((cap,), jnp.float32).at[dest].set(gates.reshape(NK)[order])
    block_e = jnp.minimum(jnp.searchsorted(pad_end, jnp.arange(n_blocks, dtype=jnp.int32) * MOE_BLOCK,
                                           side='right'), N_EXPERTS - 1)
    h_pad = jnp.concatenate([hf, jnp.zeros((1, D), hf.dtype)], axis=0)

    def expert_block(args):
        rb, gb, e = args
        xb = h_pad[rb]
        gu = xb @ w_gu[e] + b_gu[e]
        gate = jnp.minimum(gu[:, :D_EXPERT], SWIGLU_LIMIT)
        up = jnp.clip(gu[:, D_EXPERT:], -SWIGLU_LIMIT, SWIGLU_LIMIT)
        act = (up + 1.0) * gate * jax.nn.sigmoid(SWIGLU_ALPHA * gate)
        y = act @ w_dn[e] + b_dn[e]
        return y * gb[:, None].astype(y.dtype)

    y = lax.map(expert_block, (rows.reshape(n_blocks, MOE_BLOCK), gate_rows.reshape(n_blocks, MOE_BLOCK), block_e))
    out = jnp.zeros((N + 1, D), y.dtype).at[rows].add(y.reshape(cap, D))
    return out[:N].reshape(B, T, D).astype(h.dtype)


def setup_inputs(seed: int = 0) -> dict:
    key = jax.random.key(seed)
    ks = jax.random.split(key, 24)

    def nrm(k, shape, scale):
        return jax.random.normal(k, shape, jnp.float32) * scale

    return {
        'x': nrm(ks[0], (BATCH, SEQ, D_MODEL), 1.0),
        'w_in_even': nrm(ks[1], (N_EVEN, D_MODEL, EVEN_COLS), D_MODEL ** -0.5),
        'b_in_even': nrm(ks[2], (N_EVEN, EVEN_COLS), 0.02),
        'attn_sinks': nrm(ks[3], (N_EVEN, A_Q_HEADS), 0.5),
        'hgrn_lb_logits': nrm(ks[4], (N_EVEN, B_HEADS * B_KEY_DIM), 1.0),
        'hgrn_norm': 1.0 + nrm(ks[5], (N_EVEN, B_HEADS * B_VAL_DIM), 0.02),
        'w_out_even': nrm(ks[6], (N_EVEN, EVEN_MIX, D_MODEL), EVEN_MIX ** -0.5 * DN_BETA),
        'w_in_odd': nrm(ks[7], (N_ODD, D_MODEL, ODD_COLS), D_MODEL ** -0.5),
        'b_in_odd': nrm(ks[8], (N_ODD, ODD_COLS), 0.02),
        'ret_norm': 1.0 + nrm(ks[9], (N_ODD, C_HEADS * C_VAL_DIM), 0.02),
        'w_out_odd': nrm(ks[10], (N_ODD, ODD_MIX, D_MODEL), ODD_MIX ** -0.5 * DN_BETA),
        'ln1_g': 1.0 + nrm(ks[11], (DEPTH, D_MODEL), 0.02),
        'ln1_b': nrm(ks[12], (DEPTH, D_MODEL), 0.02),
        'ln2_g': 1.0 + nrm(ks[13], (DEPTH, D_MODEL), 0.02),
        'ln2_b': nrm(ks[14], (DEPTH, D_MODEL), 0.02),
        'router_w': nrm(ks[15], (DEPTH, D_MODEL, N_EXPERTS), D_MODEL ** -0.5),
        'router_b': nrm(ks[16], (DEPTH, N_EXPERTS), 0.01),
        'expert_w_gu': nrm(ks[17], (DEPTH, N_EXPERTS, D_MODEL, 2 * D_EXPERT), D_MODEL ** -0.5),
        'expert_b_gu': nrm(ks[18], (DEPTH, N_EXPERTS, 2 * D_EXPERT), 0.02),
        'expert_w_dn': nrm(ks[19], (DEPTH, N_EXPERTS, D_EXPERT, D_MODEL), D_EXPERT ** -0.5 * DN_BETA),
        'expert_b_dn': nrm(ks[20], (DEPTH, N_EXPERTS, D_MODEL), 0.02),
    }


def reference(x, w_in_even, b_in_even, attn_sinks, hgrn_lb_logits, hgrn_norm, w_out_even,
              w_in_odd, b_in_odd, ret_norm, w_out_odd, ln1_g, ln1_b, ln2_g, ln2_b,
              router_w, router_b, expert_w_gu, expert_b_gu, expert_w_dn, expert_b_dn):
    T = x.shape[1]
    pos = jnp.arange(T, dtype=jnp.float32)
    rope_inv = 1.0 / (ROPE_THETA ** (jnp.arange(0, ROPE_DIM, 2, dtype=jnp.float32) / ROPE_DIM))
    ret_inv = 1.0 / (RET_THETA ** jnp.linspace(0.0, 1.0, C_KEY_DIM // 2, dtype=jnp.float32))
    rope = rotary_tables(pos, rope_inv)
    ret_rope = rotary_tables(pos, ret_inv)
    lb_soft = jax.nn.softmax(hgrn_lb_logits.astype(jnp.float32), axis=0)
    lower_bounds = jnp.concatenate([jnp.zeros_like(lb_soft[:1]), jnp.cumsum(lb_soft, axis=0)[:-1]], axis=0)
    for layer in range(DEPTH):
        j = layer // 2
        if layer % 2 == 0:
            mix = even_mixer(x, w_in_even[j], b_in_even[j], attn_sinks[j], lower_bounds[j],
                             hgrn_norm[j], w_out_even[j], rope)
        else:
            mix = odd_mixer(x, w_in_odd[j], b_in_odd[j], ret_norm[j], w_out_odd[j], rope, ret_rope)
        x = layer_norm(DN_ALPHA * x + mix, ln1_g[layer], ln1_b[layer])
        ffn = moe_ffn(x, router_w[layer], router_b[layer], expert_w_gu[layer], expert_b_gu[layer],
                      expert_w_dn[layer], expert_b_dn[layer])
        x = layer_norm(DN_ALPHA * x + ffn, ln2_g[layer], ln2_b[layer])
    return x
```

```python
import numpy as np
from contextlib import ExitStack
import concourse.bass as bass
import concourse.mybir as mybir
from concourse.bass_utils import run_bass_kernel_spmd

F32 = mybir.dt.float32
BF16 = mybir.dt.bfloat16
I32 = mybir.dt.int32
U32 = mybir.dt.uint32
AF = mybir.ActivationFunctionType
ALU = mybir.AluOpType
AX = mybir.AxisListType

D = 1024
NEXP = 32
DN_ALPHA = 8 ** 0.25
LN_EPS = 1e-5
NORM_EPS = 1e-6
DPAT = (1, 4, 16)

C_ID, C_MCUR, C_MPA, C_MPD, C_MBD, C_TRI, C_ONE = 0, 128, 256, 384, 512, 640, 768
C_IOTA, C_RS, C_DT, C_QD, C_KD = 896, 928, 1440, 1952, 2464
C_RM = 2976
NCST = 2980


class DSem:
    def __init__(self, sem):
        self.sem = sem
        self.cnt = 0


class Tile:
    def __init__(self, t, name):
        self.t = t
        self.name = name
        self.ds = None

    def __getitem__(self, idx):
        return self.t[idx]


class Sched:
    def __init__(self, nc, ctx, ndsem=64):
        self.nc = nc
        self.ctx = ctx
        self.eng = {'pe': nc.tensor, 'act': nc.scalar, 'dve': nc.vector, 'pool': nc.gpsimd, 'sp': nc.sync}
        self.esem = {}
        self.ecnt = {}
        for e in ('pe', 'act', 'dve', 'pool'):
            self.esem[e] = ctx.enter_context(nc.semaphore('s_' + e))
            self.ecnt[e] = 0
        self.seen = {e: {} for e in self.eng}
        self.writers = {}
        self.readers = {}
        self.ninst = 0
        self.free_ds = [DSem(ctx.enter_context(nc.semaphore('d%d' % i))) for i in range(ndsem)]
        self.all_ds = list(self.free_ds)

    def _deps(self, reads, writes):
        need = {}
        for k in list(reads) + list(writes):
            for s, v in self.writers.get(k, {}).items():
                if need.get(s, (None, 0))[1] < v[1]:
                    need[s] = v
        for k in writes:
            for s, v in self.readers.get(k, {}).items():
                if need.get(s, (None, 0))[1] < v[1]:
                    need[s] = v
        return need

    def _wait(self, e, need, skip=None):
        eng = self.eng[e]
        seen = self.seen[e]
        for s, (sem, val) in need.items():
            if s == skip or seen.get(s, 0) >= val:
                continue
            eng.wait_ge(sem, val)
            seen[s] = val
            self.ninst += 1

    def _commit(self, ev, reads, writes):
        s = id(ev[0])
        for k in writes:
            self.writers[k] = {s: ev}
            self.readers[k] = {}
        for k in reads:
            if k not in writes:
                self.readers.setdefault(k, {})[s] = ev

    def op(self, e, fn, reads=(), writes=()):
        need = self._deps(reads, writes)
        sem = self.esem[e]
        self._wait(e, need, id(sem) if e == 'pe' else None)
        ins = fn(self.eng[e])
        self.ecnt[e] += 1
        ins.then_inc(sem, 1)
        self.ninst += 1
        self._commit((sem, self.ecnt[e]), reads, writes)

    def dma(self, q, out, in_, reads=(), writes=(), st=None, fn=None):
        if st.ds is None:
            st.ds = self.free_ds.pop()
        ds = st.ds
        need = self._deps(reads, writes)
        if ds.cnt > 0:
            need[id(ds.sem)] = (ds.sem, ds.cnt)
        self._wait(q, need)
        ins = self.eng[q].dma_start(out=out, in_=in_) if fn is None else fn(self.eng[q])
        ds.cnt += 16
        ins.then_inc(ds.sem, 16)
        self.ninst += 1
        self._commit((ds.sem, ds.cnt), reads, writes)

    def barrier(self):
        need = {}
        for e in self.esem:
            if self.ecnt[e] > 0:
                need[id(self.esem[e])] = (self.esem[e], self.ecnt[e])
        for ds in self.all_ds:
            if ds.cnt > 0:
                need[id(ds.sem)] = (ds.sem, ds.cnt)
        for e in self.eng:
            self._wait(e, need)


class Phase:
    uid = 0

    def __init__(self, S):
        self.S = S
        self.stack = ExitStack()
        self.tiles = []

    def __enter__(self):
        self.stack.__enter__()
        return self

    def sbuf(self, name, shape, dt):
        Phase.uid += 1
        name = "%s_u%d" % (name, Phase.uid)
        t = Tile(self.stack.enter_context(self.S.nc.sbuf_tensor(name, list(shape), dt)), name)
        self.tiles.append(t)
        return t

    def __exit__(self, *a):
        self.S.barrier()
        for t in self.tiles:
            if t.ds is not None:
                self.S.free_ds.append(t.ds)
                t.ds = None
        return self.stack.__exit__(*a)


class Bank:
    def __init__(self, tile):
        self.tile = tile
        self.started = False


def build(T, layer_kinds, CAP, debug=False):
    NT = T // 128
    NL = len(layer_kinds)
    n_even = sum(1 for k in layer_kinds if k == 'e')
    n_odd = NL - n_even
    nc = bass.Bass("TRN2", target_bir_lowering=False)

    def din(name, shape, dt=F32):
        return nc.dram_tensor(name, list(shape), dt, kind="ExternalInput").ap()

    def dscr(name, shape, dt=F32):
        kind = "ExternalOutput" if debug else "Internal"
        return nc.dram_tensor(name, list(shape), dt, kind=kind).ap()

    x_in = din("x", [T, D])
    cst_d = din("cst", [128, NCST])
    tokid_d = din("tokid", [128, NT * 16], I32)
    ropeA = {d: (din("ropeC%d" % d, [64, T]), din("ropeS%d" % d, [64, T])) for d in DPAT}
    ret_t = [din("ret%d" % i, [64, T]) for i in range(4)]
    lbl_d = din("lbl", [2, 512])
    wA = [din("wA%d" % j, [1025, 1408]) for j in range(n_even)]
    wB = [din("wB%d" % j, [1025, 2048]) for j in range(n_even)]
    sinks = [din("sink%d" % j, [1, 8]) for j in range(n_even)]
    hnorm = [din("hnorm%d" % j, [1, 512]) for j in range(n_even)]
    woe = [din("woe%d" % j, [1024, 1024]) for j in range(n_even)]
    wC = [din("wC%d" % j, [1025, 2048]) for j in range(n_odd)]
    wDp = [[din("wD%d_%d" % (j, p), [1025, 1408]) for p in range(3)] for j in range(n_odd)]
    rnorm = [din("rnorm%d" % j, [1, 512]) for j in range(n_odd)]
    woo = [din("woo%d" % j, [1024, 1024]) for j in range(n_odd)]
    lnp = din("lnp", [NL, 4, 1024])
    rw = din("rw", [NL, 1024, 32])
    rb = din("rb", [NL, 32])
    ewgu = din("ewgu", [NL, NEXP, 1024, 2048])
    ebgu = din("ebgu", [NL, NEXP, 2048])
    ewdn = din("ewdn", [NL, NEXP, 1024, 1024])
    ebdn = din("ebdn", [NL, NEXP, 1024])
    out_d = nc.dram_tensor("out", [T, D], F32, kind="ExternalOutput").ap()

    XS = dscr("XS", [T, D])
    YMIX = dscr("YMIX", [T, D], BF16)
    UD = [dscr("UD%d" % p, [T, 520]) for p in range(3)]
    X1D = dscr("X1D", [T, D])
    X1B = dscr("X1B", [T, D], BF16)
    YE = dscr("YE", [NEXP * CAP, D])
    TOK = dscr("TOK", [NEXP * CAP, 16], I32)

    with ExitStack() as ctx:
        S = Sched(nc, ctx)

        def gsb(name, shape, dt):
            return Tile(ctx.enter_context(nc.sbuf_tensor("g_" + name, list(shape), dt)), name)

        P = [Bank(Tile(ctx.enter_context(nc.psum_tensor("ps%d" % i, [128, 512], F32)), "ps%d" % i)) for i in range(8)]

        def mm(bank, out, lhsT, rhs, reads):
            st = not bank.started
            bank.started = True
            S.op('pe', lambda e: e.matmul(out, lhsT=lhsT, rhs=rhs, start=st, stop=True, skip_group_check=True),
                 reads=reads, writes=[bank.tile])

        def tr(bank, out, in_, ident, reads):
            S.op('pe', lambda e: e.transpose(out, in_, ident), reads=reads, writes=[bank.tile])

        def tt(e, out, a, b, op, reads, writes):
            S.op(e, lambda g: g.tensor_tensor(out, a, b, op), reads=reads, writes=writes)

        def ts(e, out, a, s1, s2, op0, op1, reads, writes, **kw):
            if op1 is None:
                S.op(e, lambda g: g.tensor_scalar(out, a, s1, None, op0, **kw), reads=reads, writes=writes)
            else:
                S.op(e, lambda g: g.tensor_scalar(out, a, s1, s2, op0, op1, **kw), reads=reads, writes=writes)

        def act(out, in_, func, reads, writes, **kw):
            S.op('act', lambda g: g.activation(out, in_, func, **kw), reads=reads, writes=writes)

        cst = gsb("cst", [128, NCST], F32)
        S.dma('sp', cst[:], cst_d, writes=[cst], st=cst)
        cb = gsb("cb", [128, 896], BF16)
        S.op('dve', lambda g: g.tensor_copy(cb[:], cst[:, 0:896]), reads=[cst], writes=[cb])
        identb = cb[:, C_ID:C_ID + 128]
        identf = cst[:, C_ID:C_ID + 128]
        tokid = gsb("tokid", [128, NT, 16], I32)
        S.dma('sp', tokid[:].rearrange("p a b -> p (a b)"), tokid_d, writes=[tokid], st=tokid)
        onesb = gsb("onesb", [1, 512], BF16)
        S.op('dve', lambda g: g.memset(onesb[:], 1.0), writes=[onesb])
        onesf = gsb("onesf", [1, 128], F32)
        S.op('dve', lambda g: g.memset(onesf[:], 1.0), writes=[onesf])
        GS = gsb("GS", [128, NT, 4], I32)
        GT = gsb("GT", [128, NT, 4], F32)
        zer = gsb("zer", [128, 1024], I32)
        S.op('dve', lambda g: g.memset(zer[:], 0), writes=[zer])
        S.barrier()

        CK = [cst, cb]

        def load_xT(ph_t, rows_ap, n):
            xb = ph_t['xb'][n % 2]
            xT = ph_t['xT'][n % 2]
            S.dma('pool', xb[:], rows_ap, writes=[xb], st=xb)
            pt = P[0].tile[:].bitcast(BF16)
            for c in range(8):
                tr(P[0], pt[:, c * 128:(c + 1) * 128], xb[:, c * 128:(c + 1) * 128], identb, [xb] + CK)
            S.op('act', lambda g: g.copy(xT[:].rearrange("p a b -> p (a b)"), pt), reads=[P[0].tile], writes=[xT])
            return xT

        def xt_tiles(ph):
            return {'xb': [ph.sbuf("xb%d" % i, [128, 1024], BF16) for i in range(2)],
                    'xT': [ph.sbuf("xT%d" % i, [128, 8, 128], BF16) for i in range(2)]}

        def load_w(ph, name, wd, ncol):
            W = ph.sbuf(name, [128, 8, ncol], BF16)
            S.dma('pool', W[:], wd[0:1024, :].rearrange("(c p) n -> p c n", p=128), writes=[W], st=W)
            Wb = ph.sbuf(name + "b", [1, ncol], BF16)
            S.dma('pool', Wb[:], wd[1024:1025, :], writes=[Wb], st=Wb)
            return W, Wb

        def attn_pass(Xsrc, wd, d, mprev_off, tabs, sink_d, pat):
            nb = T // (128 * d)
            Xr = Xsrc.rearrange("(l r) n -> r l n", r=d)
            with Phase(S) as ph:
                W, Wb = load_w(ph, "aw", wd, 1408)
                xt = xt_tiles(ph)
                ct = [ph.sbuf("ct%d" % i, [64, 128], F32) for i in range(2)]
                stt_ = [ph.sbuf("st%d" % i, [64, 128], F32) for i in range(2)]
                T1 = ph.sbuf("T1", [64, 512], F32)
                T2 = ph.sbuf("T2", [64, 512], F32)
                QT = ph.sbuf("QT", [64, 8, 128], BF16)
                KT = [ph.sbuf("KT%d" % i, [64, 2, 128], BF16) for i in range(2)]
                VA = [ph.sbuf("VA%d" % i, [128, 2, 65], BF16) for i in range(2)]
                EE = [ph.sbuf("EE%d" % i, [128, 512], BF16) for i in range(4)]
                PM = [ph.sbuf("PM%d" % i, [128, 512], BF16) for i in range(4)]
                U = [ph.sbuf("U%d" % i, [128, 8, 65], F32) for i in range(2)]
                for i in range(2):
                    S.op('dve', lambda g, i=i: g.memset(VA[i][:], 1.0), writes=[VA[i]])
                if sink_d is not None:
                    esk = ph.sbuf("esk", [128, 8], F32)
                    S.dma('sp', esk[:], sink_d.to_broadcast([128, 8]), writes=[esk], st=esk)
                    act(esk[:], esk[:], AF.Exp, [esk], [esk])
                    DEN = ph.sbuf("DEN", [128, 8], F32)
                    YA = [ph.sbuf("YA%d" % i, [128, 8, 64], BF16) for i in range(2)]
                mcur = cb[:, C_MCUR:C_MCUR + 128]
                mprev = cb[:, mprev_off:mprev_off + 128]
                n = 0
                for r in range(d):
                    for b in range(nb):
                        cur, prv = n % 2, (n + 1) % 2
                        xT = load_xT(xt, Xr[r, 128 * b:128 * (b + 1), :], n)
                        c_t, s_t = ct[n % 2], stt_[n % 2]
                        S.dma('sp', c_t[:], tabs[0][:, n * 128:(n + 1) * 128], writes=[c_t], st=c_t)
                        S.dma('sp', s_t[:], tabs[1][:, n * 128:(n + 1) * 128], writes=[s_t], st=s_t)
                        for bk in P[1:7]:
                            bk.started = False
                        for h in range(8):
                            for (bq, off) in ((P[1 + h // 4], 0), (P[3 + h // 4], 512)):
                                o_ = bq.tile[0:64, (h % 4) * 128:(h % 4 + 1) * 128]
                                for c in range(8):
                                    mm(bq, o_, W[:, c, off + h * 64:off + (h + 1) * 64], xT[:, c, :], [W, xT])
                                mm(bq, o_, Wb[0:1, off + h * 64:off + (h + 1) * 64], onesb[0:1, 0:128], [Wb, onesb])
                        for kk in range(4):
                            o_ = P[5].tile[0:64, kk * 128:(kk + 1) * 128]
                            for c in range(8):
                                mm(P[5], o_, W[:, c, 1024 + kk * 64:1024 + (kk + 1) * 64], xT[:, c, :], [W, xT])
                            mm(P[5], o_, Wb[0:1, 1024 + kk * 64:1024 + (kk + 1) * 64], onesb[0:1, 0:128], [Wb, onesb])
                        for c in range(8):
                            mm(P[6], P[6].tile[:, 0:128], xT[:, c, :], W[:, c, 1280:1408], [W, xT])
                        mm(P[6], P[6].tile[:, 0:128], onesb[0:1, 0:128], Wb[0:1, 1280:1408], [Wb, onesb])
                        cbq = c_t[:].unsqueeze(1).to_broadcast([64, 4, 128])
                        sbq = s_t[:].unsqueeze(1).to_broadcast([64, 4, 128])
                        for g_ in range(2):
                            tt('dve', T1[:].rearrange("p (a b) -> p a b", a=4),
                               P[1 + g_].tile[0:64, :].rearrange("p (a b) -> p a b", a=4), cbq, ALU.mult,
                               [P[1 + g_].tile, c_t], [T1])
                            tt('dve', T2[:].rearrange("p (a b) -> p a b", a=4),
                               P[3 + g_].tile[0:64, :].rearrange("p (a b) -> p a b", a=4), sbq, ALU.mult,
                               [P[3 + g_].tile, s_t], [T2])
                            tt('dve', QT[:, 4 * g_:4 * g_ + 4, :].rearrange("p a b -> p (a b)"), T1[:], T2[:], ALU.add,
                               [T1, T2], [QT])
                        cbk = c_t[:].unsqueeze(1).to_broadcast([64, 2, 128])
                        sbk = s_t[:].unsqueeze(1).to_broadcast([64, 2, 128])
                        tt('dve', T1[:, 0:256].rearrange("p (a b) -> p a b", a=2),
                           P[5].tile[0:64, 0:256].rearrange("p (a b) -> p a b", a=2), cbk, ALU.mult,
                           [P[5].tile, c_t], [T1])
                        tt('dve', T2[:, 0:256].rearrange("p (a b) -> p a b", a=2),
                           P[5].tile[0:64, 256:512].rearrange("p (a b) -> p a b", a=2), sbk, ALU.mult,
                           [P[5].tile, s_t], [T2])
                        tt('dve', KT[cur][:].rearrange("p a b -> p (a b)"), T1[:, 0:256], T2[:, 0:256], ALU.add,
                           [T1, T2], [KT[cur]])
                        S.op('act', lambda g: g.copy(VA[cur][:, :, 0:64],
                                                     P[6].tile[:, 0:128].rearrange("p (a b) -> p a b", a=2)),
                             reads=[P[6].tile], writes=[VA[cur]])
                        for bk in P[1:7]:
                            bk.started = False
                        combos = []
                        for j in range(2):
                            mm(P[1 + j], P[1 + j].tile[:, :], KT[cur][:, j, :],
                               QT[:, 4 * j:4 * j + 4, :].rearrange("p a b -> p (a b)"), [KT[cur], QT])
                            combos.append((j, cur, P[1 + j], mcur, j))
                            if b > 0:
                                mm(P[3 + j], P[3 + j].tile[:, :], KT[prv][:, j, :],
                                   QT[:, 4 * j:4 * j + 4, :].rearrange("p a b -> p (a b)"), [KT[prv], QT])
                                combos.append((j, prv, P[3 + j], mprev, 2 + j))
                        for (j, pc, bk, msk, ei) in combos:
                            act(EE[ei][:], bk.tile[:, :], AF.Exp, [bk.tile], [EE[ei]], scale=0.125)
                            tt('dve', PM[ei][:].rearrange("p (a b) -> p a b", a=4),
                               EE[ei][:].rearrange("p (a b) -> p a b", a=4),
                               msk.unsqueeze(1).to_broadcast([128, 4, 128]), ALU.mult, [EE[ei]] + CK, [PM[ei]])
                        for (j, pc, bk, msk, ei) in combos:
                            for hh in range(4):
                                h = 4 * j + hh
                                ob = P[5 + h // 4]
                                mm(ob, ob.tile[:, (h % 4) * 65:(h % 4 + 1) * 65], PM[ei][:, hh * 128:(hh + 1) * 128],
                                   VA[pc][:, j, :], [PM[ei], VA[pc]])
                        Ut = U[n % 2]
                        for g_ in range(2):
                            S.op('act', lambda g, g_=g_: g.copy(Ut[:, 4 * g_:4 * g_ + 4, :].rearrange("p a b -> p (a b)"),
                                                                P[5 + g_].tile[:, 0:260]),
                                 reads=[P[5 + g_].tile], writes=[Ut])
                        rows = slice(128 * b, 128 * (b + 1))
                        if sink_d is not None:
                            tt('dve', DEN[:], Ut[:, :, 64], esk[:], ALU.add, [Ut, esk], [DEN])
                            S.op('dve', lambda g: g.reciprocal(DEN[:], DEN[:]), reads=[DEN], writes=[DEN])
                            ya = YA[n % 2]
                            tt('dve', ya[:], Ut[:, :, 0:64], DEN[:].unsqueeze(2).to_broadcast([128, 8, 64]), ALU.mult,
                               [Ut, DEN], [ya])
                            S.dma('sp', YMIX[rows, 0:512], ya[:].rearrange("p a b -> p (a b)"), reads=[ya], st=ya)
                        else:
                            S.dma('sp', UD[pat].rearrange("(l r) n -> r l n", r=d)[r, rows, :],
                                  Ut[:].rearrange("p a b -> p (a b)"), reads=[Ut], st=Ut)
                        n += 1

        def hgrn_pass(Xsrc, wd, j_even, hn_d):
            with Phase(S) as ph:
                W, Wb = load_w(ph, "bw", wd, 2048)
                xt = xt_tiles(ph)
                LB = ph.sbuf("LB", [128, 4], F32)
                OML = ph.sbuf("OML", [128, 4], F32)
                if j_even == 0:
                    S.op('dve', lambda g: g.memset(LB[:], 0.0), writes=[LB])
                else:
                    LL = ph.sbuf("LL", [128, 2, 4], F32)
                    S.dma('sp', LL[:], lbl_d.rearrange("l (h c) -> c l h", c=128), writes=[LL], st=LL,
                          fn=lambda q: q.dma_start(out=LL[:], in_=lbl_d.rearrange("l (h c) -> c l h", c=128),
                                                   allow_slow_non_contiguous=True))
                    tt('dve', LB[:], LL[:, 0, :], LL[:, 1, :], ALU.subtract, [LL], [LB])
                    act(LB[:], LB[:], AF.Sigmoid, [LB], [LB])
                ts('dve', OML[:], LB[:], -1.0, 1.0, ALU.mult, ALU.add, [LB], [OML])
                GB = ph.sbuf("GB", [128, 512], F32)
                S.dma('sp', GB[:], hn_d.to_broadcast([128, 512]), writes=[GB], st=GB)
                Fv = ph.sbuf("Fv", [128, 4, 128], F32)
                LF = ph.sbuf("LF", [128, 512], F32)
                KEY = ph.sbuf("KEY", [128, 512], F32)
                Bc = ph.sbuf("Bc", [128, 512], F32)
                EB = ph.sbuf("EB", [128, 512], F32)
                QTl = ph.sbuf("QTl", [128, 4, 128], BF16)
                KTl = ph.sbuf("KTl", [128, 4, 128], BF16)
                KB = ph.sbuf("KB", [128, 4, 128], BF16)
                EG = ph.sbuf("EG", [128, 16], F32)
                ATm = ph.sbuf("ATm", [128, 512], BF16)
                KBT = ph.sbuf("KBT", [128, 4, 128], BF16)
                V = ph.sbuf("V", [128, 512], BF16)
                SG = ph.sbuf("SG", [128, 512], F32)
                St = [ph.sbuf("St%d" % h, [128, 128], F32) for h in range(4)]
                Sb = [ph.sbuf("Sb%d" % h, [128, 128], BF16) for h in range(4)]
                SQ = ph.sbuf("SQ", [128, 512], F32)
                SS = ph.sbuf("SS", [128, 4], F32)
                YB = [ph.sbuf("YB%d" % i, [128, 512], BF16) for i in range(2)]
                QZ = [ph.sbuf("QZ%d" % h, [128, 4, 128], BF16) for h in range(4)]
                KZ = [ph.sbuf("KZ%d" % h, [128, 512], BF16) for h in range(4)]
                for h in range(4):
                    S.op('dve', lambda g, h=h: g.memset(St[h][:], 0.0), writes=[St[h]])
                    S.op('dve', lambda g, h=h: g.memset(Sb[h][:], 0.0), writes=[Sb[h]])
                    S.op('dve', lambda g, h=h: g.memset(QZ[h][:], 0.0), writes=[QZ[h]])
                rs = cst[:, C_RS:C_RS + 512]
                mbd = cb[:, C_MBD:C_MBD + 128]
                F2 = Fv[:].rearrange("p a b -> p (a b)")
                for i in range(NT):
                    xT = load_xT(xt, Xsrc[128 * i:128 * (i + 1), :], i)
                    for bk in P[1:8]:
                        bk.started = False
                    for h in range(4):
                        for (bk, off) in ((P[1], 0), (P[2], 512)):
                            o_ = bk.tile[:, h * 128:(h + 1) * 128]
                            for c in range(8):
                                mm(bk, o_, W[:, c, off + h * 128:off + (h + 1) * 128], xT[:, c, :], [W, xT])
                            mm(bk, o_, Wb[0:1, off + h * 128:off + (h + 1) * 128], onesb[0:1, 0:128], [Wb, onesb])
                    for (bk, off) in ((P[5], 1024), (P[6], 1536)):
                        for c in range(8):
                            mm(bk, bk.tile[:, :], xT[:, c, :], W[:, c, off:off + 512], [W, xT])
                        mm(bk, bk.tile[:, :], onesb[0:1, 0:128], Wb[0:1, off:off + 512], [Wb, onesb])
                    act(F2, P[2].tile[:, :], AF.Sigmoid, [P[2].tile], [Fv])
                    tt('dve', Fv[:], Fv[:], OML[:].unsqueeze(2).to_broadcast([128, 4, 128]), ALU.mult, [Fv, OML], [Fv])
                    tt('dve', Fv[:], Fv[:], LB[:].unsqueeze(2).to_broadcast([128, 4, 128]), ALU.add, [Fv, LB], [Fv])
                    ts('dve', F2, F2, 1e-30, None, ALU.max, None, [Fv], [Fv])
                    act(LF[:], F2, AF.Ln, [Fv], [LF])
                    ts('dve', KEY[:], F2, -1.0, 1.0, ALU.mult, ALU.add, [Fv], [KEY])
                    S.op('dve', lambda g: g.tensor_tensor_scan(Bc[:], rs, LF[:], 0.0, ALU.mult, ALU.add),
                         reads=[LF] + CK, writes=[Bc])
                    act(EB[:], Bc[:], AF.Exp, [Bc], [EB])
                    tt('dve', QTl[:].rearrange("p a b -> p (a b)"), P[1].tile[:, :], EB[:], ALU.mult, [P[1].tile, EB], [QTl])
                    act(EB[:], Bc[:], AF.Exp, [Bc], [EB], scale=-1.0)
                    tt('dve', KTl[:].rearrange("p a b -> p (a b)"), KEY[:], EB[:], ALU.mult, [KEY, EB], [KTl])
                    BL = Bc[:].rearrange("p (a b) -> p a b", b=32)[:, :, 31:32]
                    tt('dve', LF[:].rearrange("p (a b) -> p a b", b=32), BL.to_broadcast([128, 16, 32]),
                       Bc[:].rearrange("p (a b) -> p a b", b=32), ALU.subtract, [Bc], [LF])
                    act(EB[:], LF[:], AF.Exp, [LF], [EB])
                    tt('dve', KB[:].rearrange("p a b -> p (a b)"), KEY[:], EB[:], ALU.mult, [KEY, EB], [KB])
                    act(EG[:].unsqueeze(2), BL, AF.Exp, [Bc], [EG])
                    for h in range(4):
                        mm(P[3], P[3].tile[:, h * 128:(h + 1) * 128], KTl[:, h, :], QTl[:, h, :], [KTl, QTl])
                    tt('dve', ATm[:].rearrange("p (a b) -> p a b", a=4), P[3].tile[:, :].rearrange("p (a b) -> p a b", a=4),
                       mbd.unsqueeze(1).to_broadcast([128, 4, 128]), ALU.mult, [P[3].tile] + CK, [ATm])
                    p4 = P[4].tile[:].bitcast(BF16)
                    for h in range(4):
                        tr(P[4], p4[:, h * 128:(h + 1) * 128], KB[:, h, :], identb, [KB] + CK)
                    S.op('act', lambda g: g.copy(KBT[:].rearrange("p a b -> p (a b)"), p4[:, 0:512]),
                         reads=[P[4].tile], writes=[KBT])
                    S.op('act', lambda g: g.copy(V[:], P[5].tile[:, :]), reads=[P[5].tile], writes=[V])
                    act(SG[:], P[6].tile[:, :], AF.Silu, [P[6].tile], [SG])
                    tt('dve', SG[:], SG[:], GB[:], ALU.mult, [SG, GB], [SG])
                    for h in range(4):
                        mm(P[7], P[7].tile[:, h * 128:(h + 1) * 128], ATm[:, h * 128:(h + 1) * 128],
                           V[:, h * 128:(h + 1) * 128], [ATm, V])
                    for jc in range(4):
                        pr = slice(32 * jc, 32 * jc + 32)
                        S.op('dve', lambda g, jc=jc, pr=pr: g.tensor_copy(QZ[jc][:, :, pr], QTl[:, :, pr]),
                             reads=[QTl], writes=[QZ[jc]])
                        S.op('act', lambda g, jc=jc: g.activation(KZ[jc][:], KBT[:].rearrange("p a b -> p (a b)"), AF.Copy,
                                                                  scale=cst[:, C_RM + jc:C_RM + jc + 1]),
                             reads=[KBT] + CK, writes=[KZ[jc]])
                    for jc in range(4):
                        for h in range(4):
                            mm(P[7], P[7].tile[:, h * 128:(h + 1) * 128], QZ[jc][:, h, :], Sb[h][:], [QZ[jc], Sb[h]])
                        P[2].started = False
                        for h in range(4):
                            mm(P[2], P[2].tile[:, h * 128:(h + 1) * 128], KZ[jc][:, h * 128:(h + 1) * 128],
                               V[:, h * 128:(h + 1) * 128], [KZ[jc], V])
                        for h in range(4):
                            S.op('dve', lambda g, h=h: g.scalar_tensor_tensor(
                                St[h][:], St[h][:], EG[:, h * 4 + jc:h * 4 + jc + 1], P[2].tile[:, h * 128:(h + 1) * 128],
                                ALU.mult, ALU.add), reads=[St[h], EG, P[2].tile], writes=[St[h]])
                            S.op('act', lambda g, h=h: g.copy(Sb[h][:], St[h][:]), reads=[St[h]], writes=[Sb[h]])
                    act(SQ[:], P[7].tile[:, :], AF.Square, [P[7].tile], [SQ])
                    S.op('dve', lambda g: g.tensor_reduce(SS[:], SQ[:].rearrange("p (a b) -> p a b", a=4), AX.X, ALU.add),
                         reads=[SQ], writes=[SS])
                    ts('dve', SS[:], SS[:], 1.0 / 128, NORM_EPS, ALU.mult, ALU.add, [SS], [SS])
                    act(SS[:], SS[:], AF.Sqrt, [SS], [SS])
                    S.op('dve', lambda g: g.reciprocal(SS[:], SS[:]), reads=[SS], writes=[SS])
                    tt('dve', SQ[:].rearrange("p (a b) -> p a b", a=4), P[7].tile[:, :].rearrange("p (a b) -> p a b", a=4),
                       SS[:].unsqueeze(2).to_broadcast([128, 4, 128]), ALU.mult, [P[7].tile, SS], [SQ])
                    yb = YB[i % 2]
                    tt('dve', yb[:], SQ[:], SG[:], ALU.mult, [SQ, SG], [yb])
                    S.dma('sp', YMIX[128 * i:128 * (i + 1), 512:1024], yb[:], reads=[yb], st=yb)

        def ret_pass(Xsrc, wd, rn_d):
            lg = [np.log1p(-2.0 ** (-5.0 - h)) for h in range(4)]
            with Phase(S) as ph:
                W, Wb = load_w(ph, "cw", wd, 2048)
                xt = xt_tiles(ph)
                GB = ph.sbuf("GB", [128, 512], F32)
                S.dma('sp', GB[:], rn_d.to_broadcast([128, 512]), writes=[GB], st=GB)
                tb = [[ph.sbuf("tb%d_%d" % (k, i), [64, 128], F32) for i in range(2)] for k in range(4)]
                T1 = ph.sbuf("T1", [64, 512], F32)
                T2 = ph.sbuf("T2", [64, 512], F32)
                QT = ph.sbuf("QT", [64, 4, 128], BF16)
                KT = ph.sbuf("KT", [64, 4, 128], BF16)
                QH = ph.sbuf("QH", [64, 4, 128], BF16)
                KL = ph.sbuf("KL", [64, 4, 128], BF16)
                KTT = ph.sbuf("KTT", [128, 4, 64], BF16)
                SCm = ph.sbuf("SCm", [128, 512], BF16)
                V = ph.sbuf("V", [128, 512], BF16)
                SG = ph.sbuf("SG", [128, 512], F32)
                St = [ph.sbuf("St%d" % h, [64, 128], F32) for h in range(4)]
                Sb = [ph.sbuf("Sb%d" % h, [64, 128], BF16) for h in range(4)]
                OC = ph.sbuf("OC", [128, 512], F32)
                SQ = ph.sbuf("SQ", [128, 512], F32)
                SS = ph.sbuf("SS", [128, 4], F32)
                MS = ph.sbuf("MS", [128, 4], F32)
                YB = [ph.sbuf("YB%d" % i, [128, 512], BF16) for i in range(2)]
                for h in range(4):
                    S.op('dve', lambda g, h=h: g.memset(St[h][:], 0.0), writes=[St[h]])
                    S.op('dve', lambda g, h=h: g.memset(Sb[h][:], 0.0), writes=[Sb[h]])
                DT = cst[:, C_DT:C_DT + 512]
                QD = cst[0:64, C_QD:C_QD + 512]
                KD = cst[0:64, C_KD:C_KD + 512]
                for i in range(NT):
                    xT = load_xT(xt, Xsrc[128 * i:128 * (i + 1), :], i)
                    tbs = [tb[k][i % 2] for k in range(4)]
                    for k in range(4):
                        S.dma('sp', tbs[k][:], ret_t[k][:, 128 * i:128 * (i + 1)], writes=[tbs[k]], st=tbs[k])
                    for bk in P[1:8]:
                        bk.started = False
                    for h in range(4):
                        for (bk, off) in ((P[1], 0), (P[2], 256), (P[3], 512), (P[4], 768)):
                            o_ = bk.tile[0:64, h * 128:(h + 1) * 128]
                            for c in range(8):
                                mm(bk, o_, W[:, c, off + h * 64:off + (h + 1) * 64], xT[:, c, :], [W, xT])
                            mm(bk, o_, Wb[0:1, off + h * 64:off + (h + 1) * 64], onesb[0:1, 0:128], [Wb, onesb])
                    for (bk, off) in ((P[5], 1024), (P[6], 1536)):
                        for c in range(8):
                            mm(bk, bk.tile[:, :], xT[:, c, :], W[:, c, off:off + 512], [W, xT])
                        mm(bk, bk.tile[:, :], onesb[0:1, 0:128], Wb[0:1, off:off + 512], [Wb, onesb])
                    for (dst, pa, pb, ta, tb_) in ((QT, P[1], P[2], tbs[0], tbs[1]), (KT, P[3], P[4], tbs[2], tbs[3])):
                        tt('dve', T1[:].rearrange("p (a b) -> p a b", a=4), pa.tile[0:64, :].rearrange("p (a b) -> p a b", a=4),
                           ta[:].unsqueeze(1).to_broadcast([64, 4, 128]), ALU.mult, [pa.tile, ta], [T1])
                        tt('dve', T2[:].rearrange("p (a b) -> p a b", a=4), pb.tile[0:64, :].rearrange("p (a b) -> p a b", a=4),
                           tb_[:].unsqueeze(1).to_broadcast([64, 4, 128]), ALU.mult, [pb.tile, tb_], [T2])
                        tt('dve', dst[:].rearrange("p a b -> p (a b)"), T1[:], T2[:], ALU.add, [T1, T2], [dst])
                    tt('dve', QH[:].rearrange("p a b -> p (a b)"), QT[:].rearrange("p a b -> p (a b)"), QD, ALU.mult,
                       [QT] + CK, [QH])
                    tt('dve', KL[:].rearrange("p a b -> p (a b)"), KT[:].rearrange("p a b -> p (a b)"), KD, ALU.mult,
                       [KT] + CK, [KL])
                    S.op('act', lambda g: g.copy(V[:], P[5].tile[:, :]), reads=[P[5].tile], writes=[V])
                    act(SG[:], P[6].tile[:, :], AF.Silu, [P[6].tile], [SG])
                    tt('dve', SG[:], SG[:], GB[:], ALU.mult, [SG, GB], [SG])
                    P[1].started = False
                    for h in range(4):
                        mm(P[1], P[1].tile[:, h * 128:(h + 1) * 128], KT[:, h, :], QT[:, h, :], [KT, QT])
                    tt('dve', SCm[:], P[1].tile[:, :], DT, ALU.mult, [P[1].tile] + CK, [SCm])
                    p3 = P[3].tile[:].bitcast(BF16)
                    for h in range(4):
                        tr(P[3], p3[:, h * 64:(h + 1) * 64], KL[:, h, :], cb[0:64, C_ID:C_ID + 64], [KL] + CK)
                    S.op('act', lambda g: g.copy(KTT[:].rearrange("p a b -> p (a b)"), p3[:, 0:256]),
                         reads=[P[3].tile], writes=[KTT])
                    for h in range(4):
                        mm(P[7], P[7].tile[:, h * 128:(h + 1) * 128], SCm[:, h * 128:(h + 1) * 128],
                           V[:, h * 128:(h + 1) * 128], [SCm, V])
                    for h in range(4):
                        mm(P[7], P[7].tile[:, h * 128:(h + 1) * 128], QH[:, h, :], Sb[h][:], [QH, Sb[h]])
                    P[2].started = False
                    for h in range(4):
                        mm(P[2], P[2].tile[0:64, h * 128:(h + 1) * 128], KTT[:, h, :], V[:, h * 128:(h + 1) * 128], [KTT, V])
                    for h in range(4):
                        cdh = float(np.exp(lg[h] * 128))
                        S.op('dve', lambda g, h=h, cdh=cdh: g.scalar_tensor_tensor(
                            St[h][:], St[h][:], cdh, P[2].tile[0:64, h * 128:(h + 1) * 128], ALU.mult, ALU.add),
                            reads=[St[h], P[2].tile], writes=[St[h]])
                        S.op('act', lambda g, h=h: g.copy(Sb[h][:], St[h][:]), reads=[St[h]], writes=[Sb[h]])
                    p7 = P[7].tile[:, :].rearrange("p (a b) -> p a b", a=4)
                    S.op('dve', lambda g: g.tensor_reduce(MS[:], p7, AX.X, ALU.add), reads=[P[7].tile], writes=[MS])
                    ts('dve', MS[:], MS[:], -1.0 / 128, None, ALU.mult, None, [MS], [MS])
                    tt('dve', OC[:].rearrange("p (a b) -> p a b", a=4), p7, MS[:].unsqueeze(2).to_broadcast([128, 4, 128]),
                       ALU.add, [P[7].tile, MS], [OC])
                    act(SQ[:], OC[:], AF.Square, [OC], [SQ])
                    S.op('dve', lambda g: g.tensor_reduce(SS[:], SQ[:].rearrange("p (a b) -> p a b", a=4), AX.X, ALU.add),
                         reads=[SQ], writes=[SS])
                    ts('dve', SS[:], SS[:], 1.0 / 128, NORM_EPS, ALU.mult, ALU.add, [SS], [SS])
                    act(SS[:], SS[:], AF.Sqrt, [SS], [SS])
                    S.op('dve', lambda g: g.reciprocal(SS[:], SS[:]), reads=[SS], writes=[SS])
                    tt('dve', SQ[:].rearrange("p (a b) -> p a b", a=4), OC[:].rearrange("p (a b) -> p a b", a=4),
                       SS[:].unsqueeze(2).to_broadcast([128, 4, 128]), ALU.mult, [OC, SS], [SQ])
                    yb = YB[i % 2]
                    tt('dve', yb[:], SQ[:], SG[:], ALU.mult, [SQ, SG], [yb])
                    S.dma('sp', YMIX[128 * i:128 * (i + 1), 0:512], yb[:], reads=[yb], st=yb)

        def layer_norm(ph_t, Z, OUT, G, Bt):
            ST, MV, RS_ = ph_t['ST'], ph_t['MV'], ph_t['RS']
            for k in range(2):
                S.op('dve', lambda g, k=k: g.bn_stats(ST[:, k, :], Z[:, k * 512:(k + 1) * 512]), reads=[Z], writes=[ST])
            S.op('dve', lambda g: g.bn_aggr(MV[:], ST[:].rearrange("p a b -> p (a b)")), reads=[ST], writes=[MV])
            ts('dve', RS_[:], MV[:, 1:2], LN_EPS, None, ALU.add, None, [MV], [RS_])
            act(RS_[:], RS_[:], AF.Sqrt, [RS_], [RS_])
            S.op('dve', lambda g: g.reciprocal(RS_[:], RS_[:]), reads=[RS_], writes=[RS_])
            ts('dve', OUT[:], Z[:], MV[:, 0:1], RS_[:, 0:1], ALU.subtract, ALU.mult, [Z, MV, RS_], [OUT])
            tt('dve', OUT[:], OUT[:], G[:], ALU.mult, [OUT, G], [OUT])
            tt('dve', OUT[:], OUT[:], Bt[:], ALU.add, [OUT, Bt], [OUT])

        def ln_tiles(ph):
            return {'ST': ph.sbuf("lnST", [128, 2, 6], F32), 'MV': ph.sbuf("lnMV", [128, 2], F32),
                    'RS': ph.sbuf("lnRS", [128, 1], F32)}

        def oproj_phase(Xsrc, L, kind, wo_d):
            with Phase(S) as ph:
                Wo = ph.sbuf("Wo", [128, 8, 1024], BF16)
                S.dma('pool', Wo[:], wo_d.rearrange("(c p) n -> p c n", p=128), writes=[Wo], st=Wo)
                G = ph.sbuf("G", [128, 1024], F32)
                Bt = ph.sbuf("Bt", [128, 1024], F32)
                S.dma('sp', G[:], lnp[L, 0:1, :].to_broadcast([128, 1024]), writes=[G], st=G)
                S.dma('sp', Bt[:], lnp[L, 1:2, :].to_broadcast([128, 1024]), writes=[Bt], st=Bt)
                Wr = ph.sbuf("Wr", [128, 8, 32], F32)
                S.dma('sp', Wr[:], rw[L].rearrange("(c p) n -> p c n", p=128), writes=[Wr], st=Wr)
                rbt = ph.sbuf("rbt", [1, 32], F32)
                S.dma('sp', rbt[:], rb[L:L + 1, :], writes=[rbt], st=rbt)
                lt = ln_tiles(ph)
                ym = [ph.sbuf("ym%d" % i, [128, 1024], BF16) for i in range(2)]
                yT = ph.sbuf("yT", [128, 8, 128], BF16)
                xt_ = [ph.sbuf("xt%d" % i, [128, 1024], F32) for i in range(2)]
                Z = ph.sbuf("Z", [128, 1024], F32)
                x1 = [ph.sbuf("x1_%d" % i, [128, 1024], F32) for i in range(2)]
                x1T = ph.sbuf("x1T", [128, 8, 128], F32)
                LG = ph.sbuf("LG", [128, 32], F32)
                MX = ph.sbuf("MX", [128, 8], F32)
                MI = ph.sbuf("MI", [128, 8], U32)
                MIF = ph.sbuf("MIF", [128, 4], F32)
                NM = ph.sbuf("NM", [128, 1], F32)
                EX = ph.sbuf("EX", [128, 4], F32)
                SM = ph.sbuf("SM", [128, 1], F32)
                MK = ph.sbuf("MK", [128, 32], BF16)
                SLOT = ph.sbuf("SLOT", [128, 32], F32)
                CNT = ph.sbuf("CNT", [128, 32], F32)
                TMP = ph.sbuf("TMP", [128, 32], F32)
                SK = ph.sbuf("SK", [128, 4], F32)
                GSF = ph.sbuf("GSF", [128, 4], F32)
                S.op('dve', lambda g: g.memset(CNT[:], 0.0), writes=[CNT])
                if kind == 'o':
                    Uu = [[ph.sbuf("Uu%d_%d" % (p, i), [128, 8, 65], F32) for i in range(2)] for p in range(3)]
                    RD = ph.sbuf("RD", [128, 8], F32)
                iota32 = cst[:, C_IOTA:C_IOTA + 32]
                tri = cb[:, C_TRI:C_TRI + 128]
                ones = cb[:, C_ONE:C_ONE + 128]
                scat = [ph.sbuf("scat%d" % k, [128, 1], I32) for k in range(4)]
                for i in range(NT):
                    rows = slice(128 * i, 128 * (i + 1))
                    y = ym[i % 2]
                    if kind == 'e':
                        S.dma('sp', y[:], YMIX[rows, :], writes=[y], st=y)
                    else:
                        S.dma('sp', y[:, 0:512], YMIX[rows, 0:512], writes=[y], st=y)
                        us = [Uu[p][i % 2] for p in range(3)]
                        for p in range(3):
                            S.dma('sp', us[p][:].rearrange("p a b -> p (a b)"), UD[p][rows, :], writes=[us[p]], st=us[p])
                        tt('dve', us[0][:], us[0][:], us[1][:], ALU.add, [us[0], us[1]], [us[0]])
                        tt('dve', us[0][:], us[0][:], us[2][:], ALU.add, [us[0], us[2]], [us[0]])
                        S.op('dve', lambda g: g.reciprocal(RD[:], us[0][:, :, 64]), reads=[us[0]], writes=[RD])
                        tt('dve', y[:, 512:1024].rearrange("p (a b) -> p a b", a=8), us[0][:, :, 0:64],
                           RD[:].unsqueeze(2).to_broadcast([128, 8, 64]), ALU.mult, [us[0], RD], [y])
                    pt = P[0].tile[:].bitcast(BF16)
                    for c in range(8):
                        tr(P[0], pt[:, c * 128:(c + 1) * 128], y[:, c * 128:(c + 1) * 128], identb, [y] + CK)
                    S.op('act', lambda g: g.copy(yT[:].rearrange("p a b -> p (a b)"), pt), reads=[P[0].tile], writes=[yT])
                    for bk in P[1:8]:
                        bk.started = False
                    for k in range(2):
                        for c in range(8):
                            mm(P[1 + k], P[1 + k].tile[:, :], yT[:, c, :], Wo[:, c, k * 512:(k + 1) * 512], [yT, Wo])
                    xx = xt_[i % 2]
                    S.dma('sp', xx[:], Xsrc[rows, :], writes=[xx], st=xx)
                    for k in range(2):
                        S.op('dve', lambda g, k=k: g.scalar_tensor_tensor(
                            Z[:, k * 512:(k + 1) * 512], xx[:, k * 512:(k + 1) * 512], DN_ALPHA, P[1 + k].tile[:, :],
                            ALU.mult, ALU.add), reads=[xx, P[1 + k].tile], writes=[Z])
                    xo = x1[i % 2]
                    layer_norm(lt, Z, xo, G, Bt)
                    S.dma('sp', X1D[rows, :], xo[:], reads=[xo], st=xo)
                    S.dma('pool', X1B[rows, :], xo[:], reads=[xo], st=xo)
                    for c in range(8):
                        bk = P[3 + c // 4]
                        tr(bk, bk.tile[:, (c % 4) * 128:(c % 4 + 1) * 128], xo[:, c * 128:(c + 1) * 128], identf, [xo] + CK)
                    for k in range(2):
                        S.op('act', lambda g, k=k: g.copy(x1T[:, 4 * k:4 * k + 4, :].rearrange("p a b -> p (a b)"),
                                                          P[3 + k].tile[:, :]), reads=[P[3 + k].tile], writes=[x1T])
                    for c in range(8):
                        mm(P[5], P[5].tile[:, 0:32], x1T[:, c, :], Wr[:, c, :], [x1T, Wr])
                    mm(P[5], P[5].tile[:, 0:32], onesf[0:1, :], rbt[0:1, :], [onesf, rbt])
                    S.op('dve', lambda g: g.tensor_copy(LG[:], P[5].tile[:, 0:32]), reads=[P[5].tile], writes=[LG])
                    S.op('dve', lambda g: g.max(MX[:], LG[:]), reads=[LG], writes=[MX])
                    S.op('dve', lambda g: g.max_index(MI[:], MX[:], LG[:]), reads=[LG, MX], writes=[MI])
                    ts('dve', NM[:], MX[:, 0:1], -1.0, None, ALU.mult, None, [MX], [NM])
                    act(EX[:], MX[:, 0:4], AF.Exp, [MX, NM], [EX, SM], bias=NM[:, 0:1], accum_out=SM[:, 0:1])
                    S.op('dve', lambda g: g.reciprocal(SM[:], SM[:]), reads=[SM], writes=[SM])
                    ts('dve', GT[:, i, :], EX[:], SM[:, 0:1], None, ALU.mult, None, [EX, SM], [('GT', i)])
                    ts('dve', MK[:], LG[:], MX[:, 3:4], None, ALU.is_ge, None, [LG, MX], [MK])
                    mm(P[6], P[6].tile[:, 0:32], tri, MK[:], [MK] + CK)
                    mm(P[6], P[6].tile[:, 32:64], ones, MK[:], [MK] + CK)
                    tt('dve', SLOT[:], P[6].tile[:, 0:32], CNT[:], ALU.add, [P[6].tile, CNT], [SLOT])
                    tt('dve', CNT[:], CNT[:], P[6].tile[:, 32:64], ALU.add, [P[6].tile, CNT], [CNT])
                    S.op('dve', lambda g: g.tensor_copy(MIF[:], MI[:, 0:4]), reads=[MI], writes=[MIF])
                    for k in range(4):
                        S.op('dve', lambda g, k=k: g.scalar_tensor_tensor(
                            TMP[:], iota32, MIF[:, k:k + 1], SLOT[:], ALU.is_equal, ALU.mult, accum_out=SK[:, k:k + 1]),
                            reads=[MIF, SLOT] + CK, writes=[TMP, SK])
                    ts('dve', SK[:], SK[:], float(CAP - 1), None, ALU.min, None, [SK], [SK])
                    S.op('dve', lambda g: g.scalar_tensor_tensor(GSF[:], MIF[:], float(CAP), SK[:], ALU.mult, ALU.add),
                         reads=[MIF, SK], writes=[GSF])
                    S.op('dve', lambda g: g.tensor_copy(GS[:, i, :], GSF[:]), reads=[GSF], writes=[('GS', i)])
                    for k in range(4):
                        S.dma('pool', None, None, reads=[('GS', i)], st=scat[k],
                              fn=lambda q, k=k: q.indirect_dma_start(
                                  out=TOK, out_offset=bass.IndirectOffsetOnAxis(ap=GS[:, i, k:k + 1], axis=0),
                                  in_=tokid[:, i, :], in_offset=None))

        def expert_phase(L):
            NS = CAP // 128
            groups = [(g0, min(512, CAP - g0)) for g0 in range(0, CAP, 512)]
            with Phase(S) as ph:
                WG = [ph.sbuf("WG%d" % i, [128, 8, 2048], BF16) for i in range(2)]
                WD = [ph.sbuf("WD%d" % i, [128, 8, 1024], BF16) for i in range(2)]
                BDN = [ph.sbuf("BDN%d" % i, [128, 1024], F32) for i in range(2)]
                BGU = ph.sbuf("BGU", [128, 512], F32)
                BGT = [ph.sbuf("BGT%d" % i, [128, 128], F32) for i in range(4)]
                bsrc = ebgu[L].rearrange("e (c p) -> (e c) p", p=128)
                P[7].started = False
                for q4 in range(4):
                    S.dma('sp', BGT[q4][:], bsrc[q4 * 128:(q4 + 1) * 128, :], writes=[BGT[q4]], st=BGT[q4])
                    tr(P[7], P[7].tile[:, q4 * 128:(q4 + 1) * 128], BGT[q4][:], identf, [BGT[q4]] + CK)
                S.op('dve', lambda g: g.tensor_copy(BGU[:], P[7].tile[:, :]), reads=[P[7].tile], writes=[BGU])
                bv = BGU[:].rearrange("p (e c) -> p e c", c=16)
                ts('dve', bv[:, :, 8:16], bv[:, :, 8:16], 1.0, None, ALU.add, None, [BGU], [BGU])
                IDX = [ph.sbuf("IDX%d" % i, [128, 16], I32) for i in range(2)]
                XG = [ph.sbuf("XG%d" % i, [128, 1024], BF16) for i in range(2)]
                XGT = ph.sbuf("XGT", [128, 8, CAP], BF16)
                AT = [ph.sbuf("AT%d" % i, [128, 8, 512], BF16) for i in range(2)]
                GC = ph.sbuf("GC", [128, 512], F32)
                SGt = ph.sbuf("SGt", [128, 512], F32)
                UC = ph.sbuf("UC", [128, 512], F32)
                YO = [ph.sbuf("YO%d" % i, [128, 1024], F32) for i in range(2)]
                n = 0
                gi = 0
                for e in range(NEXP):
                    wg, wdn, bdn = WG[e % 2], WD[e % 2], BDN[e % 2]
                    S.dma('pool', wg[:], ewgu[L, e].rearrange("(c p) n -> p c n", p=128), writes=[wg], st=wg)
                    S.dma('pool', wdn[:], ewdn[L, e].rearrange("(c p) n -> p c n", p=128), writes=[wdn], st=wdn)
                    S.dma('sp', bdn[:], ebdn[L, e:e + 1, :].to_broadcast([128, 1024]), writes=[bdn], st=bdn)
                    for j in range(NS):
                        ix, xg = IDX[n % 2], XG[n % 2]
                        r0 = e * CAP + j * 128
                        S.dma('sp', ix[:], TOK[r0:r0 + 128, :], writes=[ix], st=ix)
                        S.dma('pool', None, None, reads=[ix], writes=[xg], st=xg,
                              fn=lambda q, ix=ix, xg=xg: q.indirect_dma_start(
                                  out=xg[:], out_offset=None, in_=X1B,
                                  in_offset=bass.IndirectOffsetOnAxis(ap=ix[:, 0:1], axis=0)))
                        pt = P[0].tile[:].bitcast(BF16)
                        for c in range(8):
                            tr(P[0], pt[:, c * 128:(c + 1) * 128], xg[:, c * 128:(c + 1) * 128], identb, [xg] + CK)
                        S.op('act', lambda g, j=j: g.copy(XGT[:, :, j * 128:(j + 1) * 128],
                                                          pt.rearrange("p (a b) -> p a b", a=8)),
                             reads=[P[0].tile], writes=[XGT])
                        n += 1
                    for (g0, gn) in groups:
                        at = AT[gi % 2]
                        gi += 1
                        for fc in range(8):
                            bg, bu = P[1 + (fc % 2) * 2], P[2 + (fc % 2) * 2]
                            bg.started = False
                            bu.started = False
                            for (bk, f0) in ((bg, fc * 128), (bu, 1024 + fc * 128)):
                                for c in range(8):
                                    mm(bk, bk.tile[:, 0:gn], wg[:, c, f0:f0 + 128], XGT[:, c, g0:g0 + gn], [wg, XGT])
                            ts('dve', GC[:, 0:gn], bg.tile[:, 0:gn], BGU[:, e * 16 + fc:e * 16 + fc + 1], 7.0, ALU.add, ALU.min,
                               [bg.tile, BGU], [GC])
                            act(SGt[:, 0:gn], GC[:, 0:gn], AF.Sigmoid, [GC], [SGt], scale=1.702)
                            tt('pool', GC[:, 0:gn], GC[:, 0:gn], SGt[:, 0:gn], ALU.mult, [GC, SGt], [GC])
                            ts('dve', UC[:, 0:gn], bu.tile[:, 0:gn], BGU[:, e * 16 + 8 + fc:e * 16 + 8 + fc + 1], -6.0,
                               ALU.add, ALU.max, [bu.tile, BGU], [UC])
                            S.op('dve', lambda g, fc=fc, at=at: g.scalar_tensor_tensor(
                                at[:, fc, 0:gn], UC[:, 0:gn], 8.0, GC[:, 0:gn], ALU.min, ALU.mult),
                                reads=[UC, GC], writes=[at])
                        for s0 in range(0, gn, 128):
                            yo = YO[n % 2]
                            n += 1
                            for k in range(2):
                                bk = P[5 + k]
                                bk.started = False
                                for c in range(8):
                                    mm(bk, bk.tile[:, :], at[:, c, s0:s0 + 128], wdn[:, c, k * 512:(k + 1) * 512], [at, wdn])
                                tt('dve', yo[:, k * 512:(k + 1) * 512], bk.tile[:, :], bdn[:, k * 512:(k + 1) * 512], ALU.add,
                                   [bk.tile, bdn], [yo])
                            r0 = e * CAP + g0 + s0
                            S.dma('sp', YE[r0:r0 + 128, :], yo[:], reads=[yo], st=yo)

        def combine_phase(L, Xdst):
            with Phase(S) as ph:
                G = ph.sbuf("G", [128, 1024], F32)
                Bt = ph.sbuf("Bt", [128, 1024], F32)
                S.dma('sp', G[:], lnp[L, 2:3, :].to_broadcast([128, 1024]), writes=[G], st=G)
                S.dma('sp', Bt[:], lnp[L, 3:4, :].to_broadcast([128, 1024]), writes=[Bt], st=Bt)
                lt = ln_tiles(ph)
                YK = [[ph.sbuf("YK%d_%d" % (k, i), [128, 1024], F32) for i in range(2)] for k in range(4)]
                ACC = ph.sbuf("ACC", [128, 1024], F32)
                xt_ = [ph.sbuf("xt%d" % i, [128, 1024], F32) for i in range(2)]
                XN = [ph.sbuf("XN%d" % i, [128, 1024], F32) for i in range(2)]
                for i in range(NT):
                    rows = slice(128 * i, 128 * (i + 1))
                    yk = [YK[k][i % 2] for k in range(4)]
                    for k in range(4):
                        S.dma('pool', None, None, reads=[('GS', i)], writes=[yk[k]], st=yk[k],
                              fn=lambda q, k=k: q.indirect_dma_start(
                                  out=yk[k][:], out_offset=None, in_=YE,
                                  in_offset=bass.IndirectOffsetOnAxis(ap=GS[:, i, k:k + 1], axis=0)))
                    xx = xt_[i % 2]
                    S.dma('sp', xx[:], X1D[rows, :], writes=[xx], st=xx)
                    ts('dve', ACC[:], yk[0][:], GT[:, i, 0:1], None, ALU.mult, None, [yk[0], ('GT', i)], [ACC])
                    for k in range(1, 4):
                        S.op('dve', lambda g, k=k: g.scalar_tensor_tensor(
                            ACC[:], yk[k][:], GT[:, i, k:k + 1], ACC[:], ALU.mult, ALU.add),
                            reads=[yk[k], ('GT', i), ACC], writes=[ACC])
                    S.op('dve', lambda g: g.scalar_tensor_tensor(ACC[:], xx[:], DN_ALPHA, ACC[:], ALU.mult, ALU.add),
                         reads=[xx, ACC], writes=[ACC])
                    xn = XN[i % 2]
                    layer_norm(lt, ACC, xn, G, Bt)
                    S.dma('sp', Xdst[rows, :], xn[:], reads=[xn], st=xn)

        def zero_tok():
            tk = TOK.rearrange("(p a) b -> p (a b)", p=128)
            ncols = NEXP * CAP * 16 // 128
            for c0 in range(0, ncols, 1024):
                cn = min(1024, ncols - c0)
                S.dma('sp', tk[:, c0:c0 + cn], zer[:, 0:cn], reads=[zer], st=zer)

        je = jo = 0
        Xcur = x_in
        for L, kind in enumerate(layer_kinds):
            zero_tok()
            if kind == 'e':
                attn_pass(Xcur, wA[je], 1, C_MPA, ropeA[1], sinks[je], None)
                hgrn_pass(Xcur, wB[je], je, hnorm[je])
                oproj_phase(Xcur, L, 'e', woe[je])
                je += 1
            else:
                ret_pass(Xcur, wC[jo], rnorm[jo])
                for p, d in enumerate(DPAT):
                    attn_pass(Xcur, wDp[jo][p], d, C_MPD, ropeA[d], None, p)
                oproj_phase(Xcur, L, 'o', woo[jo])
                jo += 1
            expert_phase(L)
            Xdst = out_d if L == NL - 1 else XS
            combine_phase(L, Xdst)
            Xcur = XS
        S.barrier()
        print("bass program: ninst=%d" % S.ninst)
    return nc


def _rot_perm(nheads, hd, half):
    idx = np.arange(nheads * hd).reshape(nheads, hd).copy()
    for h in range(nheads):
        base = h * hd
        idx[h, :half] = base + np.arange(half, 2 * half)
        idx[h, half:2 * half] = base + np.arange(0, half)
    return idx.reshape(-1)


def _consts(T):
    NT = T // 128
    cst = np.zeros((128, NCST), np.float32)
    i = np.arange(128)
    cst[:, C_ID:C_ID + 128] = np.eye(128)
    kj, qi = i[:, None], i[None, :]
    cst[:, C_MCUR:C_MCUR + 128] = (qi >= kj)
    cst[:, C_MPA:C_MPA + 128] = (kj >= qi + 1)
    cst[:, C_MPD:C_MPD + 128] = (kj >= qi)
    cst[:, C_MBD:C_MBD + 128] = ((kj // 32) == (qi // 32)) & (kj <= qi)
    cst[:, C_TRI:C_TRI + 128] = (kj < qi)
    cst[:, C_ONE:C_ONE + 128] = 1.0
    cst[:, C_IOTA:C_IOTA + 32] = np.arange(32)[None, :]
    rs = np.ones(512)
    rs[::32] = 0.0
    cst[:, C_RS:C_RS + 512] = rs[None, :]
    lg = np.log1p(-np.exp2(-5.0 - np.arange(4, dtype=np.float64)))
    for h in range(4):
        rel = (qi - kj).astype(np.float64)
        cst[:, C_DT + h * 128:C_DT + (h + 1) * 128] = np.where(rel >= 0, np.exp(lg[h] * np.maximum(rel, 0)), 0.0)
        cst[:64, C_QD + h * 128:C_QD + (h + 1) * 128] = np.exp(lg[h] * (i + 1.0))[None, :]
        cst[:64, C_KD + h * 128:C_KD + (h + 1) * 128] = np.exp(lg[h] * (127.0 - i))[None, :]
    for jc in range(4):
        cst[:, C_RM + jc] = (i // 32 == jc)
    tokid = np.zeros((128, NT, 16), np.int32)
    tokid[:] = (np.arange(NT)[None, :, None] * 128 + np.arange(128)[:, None, None])
    pos = np.arange(T, dtype=np.float32)
    inv = (1.0 / (np.float32(500000.0) ** (np.arange(0, 16, 2, dtype=np.float32) / np.float32(16)))).astype(np.float32)
    ang = (pos[:, None] * inv[None, :]).astype(np.float32).astype(np.float64)
    Cn = np.ones((64, T))
    Sn = np.zeros((64, T))
    Cn[0:8] = np.cos(ang).T
    Cn[8:16] = np.cos(ang).T
    Sn[0:8] = -np.sin(ang).T
    Sn[8:16] = np.sin(ang).T
    rope = {}
    for d in DPAT:
        nb = T // (128 * d)
        perm = np.concatenate([r + d * (128 * b + np.arange(128)) for r in range(d) for b in range(nb)])
        rope[d] = (np.ascontiguousarray(Cn[:, perm], dtype=np.float32), np.ascontiguousarray(Sn[:, perm], dtype=np.float32))
    rinv = (1.0 / (np.float32(10000.0) ** np.linspace(0.0, 1.0, 32, dtype=np.float32))).astype(np.float32)
    rang = (pos[:, None] * rinv[None, :]).astype(np.float32).astype(np.float64)
    RC = np.concatenate([np.cos(rang).T, np.cos(rang).T], 0)
    RSn = np.concatenate([-np.sin(rang).T, np.sin(rang).T], 0)
    ret = [RC, RSn, RC * 0.125, RSn * 0.125]
    ret = [np.ascontiguousarray(a, dtype=np.float32) for a in ret]
    return cst, tokid.reshape(128, NT * 16), rope, ret


def _prep_shared(inp, layer_kinds, T):
    f = lambda a: np.ascontiguousarray(np.asarray(a), dtype=np.float32)
    cst, tokid, rope, ret = _consts(T)
    m = {"cst": cst, "tokid": tokid}
    for d in DPAT:
        m["ropeC%d" % d], m["ropeS%d" % d] = rope[d]
    for i in range(4):
        m["ret%d" % i] = ret[i]
    m["lbl"] = f(inp["hgrn_lb_logits"])[:2]
    pq = _rot_perm(8, 64, 8)
    pk = _rot_perm(2, 64, 8)
    pc = _rot_perm(4, 64, 32)
    NL = len(layer_kinds)
    je = jo = 0
    for L, k in enumerate(layer_kinds):
        if k == 'e':
            w = np.concatenate([f(inp["w_in_even"])[je], f(inp["b_in_even"])[je][None, :]], 0)
            q, kk, v = w[:, 0:512], w[:, 512:640], w[:, 640:768]
            m["wA%d" % je] = np.ascontiguousarray(np.concatenate([q, q[:, pq], kk, kk[:, pk], v], 1))
            m["wB%d" % je] = np.ascontiguousarray(w[:, 768:2816])
            m["sink%d" % je] = f(inp["attn_sinks"])[je][None, :]
            m["hnorm%d" % je] = f(inp["hgrn_norm"])[je][None, :]
            m["woe%d" % je] = f(inp["w_out_even"])[je]
            je += 1
        else:
            w = np.concatenate([f(inp["w_in_odd"])[jo], f(inp["b_in_odd"])[jo][None, :]], 0)
            cq, ck, cv, cg = w[:, 0:256], w[:, 256:512], w[:, 512:1024], w[:, 1024:1536]
            m["wC%d" % jo] = np.ascontiguousarray(np.concatenate([cq, cq[:, pc], ck, ck[:, pc], cv, cg], 1))
            for p in range(3):
                o = 1536 + p * 768
                q, kk, v = w[:, o:o + 512], w[:, o + 512:o + 640], w[:, o + 640:o + 768]
                m["wD%d_%d" % (jo, p)] = np.ascontiguousarray(np.concatenate([q, q[:, pq], kk, kk[:, pk], v], 1))
            m["rnorm%d" % jo] = f(inp["ret_norm"])[jo][None, :]
            m["woo%d" % jo] = f(inp["w_out_odd"])[jo]
            jo += 1
    m["lnp"] = np.ascontiguousarray(np.stack([f(inp["ln1_g"])[:NL], f(inp["ln1_b"])[:NL], f(inp["ln2_g"])[:NL],
                                              f(inp["ln2_b"])[:NL]], 1))
    m["rw"] = f(inp["router_w"])[:NL]
    m["rb"] = f(inp["router_b"])[:NL]
    m["ewgu"] = f(inp["expert_w_gu"])[:NL]
    m["ebgu"] = f(inp["expert_b_gu"])[:NL]
    m["ewdn"] = f(inp["expert_w_dn"])[:NL]
    m["ebdn"] = f(inp["expert_b_dn"])[:NL]
    return m


def run(inp, layer_kinds, CAP, debug=False):
    x = np.ascontiguousarray(np.asarray(inp["x"]), dtype=np.float32)
    B, T, _ = x.shape
    nc = build(T, layer_kinds, CAP, debug=debug)
    shared = _prep_shared(inp, layer_kinds, T)
    in_maps = []
    for b in range(B):
        mm_ = dict(shared)
        mm_["x"] = x[b]
        in_maps.append(mm_)
    res = run_bass_kernel_spmd(nc, in_maps, core_ids=list(range(B)))
    return res


def kernel(**inputs):
    res = run(inputs, ['e', 'o', 'e', 'o'], 1280)
    return np.stack([np.asarray(r["out"]) for r in res.results], 0).astype(np.float32)
```

```python
import numpy as np
from contextlib import ExitStack
import concourse.bass as bass
import concourse.mybir as mybir
from concourse.bass_utils import run_bass_kernel_spmd

F32 = mybir.dt.float32
BF16 = mybir.dt.bfloat16
I32 = mybir.dt.int32
U32 = mybir.dt.uint32
AF = mybir.ActivationFunctionType
ALU = mybir.AluOpType
AX = mybir.AxisListType

D = 1024
NEXP = 32
DN_ALPHA = 8 ** 0.25
LN_EPS = 1e-5
NORM_EPS = 1e-6
DPAT = (1, 4, 16)

C_ID, C_MCUR, C_MPA, C_MPD, C_MBD, C_TRI, C_ONE = 0, 128, 256, 384, 512, 640, 768
C_IOTA, C_RS, C_DT, C_QD, C_KD = 896, 928, 1440, 1952, 2464
C_RM = 2976
NCST = 2980


class DSem:
    def __init__(self, sem):
        self.sem = sem
        self.cnt = 0


class Tile:
    def __init__(self, t, name):
        self.t = t
        self.name = name
        self.ds = None

    def __getitem__(self, idx):
        return self.t[idx]


class Sched:
    def __init__(self, nc, ctx, ndsem=64):
        self.nc = nc
        self.ctx = ctx
        self.eng = {'pe': nc.tensor, 'act': nc.scalar, 'dve': nc.vector, 'pool': nc.gpsimd, 'sp': nc.sync}
        self.esem = {}
        self.ecnt = {}
        for e in ('pe', 'act', 'dve', 'pool'):
            self.esem[e] = ctx.enter_context(nc.semaphore('s_' + e))
            self.ecnt[e] = 0
        self.seen = {e: {} for e in self.eng}
        self.writers = {}
        self.readers = {}
        self.ninst = 0
        self.free_ds = [DSem(ctx.enter_context(nc.semaphore('d%d' % i))) for i in range(ndsem)]
        self.all_ds = list(self.free_ds)

    def _deps(self, reads, writes):
        need = {}
        for k in list(reads) + list(writes):
            for s, v in self.writers.get(k, {}).items():
                if need.get(s, (None, 0))[1] < v[1]:
                    need[s] = v
        for k in writes:
            for s, v in self.readers.get(k, {}).items():
                if need.get(s, (None, 0))[1] < v[1]:
                    need[s] = v
        return need

    def _wait(self, e, need, skip=None):
        eng = self.eng[e]
        seen = self.seen[e]
        for s, (sem, val) in need.items():
            if s == skip or seen.get(s, 0) >= val:
                continue
            eng.wait_ge(sem, val)
            seen[s] = val
            self.ninst += 1

    def _commit(self, ev, reads, writes):
        s = id(ev[0])
        for k in writes:
            self.writers[k] = {s: ev}
            self.readers[k] = {}
        for k in reads:
            if k not in writes:
                self.readers.setdefault(k, {})[s] = ev

    def op(self, e, fn, reads=(), writes=()):
        need = self._deps(reads, writes)
        sem = self.esem[e]
        self._wait(e, need, id(sem) if e == 'pe' else None)
        ins = fn(self.eng[e])
        self.ecnt[e] += 1
        ins.then_inc(sem, 1)
        self.ninst += 1
        self._commit((sem, self.ecnt[e]), reads, writes)

    def dma(self, q, out, in_, reads=(), writes=(), st=None, fn=None):
        if st.ds is None:
            st.ds = self.free_ds.pop()
        ds = st.ds
        need = self._deps(reads, writes)
        if ds.cnt > 0:
            need[id(ds.sem)] = (ds.sem, ds.cnt)
        self._wait(q, need)
        ins = self.eng[q].dma_start(out=out, in_=in_) if fn is None else fn(self.eng[q])
        ds.cnt += 16
        ins.then_inc(ds.sem, 16)
        self.ninst += 1
        self._commit((ds.sem, ds.cnt), reads, writes)

    def barrier(self):
        need = {}
        for e in self.esem:
            if self.ecnt[e] > 0:
                need[id(self.esem[e])] = (self.esem[e], self.ecnt[e])
        for ds in self.all_ds:
            if ds.cnt > 0:
                need[id(ds.sem)] = (ds.sem, ds.cnt)
        for e in self.eng:
            self._wait(e, need)


class Phase:
    uid = 0

    def __init__(self, S):
        self.S = S
        self.stack = ExitStack()
        self.tiles = []

    def __enter__(self):
        self.stack.__enter__()
        return self

    def sbuf(self, name, shape, dt):
        Phase.uid += 1
        name = "%s_u%d" % (name, Phase.uid)
        t = Tile(self.stack.enter_context(self.S.nc.sbuf_tensor(name, list(shape), dt)), name)
        self.tiles.append(t)
        return t

    def __exit__(self, *a):
        self.S.barrier()
        for t in self.tiles:
            if t.ds is not None:
                self.S.free_ds.append(t.ds)
                t.ds = None
        return self.stack.__exit__(*a)


class Bank:
    def __init__(self, tile):
        self.tile = tile
        self.started = False


def build(T, layer_kinds, CAP, debug=False):
    NT = T // 128
    NL = len(layer_kinds)
    n_even = sum(1 for k in layer_kinds if k == 'e')
    n_odd = NL - n_even
    nc = bass.Bass("TRN2", target_bir_lowering=False)

    def din(name, shape, dt=F32):
        return nc.dram_tensor(name, list(shape), dt, kind="ExternalInput").ap()

    def dscr(name, shape, dt=F32):
        kind = "ExternalOutput" if debug else "Internal"
        return nc.dram_tensor(name, list(shape), dt, kind=kind).ap()

    x_in = din("x", [T, D])
    cst_d = din("cst", [128, NCST])
    tokid_d = din("tokid", [128, NT * 16], I32)
    ropeA = {d: (din("ropeC%d" % d, [64, T]), din("ropeS%d" % d, [64, T])) for d in DPAT}
    ret_t = [din("ret%d" % i, [64, T]) for i in range(4)]
    lbl_d = din("lbl", [2, 512])
    wA = [din("wA%d" % j, [1025, 1408]) for j in range(n_even)]
    wB = [din("wB%d" % j, [1025, 2048]) for j in range(n_even)]
    sinks = [din("sink%d" % j, [1, 8]) for j in range(n_even)]
    hnorm = [din("hnorm%d" % j, [1, 512]) for j in range(n_even)]
    woe = [din("woe%d" % j, [1024, 1024]) for j in range(n_even)]
    wC = [din("wC%d" % j, [1025, 2048]) for j in range(n_odd)]
    wDp = [[din("wD%d_%d" % (j, p), [1025, 1408]) for p in range(3)] for j in range(n_odd)]
    rnorm = [din("rnorm%d" % j, [1, 512]) for j in range(n_odd)]
    woo = [din("woo%d" % j, [1024, 1024]) for j in range(n_odd)]
    lnp = din("lnp", [NL, 4, 1024])
    rw = din("rw", [NL, 1024, 32])
    rb = din("rb", [NL, 32])
    ewgu = din("ewgu", [NL, NEXP, 1024, 2048])
    ebgu = din("ebgu", [NL, NEXP, 2048])
    ewdn = din("ewdn", [NL, NEXP, 1024, 1024])
    ebdn = din("ebdn", [NL, NEXP, 1024])
    out_d = nc.dram_tensor("out", [T, D], F32, kind="ExternalOutput").ap()

    XS = dscr("XS", [T, D])
    YMIX = dscr("YMIX", [T, D], BF16)
    UD = [dscr("UD%d" % p, [T, 520]) for p in range(3)]
    X1D = dscr("X1D", [T, D])
    X1B = dscr("X1B", [T, D], BF16)
    YE = dscr("YE", [NEXP * CAP, D])
    TOK = dscr("TOK", [NEXP * CAP, 16], I32)

    with ExitStack() as ctx:
        S = Sched(nc, ctx)

        def gsb(name, shape, dt):
            return Tile(ctx.enter_context(nc.sbuf_tensor("g_" + name, list(shape), dt)), name)

        P = [Bank(Tile(ctx.enter_context(nc.psum_tensor("ps%d" % i, [128, 512], F32)), "ps%d" % i)) for i in range(8)]

        def mm(bank, out, lhsT, rhs, reads):
            st = not bank.started
            bank.started = True
            S.op('pe', lambda e: e.matmul(out, lhsT=lhsT, rhs=rhs, start=st, stop=True, skip_group_check=True),
                 reads=reads, writes=[bank.tile])

        def tr(bank, out, in_, ident, reads):
            S.op('pe', lambda e: e.transpose(out, in_, ident), reads=reads, writes=[bank.tile])

        def tt(e, out, a, b, op, reads, writes):
            S.op(e, lambda g: g.tensor_tensor(out, a, b, op), reads=reads, writes=writes)

        def ts(e, out, a, s1, s2, op0, op1, reads, writes, **kw):
            if op1 is None:
                S.op(e, lambda g: g.tensor_scalar(out, a, s1, None, op0, **kw), reads=reads, writes=writes)
            else:
                S.op(e, lambda g: g.tensor_scalar(out, a, s1, s2, op0, op1, **kw), reads=reads, writes=writes)

        def act(out, in_, func, reads, writes, **kw):
            S.op('act', lambda g: g.activation(out, in_, func, **kw), reads=reads, writes=writes)

        cst = gsb("cst", [128, NCST], F32)
        S.dma('sp', cst[:], cst_d, writes=[cst], st=cst)
        cb = gsb("cb", [128, 896], BF16)
        S.op('dve', lambda g: g.tensor_copy(cb[:], cst[:, 0:896]), reads=[cst], writes=[cb])
        identb = cb[:, C_ID:C_ID + 128]
        identf = cst[:, C_ID:C_ID + 128]
        tokid = gsb("tokid", [128, NT, 16], I32)
        S.dma('sp', tokid[:].rearrange("p a b -> p (a b)"), tokid_d, writes=[tokid], st=tokid)
        onesb = gsb("onesb", [1, 512], BF16)
        S.op('dve', lambda g: g.memset(onesb[:], 1.0), writes=[onesb])
        onesf = gsb("onesf", [1, 128], F32)
        S.op('dve', lambda g: g.memset(onesf[:], 1.0), writes=[onesf])
        GS = gsb("GS", [128, NT, 4], I32)
        GT = gsb("GT", [128, NT, 4], F32)
        zer = gsb("zer", [128, 1024], I32)
        S.op('dve', lambda g: g.memset(zer[:], 0), writes=[zer])
        S.barrier()

        CK = [cst, cb]

        def load_xT(ph_t, rows_ap, n):
            xb = ph_t['xb'][n % 2]
            xT = ph_t['xT'][n % 2]
            S.dma('pool', xb[:], rows_ap, writes=[xb], st=xb)
            pt = P[0].tile[:].bitcast(BF16)
            for c in range(8):
                tr(P[0], pt[:, c * 128:(c + 1) * 128], xb[:, c * 128:(c + 1) * 128], identb, [xb] + CK)
            S.op('act', lambda g: g.copy(xT[:].rearrange("p a b -> p (a b)"), pt), reads=[P[0].tile], writes=[xT])
            return xT

        def xt_tiles(ph):
            return {'xb': [ph.sbuf("xb%d" % i, [128, 1024], BF16) for i in range(2)],
                    'xT': [ph.sbuf("xT%d" % i, [128, 8, 128], BF16) for i in range(2)]}

        def load_w(ph, name, wd, ncol):
            W = ph.sbuf(name, [128, 8, ncol], BF16)
            S.dma('pool', W[:], wd[0:1024, :].rearrange("(c p) n -> p c n", p=128), writes=[W], st=W)
            Wb = ph.sbuf(name + "b", [1, ncol], BF16)
            S.dma('pool', Wb[:], wd[1024:1025, :], writes=[Wb], st=Wb)
            return W, Wb

        def attn_pass(Xsrc, wd, d, mprev_off, tabs, sink_d, pat):
            nb = T // (128 * d)
            Xr = Xsrc.rearrange("(l r) n -> r l n", r=d)
            with Phase(S) as ph:
                W, Wb = load_w(ph, "aw", wd, 1408)
                xt = xt_tiles(ph)
                ct = [ph.sbuf("ct%d" % i, [64, 128], F32) for i in range(2)]
                stt_ = [ph.sbuf("st%d" % i, [64, 128], F32) for i in range(2)]
                T1 = ph.sbuf("T1", [64, 512], F32)
                T2 = ph.sbuf("T2", [64, 512], F32)
                QT = ph.sbuf("QT", [64, 8, 128], BF16)
                KT = [ph.sbuf("KT%d" % i, [64, 2, 128], BF16) for i in range(2)]
                VA = [ph.sbuf("VA%d" % i, [128, 2, 65], BF16) for i in range(2)]
                EE = [ph.sbuf("EE%d" % i, [128, 512], BF16) for i in range(4)]
                PM = [ph.sbuf("PM%d" % i, [128, 512], BF16) for i in range(4)]
                U = [ph.sbuf("U%d" % i, [128, 8, 65], F32) for i in range(2)]
                for i in range(2):
                    S.op('dve', lambda g, i=i: g.memset(VA[i][:], 1.0), writes=[VA[i]])
                if sink_d is not None:
                    esk = ph.sbuf("esk", [128, 8], F32)
                    S.dma('sp', esk[:], sink_d.to_broadcast([128, 8]), writes=[esk], st=esk)
                    act(esk[:], esk[:], AF.Exp, [esk], [esk])
                    DEN = ph.sbuf("DEN", [128, 8], F32)
                    YA = [ph.sbuf("YA%d" % i, [128, 8, 64], BF16) for i in range(2)]
                mcur = cb[:, C_MCUR:C_MCUR + 128]
                mprev = cb[:, mprev_off:mprev_off + 128]
                n = 0
                for r in range(d):
                    for b in range(nb):
                        cur, prv = n % 2, (n + 1) % 2
                        xT = load_xT(xt, Xr[r, 128 * b:128 * (b + 1), :], n)
                        c_t, s_t = ct[n % 2], stt_[n % 2]
                        S.dma('sp', c_t[:], tabs[0][:, n * 128:(n + 1) * 128], writes=[c_t], st=c_t)
                        S.dma('sp', s_t[:], tabs[1][:, n * 128:(n + 1) * 128], writes=[s_t], st=s_t)
                        for bk in P[1:7]:
                            bk.started = False
                        for h in range(8):
                            for (bq, off) in ((P[1 + h // 4], 0), (P[3 + h // 4], 512)):
                                o_ = bq.tile[0:64, (h % 4) * 128:(h % 4 + 1) * 128]
                                for c in range(8):
                                    mm(bq, o_, W[:, c, off + h * 64:off + (h + 1) * 64], xT[:, c, :], [W, xT])
                                mm(bq, o_, Wb[0:1, off + h * 64:off + (h + 1) * 64], onesb[0:1, 0:128], [Wb, onesb])
                        for kk in range(4):
                            o_ = P[5].tile[0:64, kk * 128:(kk + 1) * 128]
                            for c in range(8):
                                mm(P[5], o_, W[:, c, 1024 + kk * 64:1024 + (kk + 1) * 64], xT[:, c, :], [W, xT])
                            mm(P[5], o_, Wb[0:1, 1024 + kk * 64:1024 + (kk + 1) * 64], onesb[0:1, 0:128], [Wb, onesb])
                        for c in range(8):
                            mm(P[6], P[6].tile[:, 0:128], xT[:, c, :], W[:, c, 1280:1408], [W, xT])
                        mm(P[6], P[6].tile[:, 0:128], onesb[0:1, 0:128], Wb[0:1, 1280:1408], [Wb, onesb])
                        cbq = c_t[:].unsqueeze(1).to_broadcast([64, 4, 128])
                        sbq = s_t[:].unsqueeze(1).to_broadcast([64, 4, 128])
                        for g_ in range(2):
                            tt('dve', T1[:].rearrange("p (a b) -> p a b", a=4),
                               P[1 + g_].tile[0:64, :].rearrange("p (a b) -> p a b", a=4), cbq, ALU.mult,
                               [P[1 + g_].tile, c_t], [T1])
                            tt('dve', T2[:].rearrange("p (a b) -> p a b", a=4),
                               P[3 + g_].tile[0:64, :].rearrange("p (a b) -> p a b", a=4), sbq, ALU.mult,
                               [P[3 + g_].tile, s_t], [T2])
                            tt('dve', QT[:, 4 * g_:4 * g_ + 4, :].rearrange("p a b -> p (a b)"), T1[:], T2[:], ALU.add,
                               [T1, T2], [QT])
                        cbk = c_t[:].unsqueeze(1).to_broadcast([64, 2, 128])
                        sbk = s_t[:].unsqueeze(1).to_broadcast([64, 2, 128])
                        tt('dve', T1[:, 0:256].rearrange("p (a b) -> p a b", a=2),
                           P[5].tile[0:64, 0:256].rearrange("p (a b) -> p a b", a=2), cbk, ALU.mult,
                           [P[5].tile, c_t], [T1])
                        tt('dve', T2[:, 0:256].rearrange("p (a b) -> p a b", a=2),
                           P[5].tile[0:64, 256:512].rearrange("p (a b) -> p a b", a=2), sbk, ALU.mult,
                           [P[5].tile, s_t], [T2])
                        tt('dve', KT[cur][:].rearrange("p a b -> p (a b)"), T1[:, 0:256], T2[:, 0:256], ALU.add,
                           [T1, T2], [KT[cur]])
                        S.op('act', lambda g: g.copy(VA[cur][:, :, 0:64],
                                                     P[6].tile[:, 0:128].rearrange("p (a b) -> p a b", a=2)),
                             reads=[P[6].tile], writes=[VA[cur]])
                        for bk in P[1:7]:
                            bk.started = False
                        combos = []
                        for j in range(2):
                            mm(P[1 + j], P[1 + j].tile[:, :], KT[cur][:, j, :],
                               QT[:, 4 * j:4 * j + 4, :].rearrange("p a b -> p (a b)"), [KT[cur], QT])
                            combos.append((j, cur, P[1 + j], mcur, j))
                            if b > 0:
                                mm(P[3 + j], P[3 + j].tile[:, :], KT[prv][:, j, :],
                                   QT[:, 4 * j:4 * j + 4, :].rearrange("p a b -> p (a b)"), [KT[prv], QT])
                                combos.append((j, prv, P[3 + j], mprev, 2 + j))
                        for (j, pc, bk, msk, ei) in combos:
                            act(EE[ei][:], bk.tile[:, :], AF.Exp, [bk.tile], [EE[ei]], scale=0.125)
                            tt('dve', PM[ei][:].rearrange("p (a b) -> p a b", a=4),
                               EE[ei][:].rearrange("p (a b) -> p a b", a=4),
                               msk.unsqueeze(1).to_broadcast([128, 4, 128]), ALU.mult, [EE[ei]] + CK, [PM[ei]])
                        for (j, pc, bk, msk, ei) in combos:
                            for hh in range(4):
                                h = 4 * j + hh
                                ob = P[5 + h // 4]
                                mm(ob, ob.tile[:, (h % 4) * 65:(h % 4 + 1) * 65], PM[ei][:, hh * 128:(hh + 1) * 128],
                                   VA[pc][:, j, :], [PM[ei], VA[pc]])
                        Ut = U[n % 2]
                        for g_ in range(2):
                            S.op('act', lambda g, g_=g_: g.copy(Ut[:, 4 * g_:4 * g_ + 4, :].rearrange("p a b -> p (a b)"),
                                                                P[5 + g_].tile[:, 0:260]),
                                 reads=[P[5 + g_].tile], writes=[Ut])
                        rows = slice(128 * b, 128 * (b + 1))
                        if sink_d is not None:
                            tt('dve', DEN[:], Ut[:, :, 64], esk[:], ALU.add, [Ut, esk], [DEN])
                            S.op('dve', lambda g: g.reciprocal(DEN[:], DEN[:]), reads=[DEN], writes=[DEN])
                            ya = YA[n % 2]
                            tt('dve', ya[:], Ut[:, :, 0:64], DEN[:].unsqueeze(2).to_broadcast([128, 8, 64]), ALU.mult,
                               [Ut, DEN], [ya])
                            S.dma('sp', YMIX[rows, 0:512], ya[:].rearrange("p a b -> p (a b)"), reads=[ya], st=ya)
                        else:
                            S.dma('sp', UD[pat].rearrange("(l r) n -> r l n", r=d)[r, rows, :],
                                  Ut[:].rearrange("p a b -> p (a b)"), reads=[Ut], st=Ut)
                        n += 1

        def hgrn_pass(Xsrc, wd, j_even, hn_d):
            with Phase(S) as ph:
                W, Wb = load_w(ph, "bw", wd, 2048)
                xt = xt_tiles(ph)
                LB = ph.sbuf("LB", [128, 4], F32)
                OML = ph.sbuf("OML", [128, 4], F32)
                if j_even == 0:
                    S.op('dve', lambda g: g.memset(LB[:], 0.0), writes=[LB])
                else:
                    LL = ph.sbuf("LL", [128, 2, 4], F32)
                    S.dma('sp', LL[:], lbl_d.rearrange("l (h c) -> c l h", c=128), writes=[LL], st=LL,
                          fn=lambda q: q.dma_start(out=LL[:], in_=lbl_d.rearrange("l (h c) -> c l h", c=128),
                                                   allow_slow_non_contiguous=True))
                    tt('dve', LB[:], LL[:, 0, :], LL[:, 1, :], ALU.subtract, [LL], [LB])
                    act(LB[:], LB[:], AF.Sigmoid, [LB], [LB])
                ts('dve', OML[:], LB[:], -1.0, 1.0, ALU.mult, ALU.add, [LB], [OML])
                GB = ph.sbuf("GB", [128, 512], F32)
                S.dma('sp', GB[:], hn_d.to_broadcast([128, 512]), writes=[GB], st=GB)
                Fv = ph.sbuf("Fv", [128, 4, 128], F32)
                LF = ph.sbuf("LF", [128, 512], F32)
                KEY = ph.sbuf("KEY", [128, 512], F32)
                Bc = ph.sbuf("Bc", [128, 512], F32)
                EB = ph.sbuf("EB", [128, 512], F32)
                QTl = ph.sbuf("QTl", [128, 4, 128], BF16)
                KTl = ph.sbuf("KTl", [128, 4, 128], BF16)
                KB = ph.sbuf("KB", [128, 4, 128], BF16)
                EG = ph.sbuf("EG", [128, 16], F32)
                ATm = ph.sbuf("ATm", [128, 512], BF16)
                KBT = ph.sbuf("KBT", [128, 4, 128], BF16)
                V = ph.sbuf("V", [128, 512], BF16)
                SG = ph.sbuf("SG", [128, 512], F32)
                St = [ph.sbuf("St%d" % h, [128, 128], F32) for h in range(4)]
                Sb = [ph.sbuf("Sb%d" % h, [128, 128], BF16) for h in range(4)]
                SQ = ph.sbuf("SQ", [128, 512], F32)
                SS = ph.sbuf("SS", [128, 4], F32)
                YB = [ph.sbuf("YB%d" % i, [128, 512], BF16) for i in range(2)]
                QZ = [ph.sbuf("QZ%d" % h, [128, 4, 128], BF16) for h in range(4)]
                KZ = [ph.sbuf("KZ%d" % h, [128, 512], BF16) for h in range(4)]
                for h in range(4):
                    S.op('dve', lambda g, h=h: g.memset(St[h][:], 0.0), writes=[St[h]])
                    S.op('dve', lambda g, h=h: g.memset(Sb[h][:], 0.0), writes=[Sb[h]])
                    S.op('dve', lambda g, h=h: g.memset(QZ[h][:], 0.0), writes=[QZ[h]])
                rs = cst[:, C_RS:C_RS + 512]
                mbd = cb[:, C_MBD:C_MBD + 128]
                F2 = Fv[:].rearrange("p a b -> p (a b)")
                for i in range(NT):
                    xT = load_xT(xt, Xsrc[128 * i:128 * (i + 1), :], i)
                    for bk in P[1:8]:
                        bk.started = False
                    for h in range(4):
                        for (bk, off) in ((P[1], 0), (P[2], 512)):
                            o_ = bk.tile[:, h * 128:(h + 1) * 128]
                            for c in range(8):
                                mm(bk, o_, W[:, c, off + h * 128:off + (h + 1) * 128], xT[:, c, :], [W, xT])
                            mm(bk, o_, Wb[0:1, off + h * 128:off + (h + 1) * 128], onesb[0:1, 0:128], [Wb, onesb])
                    for (bk, off) in ((P[5], 1024), (P[6], 1536)):
                        for c in range(8):
                            mm(bk, bk.tile[:, :], xT[:, c, :], W[:, c, off:off + 512], [W, xT])
                        mm(bk, bk.tile[:, :], onesb[0:1, 0:128], Wb[0:1, off:off + 512], [Wb, onesb])
                    act(F2, P[2].tile[:, :], AF.Sigmoid, [P[2].tile], [Fv])
                    tt('dve', Fv[:], Fv[:], OML[:].unsqueeze(2).to_broadcast([128, 4, 128]), ALU.mult, [Fv, OML], [Fv])
                    tt('dve', Fv[:], Fv[:], LB[:].unsqueeze(2).to_broadcast([128, 4, 128]), ALU.add, [Fv, LB], [Fv])
                    ts('dve', F2, F2, 1e-30, None, ALU.max, None, [Fv], [Fv])
                    act(LF[:], F2, AF.Ln, [Fv], [LF])
                    ts('dve', KEY[:], F2, -1.0, 1.0, ALU.mult, ALU.add, [Fv], [KEY])
                    S.op('dve', lambda g: g.tensor_tensor_scan(Bc[:], rs, LF[:], 0.0, ALU.mult, ALU.add),
                         reads=[LF] + CK, writes=[Bc])
                    act(EB[:], Bc[:], AF.Exp, [Bc], [EB])
                    tt('dve', QTl[:].rearrange("p a b -> p (a b)"), P[1].tile[:, :], EB[:], ALU.mult, [P[1].tile, EB], [QTl])
                    act(EB[:], Bc[:], AF.Exp, [Bc], [EB], scale=-1.0)
                    tt('dve', KTl[:].rearrange("p a b -> p (a b)"), KEY[:], EB[:], ALU.mult, [KEY, EB], [KTl])
                    BL = Bc[:].rearrange("p (a b) -> p a b", b=32)[:, :, 31:32]
                    tt('dve', LF[:].rearrange("p (a b) -> p a b", b=32), BL.to_broadcast([128, 16, 32]),
                       Bc[:].rearrange("p (a b) -> p a b", b=32), ALU.subtract, [Bc], [LF])
                    act(EB[:], LF[:], AF.Exp, [LF], [EB])
                    tt('dve', KB[:].rearrange("p a b -> p (a b)"), KEY[:], EB[:], ALU.mult, [KEY, EB], [KB])
                    act(EG[:].unsqueeze(2), BL, AF.Exp, [Bc], [EG])
                    for h in range(4):
                        mm(P[3], P[3].tile[:, h * 128:(h + 1) * 128], KTl[:, h, :], QTl[:, h, :], [KTl, QTl])
                    tt('dve', ATm[:].rearrange("p (a b) -> p a b", a=4), P[3].tile[:, :].rearrange("p (a b) -> p a b", a=4),
                       mbd.unsqueeze(1).to_broadcast([128, 4, 128]), ALU.mult, [P[3].tile] + CK, [ATm])
                    p4 = P[4].tile[:].bitcast(BF16)
                    for h in range(4):
                        tr(P[4], p4[:, h * 128:(h + 1) * 128], KB[:, h, :], identb, [KB] + CK)
                    S.op('act', lambda g: g.copy(KBT[:].rearrange("p a b -> p (a b)"), p4[:, 0:512]),
                         reads=[P[4].tile], writes=[KBT])
                    S.op('act', lambda g: g.copy(V[:], P[5].tile[:, :]), reads=[P[5].tile], writes=[V])
                    act(SG[:], P[6].tile[:, :], AF.Silu, [P[6].tile], [SG])
                    tt('dve', SG[:], SG[:], GB[:], ALU.mult, [SG, GB], [SG])
                    for h in range(4):
                        mm(P[7], P[7].tile[:, h * 128:(h + 1) * 128], ATm[:, h * 128:(h + 1) * 128],
                           V[:, h * 128:(h + 1) * 128], [ATm, V])
                    for jc in range(4):
                        pr = slice(32 * jc, 32 * jc + 32)
                        S.op('dve', lambda g, jc=jc, pr=pr: g.tensor_copy(QZ[jc][:, :, pr], QTl[:, :, pr]),
                             reads=[QTl], writes=[QZ[jc]])
                        S.op('act', lambda g, jc=jc: g.activation(KZ[jc][:], KBT[:].rearrange("p a b -> p (a b)"), AF.Copy,
                                                                  scale=cst[:, C_RM + jc:C_RM + jc + 1]),
                             reads=[KBT] + CK, writes=[KZ[jc]])
                    for jc in range(4):
                        for h in range(4):
                            mm(P[7], P[7].tile[:, h * 128:(h + 1) * 128], QZ[jc][:, h, :], Sb[h][:], [QZ[jc], Sb[h]])
                        P[2].started = False
                        for h in range(4):
                            mm(P[2], P[2].tile[:, h * 128:(h + 1) * 128], KZ[jc][:, h * 128:(h + 1) * 128],
                               V[:, h * 128:(h + 1) * 128], [KZ[jc], V])
                        for h in range(4):
                            S.op('dve', lambda g, h=h: g.scalar_tensor_tensor(
                                St[h][:], St[h][:], EG[:, h * 4 + jc:h * 4 + jc + 1], P[2].tile[:, h * 128:(h + 1) * 128],
                                ALU.mult, ALU.add), reads=[St[h], EG, P[2].tile], writes=[St[h]])
                            S.op('act', lambda g, h=h: g.copy(Sb[h][:], St[h][:]), reads=[St[h]], writes=[Sb[h]])
                    act(SQ[:], P[7].tile[:, :], AF.Square, [P[7].tile], [SQ])
                    S.op('dve', lambda g: g.tensor_reduce(SS[:], SQ[:].rearrange("p (a b) -> p a b", a=4), AX.X, ALU.add),
                         reads=[SQ], writes=[SS])
                    ts('dve', SS[:], SS[:], 1.0 / 128, NORM_EPS, ALU.mult, ALU.add, [SS], [SS])
                    act(SS[:], SS[:], AF.Sqrt, [SS], [SS])
                    S.op('dve', lambda g: g.reciprocal(SS[:], SS[:]), reads=[SS], writes=[SS])
                    tt('dve', SQ[:].rearrange("p (a b) -> p a b", a=4), P[7].tile[:, :].rearrange("p (a b) -> p a b", a=4),
                       SS[:].unsqueeze(2).to_broadcast([128, 4, 128]), ALU.mult, [P[7].tile, SS], [SQ])
                    yb = YB[i % 2]
                    tt('dve', yb[:], SQ[:], SG[:], ALU.mult, [SQ, SG], [yb])
                    S.dma('sp', YMIX[128 * i:128 * (i + 1), 512:1024], yb[:], reads=[yb], st=yb)

        def ret_pass(Xsrc, wd, rn_d):
            lg = [np.log1p(-2.0 ** (-5.0 - h)) for h in range(4)]
            with Phase(S) as ph:
                W, Wb = load_w(ph, "cw", wd, 2048)
                xt = xt_tiles(ph)
                GB = ph.sbuf("GB", [128, 512], F32)
                S.dma('sp', GB[:], rn_d.to_broadcast([128, 512]), writes=[GB], st=GB)
                tb = [[ph.sbuf("tb%d_%d" % (k, i), [64, 128], F32) for i in range(2)] for k in range(4)]
                T1 = ph.sbuf("T1", [64, 512], F32)
                T2 = ph.sbuf("T2", [64, 512], F32)
                QT = ph.sbuf("QT", [64, 4, 128], BF16)
                KT = ph.sbuf("KT", [64, 4, 128], BF16)
                QH = ph.sbuf("QH", [64, 4, 128], BF16)
                KL = ph.sbuf("KL", [64, 4, 128], BF16)
                KTT = ph.sbuf("KTT", [128, 4, 64], BF16)
                SCm = ph.sbuf("SCm", [128, 512], BF16)
                V = ph.sbuf("V", [128, 512], BF16)
                SG = ph.sbuf("SG", [128, 512], F32)
                St = [ph.sbuf("St%d" % h, [64, 128], F32) for h in range(4)]
                Sb = [ph.sbuf("Sb%d" % h, [64, 128], BF16) for h in range(4)]
                OC = ph.sbuf("OC", [128, 512], F32)
                SQ = ph.sbuf("SQ", [128, 512], F32)
                SS = ph.sbuf("SS", [128, 4], F32)
                MS = ph.sbuf("MS", [128, 4], F32)
                YB = [ph.sbuf("YB%d" % i, [128, 512], BF16) for i in range(2)]
                for h in range(4):
                    S.op('dve', lambda g, h=h: g.memset(St[h][:], 0.0), writes=[St[h]])
                    S.op('dve', lambda g, h=h: g.memset(Sb[h][:], 0.0), writes=[Sb[h]])
                DT = cst[:, C_DT:C_DT + 512]
                QD = cst[0:64, C_QD:C_QD + 512]
                KD = cst[0:64, C_KD:C_KD + 512]
                for i in range(NT):
                    xT = load_xT(xt, Xsrc[128 * i:128 * (i + 1), :], i)
                    tbs = [tb[k][i % 2] for k in range(4)]
                    for k in range(4):
                        S.dma('sp', tbs[k][:], ret_t[k][:, 128 * i:128 * (i + 1)], writes=[tbs[k]], st=tbs[k])
                    for bk in P[1:8]:
                        bk.started = False
                    for h in range(4):
                        for (bk, off) in ((P[1], 0), (P[2], 256), (P[3], 512), (P[4], 768)):
                            o_ = bk.tile[0:64, h * 128:(h + 1) * 128]
                            for c in range(8):
                                mm(bk, o_, W[:, c, off + h * 64:off + (h + 1) * 64], xT[:, c, :], [W, xT])
                            mm(bk, o_, Wb[0:1, off + h * 64:off + (h + 1) * 64], onesb[0:1, 0:128], [Wb, onesb])
                    for (bk, off) in ((P[5], 1024), (P[6], 1536)):
                        for c in range(8):
                            mm(bk, bk.tile[:, :], xT[:, c, :], W[:, c, off:off + 512], [W, xT])
                        mm(bk, bk.tile[:, :], onesb[0:1, 0:128], Wb[0:1, off:off + 512], [Wb, onesb])
                    for (dst, pa, pb, ta, tb_) in ((QT, P[1], P[2], tbs[0], tbs[1]), (KT, P[3], P[4], tbs[2], tbs[3])):
                        tt('dve', T1[:].rearrange("p (a b) -> p a b", a=4), pa.tile[0:64, :].rearrange("p (a b) -> p a b", a=4),
                           ta[:].unsqueeze(1).to_broadcast([64, 4, 128]), ALU.mult, [pa.tile, ta], [T1])
                        tt('dve', T2[:].rearrange("p (a b) -> p a b", a=4), pb.tile[0:64, :].rearrange("p (a b) -> p a b", a=4),
                           tb_[:].unsqueeze(1).to_broadcast([64, 4, 128]), ALU.mult, [pb.tile, tb_], [T2])
                        tt('dve', dst[:].rearrange("p a b -> p (a b)"), T1[:], T2[:], ALU.add, [T1, T2], [dst])
                    tt('dve', QH[:].rearrange("p a b -> p (a b)"), QT[:].rearrange("p a b -> p (a b)"), QD, ALU.mult,
                       [QT] + CK, [QH])
                    tt('dve', KL[:].rearrange("p a b -> p (a b)"), KT[:].rearrange("p a b -> p (a b)"), KD, ALU.mult,
                       [KT] + CK, [KL])
                    S.op('act', lambda g: g.copy(V[:], P[5].tile[:, :]), reads=[P[5].tile], writes=[V])
                    act(SG[:], P[6].tile[:, :], AF.Silu, [P[6].tile], [SG])
                    tt('dve', SG[:], SG[:], GB[:], ALU.mult, [SG, GB], [SG])
                    P[1].started = False
                    for h in range(4):
                        mm(P[1], P[1].tile[:, h * 128:(h + 1) * 128], KT[:, h, :], QT[:, h, :], [KT, QT])
                    tt('dve', SCm[:], P[1].tile[:, :], DT, ALU.mult, [P[1].tile] + CK, [SCm])
                    p3 = P[3].tile[:].bitcast(BF16)
                    for h in range(4):
                        tr(P[3], p3[:, h * 64:(h + 1) * 64], KL[:, h, :], cb[0:64, C_ID:C_ID + 64], [KL] + CK)
                    S.op('act', lambda g: g.copy(KTT[:].rearrange("p a b -> p (a b)"), p3[:, 0:256]),
                         reads=[P[3].tile], writes=[KTT])
                    for h in range(4):
                        mm(P[7], P[7].tile[:, h * 128:(h + 1) * 128], SCm[:, h * 128:(h + 1) * 128],
                           V[:, h * 128:(h + 1) * 128], [SCm, V])
                    for h in range(4):
                        mm(P[7], P[7].tile[:, h * 128:(h + 1) * 128], QH[:, h, :], Sb[h][:], [QH, Sb[h]])
                    P[2].started = False
                    for h in range(4):
                        mm(P[2], P[2].tile[0:64, h * 128:(h + 1) * 128], KTT[:, h, :], V[:, h * 128:(h + 1) * 128], [KTT, V])
                    for h in range(4):
                        cdh = float(np.exp(lg[h] * 128))
                        S.op('dve', lambda g, h=h, cdh=cdh: g.scalar_tensor_tensor(
                            St[h][:], St[h][:], cdh, P[2].tile[0:64, h * 128:(h + 1) * 128], ALU.mult, ALU.add),
                            reads=[St[h], P[2].tile], writes=[St[h]])
                        S.op('act', lambda g, h=h: g.copy(Sb[h][:], St[h][:]), reads=[St[h]], writes=[Sb[h]])
                    p7 = P[7].tile[:, :].rearrange("p (a b) -> p a b", a=4)
                    S.op('dve', lambda g: g.tensor_reduce(MS[:], p7, AX.X, ALU.add), reads=[P[7].tile], writes=[MS])
                    ts('dve', MS[:], MS[:], -1.0 / 128, None, ALU.mult, None, [MS], [MS])
                    tt('dve', OC[:].rearrange("p (a b) -> p a b", a=4), p7, MS[:].unsqueeze(2).to_broadcast([128, 4, 128]),
                       ALU.add, [P[7].tile, MS], [OC])
                    act(SQ[:], OC[:], AF.Square, [OC], [SQ])
                    S.op('dve', lambda g: g.tensor_reduce(SS[:], SQ[:].rearrange("p (a b) -> p a b", a=4), AX.X, ALU.add),
                         reads=[SQ], writes=[SS])
                    ts('dve', SS[:], SS[:], 1.0 / 128, NORM_EPS, ALU.mult, ALU.add, [SS], [SS])
                    act(SS[:], SS[:], AF.Sqrt, [SS], [SS])
                    S.op('dve', lambda g: g.reciprocal(SS[:], SS[:]), reads=[SS], writes=[SS])
                    tt('dve', SQ[:].rearrange("p (a b) -> p a b", a=4), OC[:].rearrange("p (a b) -> p a b", a=4),
                       SS[:].unsqueeze(2).to_broadcast([128, 4, 128]), ALU.mult, [OC, SS], [SQ])
                    yb = YB[i % 2]
                    tt('dve', yb[:], SQ[:], SG[:], ALU.mult, [SQ, SG], [yb])
                    S.dma('sp', YMIX[128 * i:128 * (i + 1), 0:512], yb[:], reads=[yb], st=yb)

        def layer_norm(ph_t, Z, OUT, G, Bt):
            ST, MV, RS_ = ph_t['ST'], ph_t['MV'], ph_t['RS']
            for k in range(2):
                S.op('dve', lambda g, k=k: g.bn_stats(ST[:, k, :], Z[:, k * 512:(k + 1) * 512]), reads=[Z], writes=[ST])
            S.op('dve', lambda g: g.bn_aggr(MV[:], ST[:].rearrange("p a b -> p (a b)")), reads=[ST], writes=[MV])
            ts('dve', RS_[:], MV[:, 1:2], LN_EPS, None, ALU.add, None, [MV], [RS_])
            act(RS_[:], RS_[:], AF.Sqrt, [RS_], [RS_])
            S.op('dve', lambda g: g.reciprocal(RS_[:], RS_[:]), reads=[RS_], writes=[RS_])
            ts('dve', OUT[:], Z[:], MV[:, 0:1], RS_[:, 0:1], ALU.subtract, ALU.mult, [Z, MV, RS_], [OUT])
            tt('dve', OUT[:], OUT[:], G[:], ALU.mult, [OUT, G], [OUT])
            tt('dve', OUT[:], OUT[:], Bt[:], ALU.add, [OUT, Bt], [OUT])

        def ln_tiles(ph):
            return {'ST': ph.sbuf("lnST", [128, 2, 6], F32), 'MV': ph.sbuf("lnMV", [128, 2], F32),
                    'RS': ph.sbuf("lnRS", [128, 1], F32)}

        def oproj_phase(Xsrc, L, kind, wo_d):
            with Phase(S) as ph:
                Wo = ph.sbuf("Wo", [128, 8, 1024], BF16)
                S.dma('pool', Wo[:], wo_d.rearrange("(c p) n -> p c n", p=128), writes=[Wo], st=Wo)
                G = ph.sbuf("G", [128, 1024], F32)
                Bt = ph.sbuf("Bt", [128, 1024], F32)
                S.dma('sp', G[:], lnp[L, 0:1, :].to_broadcast([128, 1024]), writes=[G], st=G)
                S.dma('sp', Bt[:], lnp[L, 1:2, :].to_broadcast([128, 1024]), writes=[Bt], st=Bt)
                Wr = ph.sbuf("Wr", [128, 8, 32], F32)
                S.dma('sp', Wr[:], rw[L].rearrange("(c p) n -> p c n", p=128), writes=[Wr], st=Wr)
                rbt = ph.sbuf("rbt", [1, 32], F32)
                S.dma('sp', rbt[:], rb[L:L + 1, :], writes=[rbt], st=rbt)
                lt_2 = [ln_tiles(ph), ln_tiles(ph)]
                ym = [ph.sbuf("ym%d" % i, [128, 1024], BF16) for i in range(2)]
                yT_2 = [ph.sbuf("yT%d" % i_, [128, 8, 128], BF16) for i_ in range(2)]
                xt_ = [ph.sbuf("xt%d" % i, [128, 1024], F32) for i in range(2)]
                Z_2 = [ph.sbuf("Z%d" % i_, [128, 1024], F32) for i_ in range(2)]
                x1 = [ph.sbuf("x1_%d" % i, [128, 1024], F32) for i in range(2)]
                x1T_2 = [ph.sbuf("x1T%d" % i_, [128, 8, 128], F32) for i_ in range(2)]
                LG_2 = [ph.sbuf("LG%d" % i_, [128, 32], F32) for i_ in range(2)]
                MX_2 = [ph.sbuf("MX%d" % i_, [128, 8], F32) for i_ in range(2)]
                MI_2 = [ph.sbuf("MI%d" % i_, [128, 8], U32) for i_ in range(2)]
                MIF_2 = [ph.sbuf("MIF%d" % i_, [128, 4], F32) for i_ in range(2)]
                NM_2 = [ph.sbuf("NM%d" % i_, [128, 1], F32) for i_ in range(2)]
                EX_2 = [ph.sbuf("EX%d" % i_, [128, 4], F32) for i_ in range(2)]
                SM_2 = [ph.sbuf("SM%d" % i_, [128, 1], F32) for i_ in range(2)]
                MK_2 = [ph.sbuf("MK%d" % i_, [128, 32], BF16) for i_ in range(2)]
                SLOT_2 = [ph.sbuf("SLOT%d" % i_, [128, 32], F32) for i_ in range(2)]
                CNT = ph.sbuf("CNT", [128, 32], F32)
                TMP_2 = [ph.sbuf("TMP%d" % i_, [128, 32], F32) for i_ in range(2)]
                SK_2 = [ph.sbuf("SK%d" % i_, [128, 4], F32) for i_ in range(2)]
                GSF_2 = [ph.sbuf("GSF%d" % i_, [128, 4], F32) for i_ in range(2)]
                S.op('dve', lambda g: g.memset(CNT[:], 0.0), writes=[CNT])
                if kind == 'o':
                    Uu = [[ph.sbuf("Uu%d_%d" % (p, i), [128, 8, 65], F32) for i in range(2)] for p in range(3)]
                    RD = ph.sbuf("RD", [128, 8], F32)
                iota32 = cst[:, C_IOTA:C_IOTA + 32]
                tri = cb[:, C_TRI:C_TRI + 128]
                ones = cb[:, C_ONE:C_ONE + 128]
                scat = [ph.sbuf("scat%d" % k, [128, 1], I32) for k in range(4)]
                def bind(i):
                    return [t2_[i % 2] for t2_ in (yT_2, Z_2, x1T_2, LG_2, MX_2, MI_2, MIF_2, NM_2, EX_2, SM_2, MK_2, SLOT_2,
                                                   TMP_2, SK_2, GSF_2)]

                def stageA(i):
                    rows = slice(128 * i, 128 * (i + 1))
                    (yT, Z, x1T, LG, MX, MI, MIF, NM, EX, SM, MK, SLOT, TMP, SK, GSF) = bind(i)
                    PL = P[5] if i % 2 == 0 else P[7]
                    lt = lt_2[i % 2]
                    y = ym[i % 2]
                    if kind == 'e':
                        S.dma('sp', y[:], YMIX[rows, :], writes=[y], st=y)
                    else:
                        S.dma('sp', y[:, 0:512], YMIX[rows, 0:512], writes=[y], st=y)
                        us = [Uu[p][i % 2] for p in range(3)]
                        for p in range(3):
                            S.dma('sp', us[p][:].rearrange("p a b -> p (a b)"), UD[p][rows, :], writes=[us[p]], st=us[p])
                        tt('dve', us[0][:], us[0][:], us[1][:], ALU.add, [us[0], us[1]], [us[0]])
                        tt('dve', us[0][:], us[0][:], us[2][:], ALU.add, [us[0], us[2]], [us[0]])
                        S.op('dve', lambda g: g.reciprocal(RD[:], us[0][:, :, 64]), reads=[us[0]], writes=[RD])
                        tt('dve', y[:, 512:1024].rearrange("p (a b) -> p a b", a=8), us[0][:, :, 0:64],
                           RD[:].unsqueeze(2).to_broadcast([128, 8, 64]), ALU.mult, [us[0], RD], [y])
                    pt = P[0].tile[:].bitcast(BF16)
                    for c in range(8):
                        tr(P[0], pt[:, c * 128:(c + 1) * 128], y[:, c * 128:(c + 1) * 128], identb, [y] + CK)
                    S.op('act', lambda g: g.copy(yT[:].rearrange("p a b -> p (a b)"), pt), reads=[P[0].tile], writes=[yT])
                    for bk in P[1:8]:
                        bk.started = False
                    for k in range(2):
                        for c in range(8):
                            mm(P[1 + k], P[1 + k].tile[:, :], yT[:, c, :], Wo[:, c, k * 512:(k + 1) * 512], [yT, Wo])
                    xx = xt_[i % 2]
                    S.dma('sp', xx[:], Xsrc[rows, :], writes=[xx], st=xx)
                    for k in range(2):
                        S.op('dve', lambda g, k=k: g.scalar_tensor_tensor(
                            Z[:, k * 512:(k + 1) * 512], xx[:, k * 512:(k + 1) * 512], DN_ALPHA, P[1 + k].tile[:, :],
                            ALU.mult, ALU.add), reads=[xx, P[1 + k].tile], writes=[Z])
                    xo = x1[i % 2]
                    layer_norm(lt, Z, xo, G, Bt)
                    S.dma('sp', X1D[rows, :], xo[:], reads=[xo], st=xo)
                    S.dma('pool', X1B[rows, :], xo[:], reads=[xo], st=xo)
                    for c in range(8):
                        bk = P[3 + c // 4]
                        tr(bk, bk.tile[:, (c % 4) * 128:(c % 4 + 1) * 128], xo[:, c * 128:(c + 1) * 128], identf, [xo] + CK)
                    for k in range(2):
                        S.op('act', lambda g, k=k: g.copy(x1T[:, 4 * k:4 * k + 4, :].rearrange("p a b -> p (a b)"),
                                                          P[3 + k].tile[:, :]), reads=[P[3 + k].tile], writes=[x1T])
                    PL.started = False
                    for c in range(8):
                        mm(PL, PL.tile[:, 0:32], x1T[:, c, :], Wr[:, c, :], [x1T, Wr])
                    mm(PL, PL.tile[:, 0:32], onesf[0:1, :], rbt[0:1, :], [onesf, rbt])

                def stageB(i):
                    (yT, Z, x1T, LG, MX, MI, MIF, NM, EX, SM, MK, SLOT, TMP, SK, GSF) = bind(i)
                    PL = P[5] if i % 2 == 0 else P[7]
                    P[6].started = False
                    S.op('dve', lambda g: g.tensor_copy(LG[:], PL.tile[:, 0:32]), reads=[PL.tile], writes=[LG])
                    S.op('dve', lambda g: g.max(MX[:], LG[:]), reads=[LG], writes=[MX])
                    S.op('dve', lambda g: g.max_index(MI[:], MX[:], LG[:]), reads=[LG, MX], writes=[MI])
                    ts('dve', NM[:], MX[:, 0:1], -1.0, None, ALU.mult, None, [MX], [NM])
                    act(EX[:], MX[:, 0:4], AF.Exp, [MX, NM], [EX, SM], bias=NM[:, 0:1], accum_out=SM[:, 0:1])
                    S.op('dve', lambda g: g.reciprocal(SM[:], SM[:]), reads=[SM], writes=[SM])
                    ts('dve', GT[:, i, :], EX[:], SM[:, 0:1], None, ALU.mult, None, [EX, SM], [('GT', i)])
                    ts('dve', MK[:], LG[:], MX[:, 3:4], None, ALU.is_ge, None, [LG, MX], [MK])
                    mm(P[6], P[6].tile[:, 0:32], tri, MK[:], [MK] + CK)
                    mm(P[6], P[6].tile[:, 32:64], ones, MK[:], [MK] + CK)
                    tt('dve', SLOT[:], P[6].tile[:, 0:32], CNT[:], ALU.add, [P[6].tile, CNT], [SLOT])
                    tt('dve', CNT[:], CNT[:], P[6].tile[:, 32:64], ALU.add, [P[6].tile, CNT], [CNT])
                    S.op('dve', lambda g: g.tensor_copy(MIF[:], MI[:, 0:4]), reads=[MI], writes=[MIF])
                    for k in range(4):
                        S.op('dve', lambda g, k=k: g.scalar_tensor_tensor(
                            TMP[:], iota32, MIF[:, k:k + 1], SLOT[:], ALU.is_equal, ALU.mult, accum_out=SK[:, k:k + 1]),
                            reads=[MIF, SLOT] + CK, writes=[TMP, SK])
                    ts('dve', SK[:], SK[:], float(CAP - 1), None, ALU.min, None, [SK], [SK])
                    S.op('dve', lambda g: g.scalar_tensor_tensor(GSF[:], MIF[:], float(CAP), SK[:], ALU.mult, ALU.add),
                         reads=[MIF, SK], writes=[GSF])
                    S.op('dve', lambda g: g.tensor_copy(GS[:, i, :], GSF[:]), reads=[GSF], writes=[('GS', i)])
                    for k in range(4):
                        S.dma('pool', None, None, reads=[('GS', i)], st=scat[k],
                              fn=lambda q, k=k: q.indirect_dma_start(
                                  out=TOK, out_offset=bass.IndirectOffsetOnAxis(ap=GS[:, i, k:k + 1], axis=0),
                                  in_=tokid[:, i, :], in_offset=None))

                stageA(0)
                for i in range(1, NT):
                    stageA(i)
                    stageB(i - 1)
                stageB(NT - 1)

        def expert_phase(L):
            NS = CAP // 128
            groups = [(g0, min(512, CAP - g0)) for g0 in range(0, CAP, 512)]
            with Phase(S) as ph:
                WG = [ph.sbuf("WG%d" % i, [128, 8, 2048], BF16) for i in range(2)]
                WD = [ph.sbuf("WD%d" % i, [128, 8, 1024], BF16) for i in range(2)]
                BDN = [ph.sbuf("BDN%d" % i, [128, 1024], F32) for i in range(2)]
                BGU = ph.sbuf("BGU", [128, 512], F32)
                BGT = [ph.sbuf("BGT%d" % i, [128, 128], F32) for i in range(4)]
                bsrc = ebgu[L].rearrange("e (c p) -> (e c) p", p=128)
                P[7].started = False
                for q4 in range(4):
                    S.dma('sp', BGT[q4][:], bsrc[q4 * 128:(q4 + 1) * 128, :], writes=[BGT[q4]], st=BGT[q4])
                    tr(P[7], P[7].tile[:, q4 * 128:(q4 + 1) * 128], BGT[q4][:], identf, [BGT[q4]] + CK)
                S.op('dve', lambda g: g.tensor_copy(BGU[:], P[7].tile[:, :]), reads=[P[7].tile], writes=[BGU])
                bv = BGU[:].rearrange("p (e c) -> p e c", c=16)
                ts('dve', bv[:, :, 8:16], bv[:, :, 8:16], 1.0, None, ALU.add, None, [BGU], [BGU])
                IDX = [ph.sbuf("IDX%d" % i, [128, 16], I32) for i in range(2)]
                XG = [ph.sbuf("XG%d" % i, [128, 1024], BF16) for i in range(2)]
                XGT = ph.sbuf("XGT", [128, 8, CAP], BF16)
                AT = [ph.sbuf("AT%d" % i, [128, 8, 512], BF16) for i in range(2)]
                GC = ph.sbuf("GC", [128, 512], F32)
                SGt = ph.sbuf("SGt", [128, 512], F32)
                UC = ph.sbuf("UC", [128, 512], F32)
                YO = [ph.sbuf("YO%d" % i, [128, 1024], F32) for i in range(2)]
                n = 0
                gi = 0
                for e in range(NEXP):
                    wg, wdn, bdn = WG[e % 2], WD[e % 2], BDN[e % 2]
                    S.dma('pool', wg[:], ewgu[L, e].rearrange("(c p) n -> p c n", p=128), writes=[wg], st=wg)
                    S.dma('pool', wdn[:], ewdn[L, e].rearrange("(c p) n -> p c n", p=128), writes=[wdn], st=wdn)
                    S.dma('sp', bdn[:], ebdn[L, e:e + 1, :].to_broadcast([128, 1024]), writes=[bdn], st=bdn)
                    for j in range(NS):
                        ix, xg = IDX[n % 2], XG[n % 2]
                        r0 = e * CAP + j * 128
                        S.dma('sp', ix[:], TOK[r0:r0 + 128, :], writes=[ix], st=ix)
                        S.dma('pool', None, None, reads=[ix], writes=[xg], st=xg,
                              fn=lambda q, ix=ix, xg=xg: q.indirect_dma_start(
                                  out=xg[:], out_offset=None, in_=X1B,
                                  in_offset=bass.IndirectOffsetOnAxis(ap=ix[:, 0:1], axis=0)))
                        pt = P[0].tile[:].bitcast(BF16)
                        for c in range(8):
                            tr(P[0], pt[:, c * 128:(c + 1) * 128], xg[:, c * 128:(c + 1) * 128], identb, [xg] + CK)
                        S.op('act', lambda g, j=j: g.copy(XGT[:, :, j * 128:(j + 1) * 128],
                                                          pt.rearrange("p (a b) -> p a b", a=8)),
                             reads=[P[0].tile], writes=[XGT])
                        n += 1
                    for (g0, gn) in groups:
                        at = AT[gi % 2]
                        gi += 1
                        for fc in range(8):
                            bg, bu = P[1 + (fc % 2) * 2], P[2 + (fc % 2) * 2]
                            bg.started = False
                            bu.started = False
                            for (bk, f0) in ((bg, fc * 128), (bu, 1024 + fc * 128)):
                                for c in range(8):
                                    mm(bk, bk.tile[:, 0:gn], wg[:, c, f0:f0 + 128], XGT[:, c, g0:g0 + gn], [wg, XGT])
                            ts('dve', GC[:, 0:gn], bg.tile[:, 0:gn], BGU[:, e * 16 + fc:e * 16 + fc + 1], 7.0, ALU.add, ALU.min,
                               [bg.tile, BGU], [GC])
                            act(SGt[:, 0:gn], GC[:, 0:gn], AF.Sigmoid, [GC], [SGt], scale=1.702)
                            tt('pool', GC[:, 0:gn], GC[:, 0:gn], SGt[:, 0:gn], ALU.mult, [GC, SGt], [GC])
                            ts('dve', UC[:, 0:gn], bu.tile[:, 0:gn], BGU[:, e * 16 + 8 + fc:e * 16 + 8 + fc + 1], -6.0,
                               ALU.add, ALU.max, [bu.tile, BGU], [UC])
                            S.op('dve', lambda g, fc=fc, at=at: g.scalar_tensor_tensor(
                                at[:, fc, 0:gn], UC[:, 0:gn], 8.0, GC[:, 0:gn], ALU.min, ALU.mult),
                                reads=[UC, GC], writes=[at])
                        for s0 in range(0, gn, 128):
                            yo = YO[n % 2]
                            n += 1
                            for k in range(2):
                                bk = P[5 + k]
                                bk.started = False
                                for c in range(8):
                                    mm(bk, bk.tile[:, :], at[:, c, s0:s0 + 128], wdn[:, c, k * 512:(k + 1) * 512], [at, wdn])
                                tt('dve', yo[:, k * 512:(k + 1) * 512], bk.tile[:, :], bdn[:, k * 512:(k + 1) * 512], ALU.add,
                                   [bk.tile, bdn], [yo])
                            r0 = e * CAP + g0 + s0
                            S.dma('sp', YE[r0:r0 + 128, :], yo[:], reads=[yo], st=yo)

        def combine_phase(L, Xdst):
            with Phase(S) as ph:
                G = ph.sbuf("G", [128, 1024], F32)
                Bt = ph.sbuf("Bt", [128, 1024], F32)
                S.dma('sp', G[:], lnp[L, 2:3, :].to_broadcast([128, 1024]), writes=[G], st=G)
                S.dma('sp', Bt[:], lnp[L, 3:4, :].to_broadcast([128, 1024]), writes=[Bt], st=Bt)
                lt = ln_tiles(ph)
                YK = [[ph.sbuf("YK%d_%d" % (k, i), [128, 1024], F32) for i in range(2)] for k in range(4)]
                ACC = ph.sbuf("ACC", [128, 1024], F32)
                xt_ = [ph.sbuf("xt%d" % i, [128, 1024], F32) for i in range(2)]
                XN = [ph.sbuf("XN%d" % i, [128, 1024], F32) for i in range(2)]
                for i in range(NT):
                    rows = slice(128 * i, 128 * (i + 1))
                    yk = [YK[k][i % 2] for k in range(4)]
                    for k in range(4):
                        S.dma('pool', None, None, reads=[('GS', i)], writes=[yk[k]], st=yk[k],
                              fn=lambda q, k=k: q.indirect_dma_start(
                                  out=yk[k][:], out_offset=None, in_=YE,
                                  in_offset=bass.IndirectOffsetOnAxis(ap=GS[:, i, k:k + 1], axis=0)))
                    xx = xt_[i % 2]
                    S.dma('sp', xx[:], X1D[rows, :], writes=[xx], st=xx)
                    ts('dve', ACC[:], yk[0][:], GT[:, i, 0:1], None, ALU.mult, None, [yk[0], ('GT', i)], [ACC])
                    for k in range(1, 4):
                        S.op('dve', lambda g, k=k: g.scalar_tensor_tensor(
                            ACC[:], yk[k][:], GT[:, i, k:k + 1], ACC[:], ALU.mult, ALU.add),
                            reads=[yk[k], ('GT', i), ACC], writes=[ACC])
                    S.op('dve', lambda g: g.scalar_tensor_tensor(ACC[:], xx[:], DN_ALPHA, ACC[:], ALU.mult, ALU.add),
                         reads=[xx, ACC], writes=[ACC])
                    xn = XN[i % 2]
                    layer_norm(lt, ACC, xn, G, Bt)
                    S.dma('sp', Xdst[rows, :], xn[:], reads=[xn], st=xn)

        def zero_tok():
            tk = TOK.rearrange("(p a) b -> p (a b)", p=128)
            ncols = NEXP * CAP * 16 // 128
            for c0 in range(0, ncols, 1024):
                cn = min(1024, ncols - c0)
                S.dma('sp', tk[:, c0:c0 + cn], zer[:, 0:cn], reads=[zer], st=zer)

        je = jo = 0
        Xcur = x_in
        for L, kind in enumerate(layer_kinds):
            zero_tok()
            if kind == 'e':
                attn_pass(Xcur, wA[je], 1, C_MPA, ropeA[1], sinks[je], None)
                hgrn_pass(Xcur, wB[je], je, hnorm[je])
                oproj_phase(Xcur, L, 'e', woe[je])
                je += 1
            else:
                ret_pass(Xcur, wC[jo], rnorm[jo])
                for p, d in enumerate(DPAT):
                    attn_pass(Xcur, wDp[jo][p], d, C_MPD, ropeA[d], None, p)
                oproj_phase(Xcur, L, 'o', woo[jo])
                jo += 1
            expert_phase(L)
            Xdst = out_d if L == NL - 1 else XS
            combine_phase(L, Xdst)
            Xcur = XS
        S.barrier()
        print("bass program: ninst=%d" % S.ninst)
    return nc


def _rot_perm(nheads, hd, half):
    idx = np.arange(nheads * hd).reshape(nheads, hd).copy()
    for h in range(nheads):
        base = h * hd
        idx[h, :half] = base + np.arange(half, 2 * half)
        idx[h, half:2 * half] = base + np.arange(0, half)
    return idx.reshape(-1)


def _consts(T):
    NT = T // 128
    cst = np.zeros((128, NCST), np.float32)
    i = np.arange(128)
    cst[:, C_ID:C_ID + 128] = np.eye(128)
    kj, qi = i[:, None], i[None, :]
    cst[:, C_MCUR:C_MCUR + 128] = (qi >= kj)
    cst[:, C_MPA:C_MPA + 128] = (kj >= qi + 1)
    cst[:, C_MPD:C_MPD + 128] = (kj >= qi)
    cst[:, C_MBD:C_MBD + 128] = ((kj // 32) == (qi // 32)) & (kj <= qi)
    cst[:, C_TRI:C_TRI + 128] = (kj < qi)
    cst[:, C_ONE:C_ONE + 128] = 1.0
    cst[:, C_IOTA:C_IOTA + 32] = np.arange(32)[None, :]
    rs = np.ones(512)
    rs[::32] = 0.0
    cst[:, C_RS:C_RS + 512] = rs[None, :]
    lg = np.log1p(-np.exp2(-5.0 - np.arange(4, dtype=np.float64)))
    for h in range(4):
        rel = (qi - kj).astype(np.float64)
        cst[:, C_DT + h * 128:C_DT + (h + 1) * 128] = np.where(rel >= 0, np.exp(lg[h] * np.maximum(rel, 0)), 0.0)
        cst[:64, C_QD + h * 128:C_QD + (h + 1) * 128] = np.exp(lg[h] * (i + 1.0))[None, :]
        cst[:64, C_KD + h * 128:C_KD + (h + 1) * 128] = np.exp(lg[h] * (127.0 - i))[None, :]
    for jc in range(4):
        cst[:, C_RM + jc] = (i // 32 == jc)
    tokid = np.zeros((128, NT, 16), np.int32)
    tokid[:] = (np.arange(NT)[None, :, None] * 128 + np.arange(128)[:, None, None])
    pos = np.arange(T, dtype=np.float32)
    inv = (1.0 / (np.float32(500000.0) ** (np.arange(0, 16, 2, dtype=np.float32) / np.float32(16)))).astype(np.float32)
    ang = (pos[:, None] * inv[None, :]).astype(np.float32).astype(np.float64)
    Cn = np.ones((64, T))
    Sn = np.zeros((64, T))
    Cn[0:8] = np.cos(ang).T
    Cn[8:16] = np.cos(ang).T
    Sn[0:8] = -np.sin(ang).T
    Sn[8:16] = np.sin(ang).T
    rope = {}
    for d in DPAT:
        nb = T // (128 * d)
        perm = np.concatenate([r + d * (128 * b + np.arange(128)) for r in range(d) for b in range(nb)])
        rope[d] = (np.ascontiguousarray(Cn[:, perm], dtype=np.float32), np.ascontiguousarray(Sn[:, perm], dtype=np.float32))
    rinv = (1.0 / (np.float32(10000.0) ** np.linspace(0.0, 1.0, 32, dtype=np.float32))).astype(np.float32)
    rang = (pos[:, None] * rinv[None, :]).astype(np.float32).astype(np.float64)
    RC = np.concatenate([np.cos(rang).T, np.cos(rang).T], 0)
    RSn = np.concatenate([-np.sin(rang).T, np.sin(rang).T], 0)
    ret = [RC, RSn, RC * 0.125, RSn * 0.125]
    ret = [np.ascontiguousarray(a, dtype=np.float32) for a in ret]
    return cst, tokid.reshape(128, NT * 16), rope, ret


def _prep_shared(inp, layer_kinds, T):
    f = lambda a: np.ascontiguousarray(np.asarray(a), dtype=np.float32)
    cst, tokid, rope, ret = _consts(T)
    m = {"cst": cst, "tokid": tokid}
    for d in DPAT:
        m["ropeC%d" % d], m["ropeS%d" % d] = rope[d]
    for i in range(4):
        m["ret%d" % i] = ret[i]
    m["lbl"] = f(inp["hgrn_lb_logits"])[:2]
    pq = _rot_perm(8, 64, 8)
    pk = _rot_perm(2, 64, 8)
    pc = _rot_perm(4, 64, 32)
    NL = len(layer_kinds)
    je = jo = 0
    for L, k in enumerate(layer_kinds):
        if k == 'e':
            w = np.concatenate([f(inp["w_in_even"])[je], f(inp["b_in_even"])[je][None, :]], 0)
            q, kk, v = w[:, 0:512], w[:, 512:640], w[:, 640:768]
            m["wA%d" % je] = np.ascontiguousarray(np.concatenate([q, q[:, pq], kk, kk[:, pk], v], 1))
            m["wB%d" % je] = np.ascontiguousarray(w[:, 768:2816])
            m["sink%d" % je] = f(inp["attn_sinks"])[je][None, :]
            m["hnorm%d" % je] = f(inp["hgrn_norm"])[je][None, :]
            m["woe%d" % je] = f(inp["w_out_even"])[je]
            je += 1
        else:
            w = np.concatenate([f(inp["w_in_odd"])[jo], f(inp["b_in_odd"])[jo][None, :]], 0)
            cq, ck, cv, cg = w[:, 0:256], w[:, 256:512], w[:, 512:1024], w[:, 1024:1536]
            m["wC%d" % jo] = np.ascontiguousarray(np.concatenate([cq, cq[:, pc], ck, ck[:, pc], cv, cg], 1))
            for p in range(3):
                o = 1536 + p * 768
                q, kk, v = w[:, o:o + 512], w[:, o + 512:o + 640], w[:, o + 640:o + 768]
                m["wD%d_%d" % (jo, p)] = np.ascontiguousarray(np.concatenate([q, q[:, pq], kk, kk[:, pk], v], 1))
            m["rnorm%d" % jo] = f(inp["ret_norm"])[jo][None, :]
            m["woo%d" % jo] = f(inp["w_out_odd"])[jo]
            jo += 1
    m["lnp"] = np.ascontiguousarray(np.stack([f(inp["ln1_g"])[:NL], f(inp["ln1_b"])[:NL], f(inp["ln2_g"])[:NL],
                                              f(inp["ln2_b"])[:NL]], 1))
    m["rw"] = f(inp["router_w"])[:NL]
    m["rb"] = f(inp["router_b"])[:NL]
    m["ewgu"] = f(inp["expert_w_gu"])[:NL]
    m["ebgu"] = f(inp["expert_b_gu"])[:NL]
    m["ewdn"] = f(inp["expert_w_dn"])[:NL]
    m["ebdn"] = f(inp["expert_b_dn"])[:NL]
    return m


def run(inp, layer_kinds, CAP, debug=False):
    x = np.ascontiguousarray(np.asarray(inp["x"]), dtype=np.float32)
    B, T, _ = x.shape
    nc = build(T, layer_kinds, CAP, debug=debug)
    shared = _prep_shared(inp, layer_kinds, T)
    in_maps = []
    for b in range(B):
        mm_ = dict(shared)
        mm_["x"] = x[b]
        in_maps.append(mm_)
    res = run_bass_kernel_spmd(nc, in_maps, core_ids=list(range(B)))
    return res


def kernel(**inputs):
    res = run(inputs, ['e', 'o', 'e', 'o'], 1280)
    return np.stack([np.asarray(r["out"]) for r in res.results], 0).astype(np.float32)
```

```python
import numpy as np
from contextlib import ExitStack
import concourse.bass as bass
import concourse.mybir as mybir
from concourse.bass_utils import run_bass_kernel_spmd

F32 = mybir.dt.float32
BF16 = mybir.dt.bfloat16
I32 = mybir.dt.int32
U32 = mybir.dt.uint32
AF = mybir.ActivationFunctionType
ALU = mybir.AluOpType
AX = mybir.AxisListType

D = 1024
NEXP = 32
DN_ALPHA = 8 ** 0.25
LN_EPS = 1e-5
NORM_EPS = 1e-6
DPAT = (1, 4, 16)

C_ID, C_MCUR, C_MPA, C_MPD, C_MBD, C_TRI, C_ONE = 0, 128, 256, 384, 512, 640, 768
C_IOTA, C_RS, C_DT, C_QD, C_KD = 896, 928, 1440, 1952, 2464
C_RM = 2976
NCST = 2980


class DSem:
    def __init__(self, sem):
        self.sem = sem
        self.cnt = 0


class Tile:
    def __init__(self, t, name):
        self.t = t
        self.name = name
        self.ds = None

    def __getitem__(self, idx):
        return self.t[idx]


class Sched:
    def __init__(self, nc, ctx, ndsem=64):
        self.nc = nc
        self.ctx = ctx
        self.eng = {'pe': nc.tensor, 'act': nc.scalar, 'dve': nc.vector, 'pool': nc.gpsimd, 'sp': nc.sync}
        self.esem = {}
        self.ecnt = {}
        for e in ('pe', 'act', 'dve', 'pool'):
            self.esem[e] = ctx.enter_context(nc.semaphore('s_' + e))
            self.ecnt[e] = 0
        self.seen = {e: {} for e in self.eng}
        self.writers = {}
        self.readers = {}
        self.ninst = 0
        self.free_ds = [DSem(ctx.enter_context(nc.semaphore('d%d' % i))) for i in range(ndsem)]
        self.all_ds = list(self.free_ds)

    def _deps(self, reads, writes):
        need = {}
        for k in list(reads) + list(writes):
            for s, v in self.writers.get(k, {}).items():
                if need.get(s, (None, 0))[1] < v[1]:
                    need[s] = v
        for k in writes:
            for s, v in self.readers.get(k, {}).items():
                if need.get(s, (None, 0))[1] < v[1]:
                    need[s] = v
        return need

    def _wait(self, e, need, skip=None):
        eng = self.eng[e]
        seen = self.seen[e]
        for s, (sem, val) in need.items():
            if s == skip or seen.get(s, 0) >= val:
                continue
            eng.wait_ge(sem, val)
            seen[s] = val
            self.ninst += 1

    def _commit(self, ev, reads, writes):
        s = id(ev[0])
        for k in writes:
            self.writers[k] = {s: ev}
            self.readers[k] = {}
        for k in reads:
            if k not in writes:
                self.readers.setdefault(k, {})[s] = ev

    def op(self, e, fn, reads=(), writes=()):
        need = self._deps(reads, writes)
        sem = self.esem[e]
        self._wait(e, need, id(sem) if e == 'pe' else None)
        ins = fn(self.eng[e])
        self.ecnt[e] += 1
        ins.then_inc(sem, 1)
        self.ninst += 1
        self._commit((sem, self.ecnt[e]), reads, writes)

    def dma(self, q, out, in_, reads=(), writes=(), st=None, fn=None):
        if st.ds is None:
            st.ds = self.free_ds.pop()
        ds = st.ds
        need = self._deps(reads, writes)
        if ds.cnt > 0:
            need[id(ds.sem)] = (ds.sem, ds.cnt)
        self._wait(q, need)
        ins = self.eng[q].dma_start(out=out, in_=in_) if fn is None else fn(self.eng[q])
        ds.cnt += 16
        ins.then_inc(ds.sem, 16)
        self.ninst += 1
        self._commit((ds.sem, ds.cnt), reads, writes)

    def barrier(self):
        need = {}
        for e in self.esem:
            if self.ecnt[e] > 0:
                need[id(self.esem[e])] = (self.esem[e], self.ecnt[e])
        for ds in self.all_ds:
            if ds.cnt > 0:
                need[id(ds.sem)] = (ds.sem, ds.cnt)
        for e in self.eng:
            self._wait(e, need)


class Phase:
    uid = 0

    def __init__(self, S):
        self.S = S
        self.stack = ExitStack()
        self.tiles = []

    def __enter__(self):
        self.stack.__enter__()
        return self

    def sbuf(self, name, shape, dt):
        Phase.uid += 1
        name = "%s_u%d" % (name, Phase.uid)
        t = Tile(self.stack.enter_context(self.S.nc.sbuf_tensor(name, list(shape), dt)), name)
        self.tiles.append(t)
        return t

    def __exit__(self, *a):
        self.S.barrier()
        for t in self.tiles:
            if t.ds is not None:
                self.S.free_ds.append(t.ds)
                t.ds = None
        return self.stack.__exit__(*a)


class Bank:
    def __init__(self, tile):
        self.tile = tile
        self.started = False


def build(T, layer_kinds, CAP, debug=False):
    NT = T // 128
    NL = len(layer_kinds)
    n_even = sum(1 for k in layer_kinds if k == 'e')
    n_odd = NL - n_even
    nc = bass.Bass("TRN2", target_bir_lowering=False)

    def din(name, shape, dt=F32):
        return nc.dram_tensor(name, list(shape), dt, kind="ExternalInput").ap()

    def dscr(name, shape, dt=F32):
        kind = "ExternalOutput" if debug else "Internal"
        return nc.dram_tensor(name, list(shape), dt, kind=kind).ap()

    x_in = din("x", [T, D])
    cst_d = din("cst", [128, NCST])
    tokid_d = din("tokid", [128, NT * 16], I32)
    ropeA = {d: (din("ropeC%d" % d, [64, T]), din("ropeS%d" % d, [64, T])) for d in DPAT}
    ret_t = [din("ret%d" % i, [64, T]) for i in range(4)]
    lbl_d = din("lbl", [2, 512])
    wA = [din("wA%d" % j, [1025, 1408]) for j in range(n_even)]
    wB = [din("wB%d" % j, [1025, 2048]) for j in range(n_even)]
    sinks = [din("sink%d" % j, [1, 8]) for j in range(n_even)]
    hnorm = [din("hnorm%d" % j, [1, 512]) for j in range(n_even)]
    woe = [din("woe%d" % j, [1024, 1024]) for j in range(n_even)]
    wC = [din("wC%d" % j, [1025, 2048]) for j in range(n_odd)]
    wDp = [[din("wD%d_%d" % (j, p), [1025, 1408]) for p in range(3)] for j in range(n_odd)]
    rnorm = [din("rnorm%d" % j, [1, 512]) for j in range(n_odd)]
    woo = [din("woo%d" % j, [1024, 1024]) for j in range(n_odd)]
    lnp = din("lnp", [NL, 4, 1024])
    rw = din("rw", [NL, 1024, 32])
    rb = din("rb", [NL, 32])
    ewgu = din("ewgu", [NL, NEXP, 1024, 2048])
    ebgu = din("ebgu", [NL, NEXP, 2048])
    ewdn = din("ewdn", [NL, NEXP, 1024, 1024])
    ebdn = din("ebdn", [NL, NEXP, 1024])
    out_d = nc.dram_tensor("out", [T, D], F32, kind="ExternalOutput").ap()

    XS = dscr("XS", [T, D])
    YMIX = dscr("YMIX", [T, D], BF16)
    UD = [dscr("UD%d" % p, [T, 520]) for p in range(3)]
    X1D = dscr("X1D", [T, D])
    X1B = dscr("X1B", [T, D], BF16)
    YE = dscr("YE", [NEXP * CAP, D])
    TOK = dscr("TOK", [NEXP * CAP, 16], I32)

    with ExitStack() as ctx:
        S = Sched(nc, ctx)

        def gsb(name, shape, dt):
            return Tile(ctx.enter_context(nc.sbuf_tensor("g_" + name, list(shape), dt)), name)

        P = [Bank(Tile(ctx.enter_context(nc.psum_tensor("ps%d" % i, [128, 512], F32)), "ps%d" % i)) for i in range(8)]

        def mm(bank, out, lhsT, rhs, reads):
            st = not bank.started
            bank.started = True
            S.op('pe', lambda e: e.matmul(out, lhsT=lhsT, rhs=rhs, start=st, stop=True, skip_group_check=True),
                 reads=reads, writes=[bank.tile])

        def tr(bank, out, in_, ident, reads):
            S.op('pe', lambda e: e.transpose(out, in_, ident), reads=reads, writes=[bank.tile])

        def tt(e, out, a, b, op, reads, writes):
            S.op(e, lambda g: g.tensor_tensor(out, a, b, op), reads=reads, writes=writes)

        def ts(e, out, a, s1, s2, op0, op1, reads, writes, **kw):
            if op1 is None:
                S.op(e, lambda g: g.tensor_scalar(out, a, s1, None, op0, **kw), reads=reads, writes=writes)
            else:
                S.op(e, lambda g: g.tensor_scalar(out, a, s1, s2, op0, op1, **kw), reads=reads, writes=writes)

        def act(out, in_, func, reads, writes, **kw):
            S.op('act', lambda g: g.activation(out, in_, func, **kw), reads=reads, writes=writes)

        cst = gsb("cst", [128, NCST], F32)
        S.dma('sp', cst[:], cst_d, writes=[cst], st=cst)
        cb = gsb("cb", [128, 896], BF16)
        S.op('dve', lambda g: g.tensor_copy(cb[:], cst[:, 0:896]), reads=[cst], writes=[cb])
        identb = cb[:, C_ID:C_ID + 128]
        identf = cst[:, C_ID:C_ID + 128]
        tokid = gsb("tokid", [128, NT, 16], I32)
        S.dma('sp', tokid[:].rearrange("p a b -> p (a b)"), tokid_d, writes=[tokid], st=tokid)
        onesb = gsb("onesb", [1, 512], BF16)
        S.op('dve', lambda g: g.memset(onesb[:], 1.0), writes=[onesb])
        onesf = gsb("onesf", [1, 128], F32)
        S.op('dve', lambda g: g.memset(onesf[:], 1.0), writes=[onesf])
        GS = gsb("GS", [128, NT, 4], I32)
        GT = gsb("GT", [128, NT, 4], F32)
        zer = gsb("zer", [128, 1024], I32)
        S.op('dve', lambda g: g.memset(zer[:], 0), writes=[zer])
        S.barrier()

        CK = [cst, cb]

        def load_xT(ph_t, rows_ap, n):
            xb = ph_t['xb'][n % 2]
            xT = ph_t['xT'][n % 2]
            S.dma('pool', xb[:], rows_ap, writes=[xb], st=xb)
            pt = P[0].tile[:].bitcast(BF16)
            for c in range(8):
                tr(P[0], pt[:, c * 128:(c + 1) * 128], xb[:, c * 128:(c + 1) * 128], identb, [xb] + CK)
            S.op('act', lambda g: g.copy(xT[:].rearrange("p a b -> p (a b)"), pt), reads=[P[0].tile], writes=[xT])
            return xT

        def xt_tiles(ph):
            return {'xb': [ph.sbuf("xb%d" % i, [128, 1024], BF16) for i in range(2)],
                    'xT': [ph.sbuf("xT%d" % i, [128, 8, 128], BF16) for i in range(2)]}

        def load_w(ph, name, wd, ncol):
            W = ph.sbuf(name, [128, 8, ncol], BF16)
            S.dma('pool', W[:], wd[0:1024, :].rearrange("(c p) n -> p c n", p=128), writes=[W], st=W)
            Wb = ph.sbuf(name + "b", [1, ncol], BF16)
            S.dma('pool', Wb[:], wd[1024:1025, :], writes=[Wb], st=Wb)
            return W, Wb

        def attn_pass(Xsrc, wd, d, mprev_off, tabs, sink_d, pat):
            nb = T // (128 * d)
            Xr = Xsrc.rearrange("(l r) n -> r l n", r=d)
            with Phase(S) as ph:
                W, Wb = load_w(ph, "aw", wd, 1408)
                xt = xt_tiles(ph)
                ct = [ph.sbuf("ct%d" % i, [64, 128], F32) for i in range(2)]
                stt_ = [ph.sbuf("st%d" % i, [64, 128], F32) for i in range(2)]
                T1 = ph.sbuf("T1", [64, 512], F32)
                T2 = ph.sbuf("T2", [64, 512], F32)
                QT = ph.sbuf("QT", [64, 8, 128], BF16)
                KT = [ph.sbuf("KT%d" % i, [64, 2, 128], BF16) for i in range(2)]
                VA = [ph.sbuf("VA%d" % i, [128, 2, 65], BF16) for i in range(2)]
                EE = [ph.sbuf("EE%d" % i, [128, 512], BF16) for i in range(4)]
                PM = [ph.sbuf("PM%d" % i, [128, 512], BF16) for i in range(4)]
                U = [ph.sbuf("U%d" % i, [128, 8, 65], F32) for i in range(2)]
                for i in range(2):
                    S.op('dve', lambda g, i=i: g.memset(VA[i][:], 1.0), writes=[VA[i]])
                if sink_d is not None:
                    esk = ph.sbuf("esk", [128, 8], F32)
                    S.dma('sp', esk[:], sink_d.to_broadcast([128, 8]), writes=[esk], st=esk)
                    act(esk[:], esk[:], AF.Exp, [esk], [esk])
                    DEN = ph.sbuf("DEN", [128, 8], F32)
                    YA = [ph.sbuf("YA%d" % i, [128, 8, 64], BF16) for i in range(2)]
                mcur = cb[:, C_MCUR:C_MCUR + 128]
                mprev = cb[:, mprev_off:mprev_off + 128]
                n = 0
                for r in range(d):
                    for b in range(nb):
                        cur, prv = n % 2, (n + 1) % 2
                        xT = load_xT(xt, Xr[r, 128 * b:128 * (b + 1), :], n)
                        c_t, s_t = ct[n % 2], stt_[n % 2]
                        S.dma('sp', c_t[:], tabs[0][:, n * 128:(n + 1) * 128], writes=[c_t], st=c_t)
                        S.dma('sp', s_t[:], tabs[1][:, n * 128:(n + 1) * 128], writes=[s_t], st=s_t)
                        for bk in P[1:7]:
                            bk.started = False
                        for h in range(8):
                            for (bq, off) in ((P[1 + h // 4], 0), (P[3 + h // 4], 512)):
                                o_ = bq.tile[0:64, (h % 4) * 128:(h % 4 + 1) * 128]
                                for c in range(8):
                                    mm(bq, o_, W[:, c, off + h * 64:off + (h + 1) * 64], xT[:, c, :], [W, xT])
                                mm(bq, o_, Wb[0:1, off + h * 64:off + (h + 1) * 64], onesb[0:1, 0:128], [Wb, onesb])
                        for kk in range(4):
                            o_ = P[5].tile[0:64, kk * 128:(kk + 1) * 128]
                            for c in range(8):
                                mm(P[5], o_, W[:, c, 1024 + kk * 64:1024 + (kk + 1) * 64], xT[:, c, :], [W, xT])
                            mm(P[5], o_, Wb[0:1, 1024 + kk * 64:1024 + (kk + 1) * 64], onesb[0:1, 0:128], [Wb, onesb])
                        for c in range(8):
                            mm(P[6], P[6].tile[:, 0:128], xT[:, c, :], W[:, c, 1280:1408], [W, xT])
                        mm(P[6], P[6].tile[:, 0:128], onesb[0:1, 0:128], Wb[0:1, 1280:1408], [Wb, onesb])
                        cbq = c_t[:].unsqueeze(1).to_broadcast([64, 4, 128])
                        sbq = s_t[:].unsqueeze(1).to_broadcast([64, 4, 128])
                        for g_ in range(2):
                            tt('dve', T1[:].rearrange("p (a b) -> p a b", a=4),
                               P[1 + g_].tile[0:64, :].rearrange("p (a b) -> p a b", a=4), cbq, ALU.mult,
                               [P[1 + g_].tile, c_t], [T1])
                            tt('dve', T2[:].rearrange("p (a b) -> p a b", a=4),
                               P[3 + g_].tile[0:64, :].rearrange("p (a b) -> p a b", a=4), sbq, ALU.mult,
                               [P[3 + g_].tile, s_t], [T2])
                            tt('dve', QT[:, 4 * g_:4 * g_ + 4, :].rearrange("p a b -> p (a b)"), T1[:], T2[:], ALU.add,
                               [T1, T2], [QT])
                        cbk = c_t[:].unsqueeze(1).to_broadcast([64, 2, 128])
                        sbk = s_t[:].unsqueeze(1).to_broadcast([64, 2, 128])
                        tt('dve', T1[:, 0:256].rearrange("p (a b) -> p a b", a=2),
                           P[5].tile[0:64, 0:256].rearrange("p (a b) -> p a b", a=2), cbk, ALU.mult,
                           [P[5].tile, c_t], [T1])
                        tt('dve', T2[:, 0:256].rearrange("p (a b) -> p a b", a=2),
                           P[5].tile[0:64, 256:512].rearrange("p (a b) -> p a b", a=2), sbk, ALU.mult,
                           [P[5].tile, s_t], [T2])
                        tt('dve', KT[cur][:].rearrange("p a b -> p (a b)"), T1[:, 0:256], T2[:, 0:256], ALU.add,
                           [T1, T2], [KT[cur]])
                        S.op('act', lambda g: g.copy(VA[cur][:, :, 0:64],
                                                     P[6].tile[:, 0:128].rearrange("p (a b) -> p a b", a=2)),
                             reads=[P[6].tile], writes=[VA[cur]])
                        for bk in P[1:7]:
                            bk.started = False
                        combos = []
                        for j in range(2):
                            mm(P[1 + j], P[1 + j].tile[:, :], KT[cur][:, j, :],
                               QT[:, 4 * j:4 * j + 4, :].rearrange("p a b -> p (a b)"), [KT[cur], QT])
                            combos.append((j, cur, P[1 + j], mcur, j))
                            if b > 0:
                                mm(P[3 + j], P[3 + j].tile[:, :], KT[prv][:, j, :],
                                   QT[:, 4 * j:4 * j + 4, :].rearrange("p a b -> p (a b)"), [KT[prv], QT])
                                combos.append((j, prv, P[3 + j], mprev, 2 + j))
                        for (j, pc, bk, msk, ei) in combos:
                            act(EE[ei][:], bk.tile[:, :], AF.Exp, [bk.tile], [EE[ei]], scale=0.125)
                            tt('dve', PM[ei][:].rearrange("p (a b) -> p a b", a=4),
                               EE[ei][:].rearrange("p (a b) -> p a b", a=4),
                               msk.unsqueeze(1).to_broadcast([128, 4, 128]), ALU.mult, [EE[ei]] + CK, [PM[ei]])
                        for (j, pc, bk, msk, ei) in combos:
                            for hh in range(4):
                                h = 4 * j + hh
                                ob = P[5 + h // 4]
                                mm(ob, ob.tile[:, (h % 4) * 65:(h % 4 + 1) * 65], PM[ei][:, hh * 128:(hh + 1) * 128],
                                   VA[pc][:, j, :], [PM[ei], VA[pc]])
                        Ut = U[n % 2]
                        for g_ in range(2):
                            S.op('act', lambda g, g_=g_: g.copy(Ut[:, 4 * g_:4 * g_ + 4, :].rearrange("p a b -> p (a b)"),
                                                                P[5 + g_].tile[:, 0:260]),
                                 reads=[P[5 + g_].tile], writes=[Ut])
                        rows = slice(128 * b, 128 * (b + 1))
                        if sink_d is not None:
                            tt('dve', DEN[:], Ut[:, :, 64], esk[:], ALU.add, [Ut, esk], [DEN])
                            S.op('dve', lambda g: g.reciprocal(DEN[:], DEN[:]), reads=[DEN], writes=[DEN])
                            ya = YA[n % 2]
                            tt('dve', ya[:], Ut[:, :, 0:64], DEN[:].unsqueeze(2).to_broadcast([128, 8, 64]), ALU.mult,
                               [Ut, DEN], [ya])
                            S.dma('sp', YMIX[rows, 0:512], ya[:].rearrange("p a b -> p (a b)"), reads=[ya], st=ya)
                        else:
                            S.dma('sp', UD[pat].rearrange("(l r) n -> r l n", r=d)[r, rows, :],
                                  Ut[:].rearrange("p a b -> p (a b)"), reads=[Ut], st=Ut)
                        n += 1

        def hgrn_pass(Xsrc, wd, j_even, hn_d):
            with Phase(S) as ph:
                W, Wb = load_w(ph, "bw", wd, 2048)
                xt = xt_tiles(ph)
                LB = ph.sbuf("LB", [128, 4], F32)
                OML = ph.sbuf("OML", [128, 4], F32)
                if j_even == 0:
                    S.op('dve', lambda g: g.memset(LB[:], 0.0), writes=[LB])
                else:
                    LL = ph.sbuf("LL", [128, 2, 4], F32)
                    S.dma('sp', LL[:], lbl_d.rearrange("l (h c) -> c l h", c=128), writes=[LL], st=LL,
                          fn=lambda q: q.dma_start(out=LL[:], in_=lbl_d.rearrange("l (h c) -> c l h", c=128),
                                                   allow_slow_non_contiguous=True))
                    tt('dve', LB[:], LL[:, 0, :], LL[:, 1, :], ALU.subtract, [LL], [LB])
                    act(LB[:], LB[:], AF.Sigmoid, [LB], [LB])
                ts('dve', OML[:], LB[:], -1.0, 1.0, ALU.mult, ALU.add, [LB], [OML])
                GB = ph.sbuf("GB", [128, 512], F32)
                S.dma('sp', GB[:], hn_d.to_broadcast([128, 512]), writes=[GB], st=GB)
                Fv = ph.sbuf("Fv", [128, 4, 128], F32)
                LF = ph.sbuf("LF", [128, 512], F32)
                KEY = ph.sbuf("KEY", [128, 512], F32)
                Bc = ph.sbuf("Bc", [128, 512], F32)
                EB = ph.sbuf("EB", [128, 512], F32)
                QTl = ph.sbuf("QTl", [128, 4, 128], BF16)
                KTl = ph.sbuf("KTl", [128, 4, 128], BF16)
                KB = ph.sbuf("KB", [128, 4, 128], BF16)
                EG = ph.sbuf("EG", [128, 16], F32)
                ATm = ph.sbuf("ATm", [128, 512], BF16)
                KBT = ph.sbuf("KBT", [128, 4, 128], BF16)
                V = ph.sbuf("V", [128, 512], BF16)
                SG = ph.sbuf("SG", [128, 512], F32)
                St = [ph.sbuf("St%d" % h, [128, 128], F32) for h in range(4)]
                Sb = [ph.sbuf("Sb%d" % h, [128, 128], BF16) for h in range(4)]
                SQ = ph.sbuf("SQ", [128, 512], F32)
                SS = ph.sbuf("SS", [128, 4], F32)
                YB = [ph.sbuf("YB%d" % i, [128, 512], BF16) for i in range(2)]
                QZ = [ph.sbuf("QZ%d" % h, [128, 4, 128], BF16) for h in range(4)]
                KZ = [ph.sbuf("KZ%d" % h, [128, 512], BF16) for h in range(4)]
                for h in range(4):
                    S.op('dve', lambda g, h=h: g.memset(St[h][:], 0.0), writes=[St[h]])
                    S.op('dve', lambda g, h=h: g.memset(Sb[h][:], 0.0), writes=[Sb[h]])
                    S.op('dve', lambda g, h=h: g.memset(QZ[h][:], 0.0), writes=[QZ[h]])
                rs = cst[:, C_RS:C_RS + 512]
                mbd = cb[:, C_MBD:C_MBD + 128]
                F2 = Fv[:].rearrange("p a b -> p (a b)")
                for i in range(NT):
                    xT = load_xT(xt, Xsrc[128 * i:128 * (i + 1), :], i)
                    for bk in P[1:8]:
                        bk.started = False
                    for h in range(4):
                        for (bk, off) in ((P[1], 0), (P[2], 512)):
                            o_ = bk.tile[:, h * 128:(h + 1) * 128]
                            for c in range(8):
                                mm(bk, o_, W[:, c, off + h * 128:off + (h + 1) * 128], xT[:, c, :], [W, xT])
                            mm(bk, o_, Wb[0:1, off + h * 128:off + (h + 1) * 128], onesb[0:1, 0:128], [Wb, onesb])
                    for (bk, off) in ((P[5], 1024), (P[6], 1536)):
                        for c in range(8):
                            mm(bk, bk.tile[:, :], xT[:, c, :], W[:, c, off:off + 512], [W, xT])
                        mm(bk, bk.tile[:, :], onesb[0:1, 0:128], Wb[0:1, off:off + 512], [Wb, onesb])
                    act(F2, P[2].tile[:, :], AF.Sigmoid, [P[2].tile], [Fv])
                    tt('dve', Fv[:], Fv[:], OML[:].unsqueeze(2).to_broadcast([128, 4, 128]), ALU.mult, [Fv, OML], [Fv])
                    tt('dve', Fv[:], Fv[:], LB[:].unsqueeze(2).to_broadcast([128, 4, 128]), ALU.add, [Fv, LB], [Fv])
                    ts('dve', F2, F2, 1e-30, None, ALU.max, None, [Fv], [Fv])
                    act(LF[:], F2, AF.Ln, [Fv], [LF])
                    ts('dve', KEY[:], F2, -1.0, 1.0, ALU.mult, ALU.add, [Fv], [KEY])
                    S.op('dve', lambda g: g.tensor_tensor_scan(Bc[:], rs, LF[:], 0.0, ALU.mult, ALU.add),
                         reads=[LF] + CK, writes=[Bc])
                    act(EB[:], Bc[:], AF.Exp, [Bc], [EB])
                    tt('dve', QTl[:].rearrange("p a b -> p (a b)"), P[1].tile[:, :], EB[:], ALU.mult, [P[1].tile, EB], [QTl])
                    act(EB[:], Bc[:], AF.Exp, [Bc], [EB], scale=-1.0)
                    tt('dve', KTl[:].rearrange("p a b -> p (a b)"), KEY[:], EB[:], ALU.mult, [KEY, EB], [KTl])
                    BL = Bc[:].rearrange("p (a b) -> p a b", b=32)[:, :, 31:32]
                    tt('dve', LF[:].rearrange("p (a b) -> p a b", b=32), BL.to_broadcast([128, 16, 32]),
                       Bc[:].rearrange("p (a b) -> p a b", b=32), ALU.subtract, [Bc], [LF])
                    act(EB[:], LF[:], AF.Exp, [LF], [EB])
                    tt('dve', KB[:].rearrange("p a b -> p (a b)"), KEY[:], EB[:], ALU.mult, [KEY, EB], [KB])
                    act(EG[:].unsqueeze(2), BL, AF.Exp, [Bc], [EG])
                    for h in range(4):
                        mm(P[3], P[3].tile[:, h * 128:(h + 1) * 128], KTl[:, h, :], QTl[:, h, :], [KTl, QTl])
                    tt('dve', ATm[:].rearrange("p (a b) -> p a b", a=4), P[3].tile[:, :].rearrange("p (a b) -> p a b", a=4),
                       mbd.unsqueeze(1).to_broadcast([128, 4, 128]), ALU.mult, [P[3].tile] + CK, [ATm])
                    p4 = P[4].tile[:].bitcast(BF16)
                    for h in range(4):
                        tr(P[4], p4[:, h * 128:(h + 1) * 128], KB[:, h, :], identb, [KB] + CK)
                    S.op('act', lambda g: g.copy(KBT[:].rearrange("p a b -> p (a b)"), p4[:, 0:512]),
                         reads=[P[4].tile], writes=[KBT])
                    S.op('act', lambda g: g.copy(V[:], P[5].tile[:, :]), reads=[P[5].tile], writes=[V])
                    act(SG[:], P[6].tile[:, :], AF.Silu, [P[6].tile], [SG])
                    tt('dve', SG[:], SG[:], GB[:], ALU.mult, [SG, GB], [SG])
                    for h in range(4):
                        mm(P[7], P[7].tile[:, h * 128:(h + 1) * 128], ATm[:, h * 128:(h + 1) * 128],
                           V[:, h * 128:(h + 1) * 128], [ATm, V])
                    for jc in range(4):
                        pr = slice(32 * jc, 32 * jc + 32)
                        S.op('dve', lambda g, jc=jc, pr=pr: g.tensor_copy(QZ[jc][:, :, pr], QTl[:, :, pr]),
                             reads=[QTl], writes=[QZ[jc]])
                        S.op('act', lambda g, jc=jc: g.activation(KZ[jc][:], KBT[:].rearrange("p a b -> p (a b)"), AF.Copy,
                                                                  scale=cst[:, C_RM + jc:C_RM + jc + 1]),
                             reads=[KBT] + CK, writes=[KZ[jc]])
                    for jc in range(4):
                        for h in range(4):
                            mm(P[7], P[7].tile[:, h * 128:(h + 1) * 128], QZ[jc][:, h, :], Sb[h][:], [QZ[jc], Sb[h]])
                        P[2].started = False
                        for h in range(4):
                            mm(P[2], P[2].tile[:, h * 128:(h + 1) * 128], KZ[jc][:, h * 128:(h + 1) * 128],
                               V[:, h * 128:(h + 1) * 128], [KZ[jc], V])
                        for h in range(4):
                            S.op('dve', lambda g, h=h: g.scalar_tensor_tensor(
                                St[h][:], St[h][:], EG[:, h * 4 + jc:h * 4 + jc + 1], P[2].tile[:, h * 128:(h + 1) * 128],
                                ALU.mult, ALU.add), reads=[St[h], EG, P[2].tile], writes=[St[h]])
                            S.op('act', lambda g, h=h: g.copy(Sb[h][:], St[h][:]), reads=[St[h]], writes=[Sb[h]])
                    act(SQ[:], P[7].tile[:, :], AF.Square, [P[7].tile], [SQ])
                    S.op('dve', lambda g: g.tensor_reduce(SS[:], SQ[:].rearrange("p (a b) -> p a b", a=4), AX.X, ALU.add),
                         reads=[SQ], writes=[SS])
                    ts('dve', SS[:], SS[:], 1.0 / 128, NORM_EPS, ALU.mult, ALU.add, [SS], [SS])
                    act(SS[:], SS[:], AF.Sqrt, [SS], [SS])
                    S.op('dve', lambda g: g.reciprocal(SS[:], SS[:]), reads=[SS], writes=[SS])
                    tt('dve', SQ[:].rearrange("p (a b) -> p a b", a=4), P[7].tile[:, :].rearrange("p (a b) -> p a b", a=4),
                       SS[:].unsqueeze(2).to_broadcast([128, 4, 128]), ALU.mult, [P[7].tile, SS], [SQ])
                    yb = YB[i % 2]
                    tt('dve', yb[:], SQ[:], SG[:], ALU.mult, [SQ, SG], [yb])
                    S.dma('sp', YMIX[128 * i:128 * (i + 1), 512:1024], yb[:], reads=[yb], st=yb)

        def ret_pass(Xsrc, wd, rn_d):
            lg = [np.log1p(-2.0 ** (-5.0 - h)) for h in range(4)]
            with Phase(S) as ph:
                W, Wb = load_w(ph, "cw", wd, 2048)
                xt = xt_tiles(ph)
                GB = ph.sbuf("GB", [128, 512], F32)
                S.dma('sp', GB[:], rn_d.to_broadcast([128, 512]), writes=[GB], st=GB)
                tb = [[ph.sbuf("tb%d_%d" % (k, i), [64, 128], F32) for i in range(2)] for k in range(4)]
                T1 = ph.sbuf("T1", [64, 512], F32)
                T2 = ph.sbuf("T2", [64, 512], F32)
                QT = ph.sbuf("QT", [64, 4, 128], BF16)
                KT = ph.sbuf("KT", [64, 4, 128], BF16)
                QH = ph.sbuf("QH", [64, 4, 128], BF16)
                KL = ph.sbuf("KL", [64, 4, 128], BF16)
                KTT = ph.sbuf("KTT", [128, 4, 64], BF16)
                SCm = ph.sbuf("SCm", [128, 512], BF16)
                V = ph.sbuf("V", [128, 512], BF16)
                SG = ph.sbuf("SG", [128, 512], F32)
                St = [ph.sbuf("St%d" % h, [64, 128], F32) for h in range(4)]
                Sb = [ph.sbuf("Sb%d" % h, [64, 128], BF16) for h in range(4)]
                OC = ph.sbuf("OC", [128, 512], F32)
                SQ = ph.sbuf("SQ", [128, 512], F32)
                SS = ph.sbuf("SS", [128, 4], F32)
                MS = ph.sbuf("MS", [128, 4], F32)
                YB = [ph.sbuf("YB%d" % i, [128, 512], BF16) for i in range(2)]
                for h in range(4):
                    S.op('dve', lambda g, h=h: g.memset(St[h][:], 0.0), writes=[St[h]])
                    S.op('dve', lambda g, h=h: g.memset(Sb[h][:], 0.0), writes=[Sb[h]])
                DT = cst[:, C_DT:C_DT + 512]
                QD = cst[0:64, C_QD:C_QD + 512]
                KD = cst[0:64, C_KD:C_KD + 512]
                for i in range(NT):
                    xT = load_xT(xt, Xsrc[128 * i:128 * (i + 1), :], i)
                    tbs = [tb[k][i % 2] for k in range(4)]
                    for k in range(4):
                        S.dma('sp', tbs[k][:], ret_t[k][:, 128 * i:128 * (i + 1)], writes=[tbs[k]], st=tbs[k])
                    for bk in P[1:8]:
                        bk.started = False
                    for h in range(4):
                        for (bk, off) in ((P[1], 0), (P[2], 256), (P[3], 512), (P[4], 768)):
                            o_ = bk.tile[0:64, h * 128:(h + 1) * 128]
                            for c in range(8):
                                mm(bk, o_, W[:, c, off + h * 64:off + (h + 1) * 64], xT[:, c, :], [W, xT])
                            mm(bk, o_, Wb[0:1, off + h * 64:off + (h + 1) * 64], onesb[0:1, 0:128], [Wb, onesb])
                    for (bk, off) in ((P[5], 1024), (P[6], 1536)):
                        for c in range(8):
                            mm(bk, bk.tile[:, :], xT[:, c, :], W[:, c, off:off + 512], [W, xT])
                        mm(bk, bk.tile[:, :], onesb[0:1, 0:128], Wb[0:1, off:off + 512], [Wb, onesb])
                    for (dst, pa, pb, ta, tb_) in ((QT, P[1], P[2], tbs[0], tbs[1]), (KT, P[3], P[4], tbs[2], tbs[3])):
                        tt('dve', T1[:].rearrange("p (a b) -> p a b", a=4), pa.tile[0:64, :].rearrange("p (a b) -> p a b", a=4),
                           ta[:].unsqueeze(1).to_broadcast([64, 4, 128]), ALU.mult, [pa.tile, ta], [T1])
                        tt('dve', T2[:].rearrange("p (a b) -> p a b", a=4), pb.tile[0:64, :].rearrange("p (a b) -> p a b", a=4),
                           tb_[:].unsqueeze(1).to_broadcast([64, 4, 128]), ALU.mult, [pb.tile, tb_], [T2])
                        tt('dve', dst[:].rearrange("p a b -> p (a b)"), T1[:], T2[:], ALU.add, [T1, T2], [dst])
                    tt('dve', QH[:].rearrange("p a b -> p (a b)"), QT[:].rearrange("p a b -> p (a b)"), QD, ALU.mult,
                       [QT] + CK, [QH])
                    tt('dve', KL[:].rearrange("p a b -> p (a b)"), KT[:].rearrange("p a b -> p (a b)"), KD, ALU.mult,
                       [KT] + CK, [KL])
                    S.op('act', lambda g: g.copy(V[:], P[5].tile[:, :]), reads=[P[5].tile], writes=[V])
                    act(SG[:], P[6].tile[:, :], AF.Silu, [P[6].tile], [SG])
                    tt('dve', SG[:], SG[:], GB[:], ALU.mult, [SG, GB], [SG])
                    P[1].started = False
                    for h in range(4):
                        mm(P[1], P[1].tile[:, h * 128:(h + 1) * 128], KT[:, h, :], QT[:, h, :], [KT, QT])
                    tt('dve', SCm[:], P[1].tile[:, :], DT, ALU.mult, [P[1].tile] + CK, [SCm])
                    p3 = P[3].tile[:].bitcast(BF16)
                    for h in range(4):
                        tr(P[3], p3[:, h * 64:(h + 1) * 64], KL[:, h, :], cb[0:64, C_ID:C_ID + 64], [KL] + CK)
                    S.op('act', lambda g: g.copy(KTT[:].rearrange("p a b -> p (a b)"), p3[:, 0:256]),
                         reads=[P[3].tile], writes=[KTT])
                    for h in range(4):
                        mm(P[7], P[7].tile[:, h * 128:(h + 1) * 128], SCm[:, h * 128:(h + 1) * 128],
                           V[:, h * 128:(h + 1) * 128], [SCm, V])
                    for h in range(4):
                        mm(P[7], P[7].tile[:, h * 128:(h + 1) * 128], QH[:, h, :], Sb[h][:], [QH, Sb[h]])
                    P[2].started = False
                    for h in range(4):
                        mm(P[2], P[2].tile[0:64, h * 128:(h + 1) * 128], KTT[:, h, :], V[:, h * 128:(h + 1) * 128], [KTT, V])
                    for h in range(4):
                        cdh = float(np.exp(lg[h] * 128))
                        S.op('dve', lambda g, h=h, cdh=cdh: g.scalar_tensor_tensor(
                            St[h][:], St[h][:], cdh, P[2].tile[0:64, h * 128:(h + 1) * 128], ALU.mult, ALU.add),
                            reads=[St[h], P[2].tile], writes=[St[h]])
                        S.op('act', lambda g, h=h: g.copy(Sb[h][:], St[h][:]), reads=[St[h]], writes=[Sb[h]])
                    p7 = P[7].tile[:, :].rearrange("p (a b) -> p a b", a=4)
                    S.op('dve', lambda g: g.tensor_reduce(MS[:], p7, AX.X, ALU.add), reads=[P[7].tile], writes=[MS])
                    ts('dve', MS[:], MS[:], -1.0 / 128, None, ALU.mult, None, [MS], [MS])
                    tt('dve', OC[:].rearrange("p (a b) -> p a b", a=4), p7, MS[:].unsqueeze(2).to_broadcast([128, 4, 128]),
                       ALU.add, [P[7].tile, MS], [OC])
                    act(SQ[:], OC[:], AF.Square, [OC], [SQ])
                    S.op('dve', lambda g: g.tensor_reduce(SS[:], SQ[:].rearrange("p (a b) -> p a b", a=4), AX.X, ALU.add),
                         reads=[SQ], writes=[SS])
                    ts('dve', SS[:], SS[:], 1.0 / 128, NORM_EPS, ALU.mult, ALU.add, [SS], [SS])
                    act(SS[:], SS[:], AF.Sqrt, [SS], [SS])
                    S.op('dve', lambda g: g.reciprocal(SS[:], SS[:]), reads=[SS], writes=[SS])
                    tt('dve', SQ[:].rearrange("p (a b) -> p a b", a=4), OC[:].rearrange("p (a b) -> p a b", a=4),
                       SS[:].unsqueeze(2).to_broadcast([128, 4, 128]), ALU.mult, [OC, SS], [SQ])
                    yb = YB[i % 2]
                    tt('dve', yb[:], SQ[:], SG[:], ALU.mult, [SQ, SG], [yb])
                    S.dma('sp', YMIX[128 * i:128 * (i + 1), 0:512], yb[:], reads=[yb], st=yb)

        def layer_norm(ph_t, Z, OUT, G, Bt):
            ST, MV, RS_ = ph_t['ST'], ph_t['MV'], ph_t['RS']
            for k in range(2):
                S.op('dve', lambda g, k=k: g.bn_stats(ST[:, k, :], Z[:, k * 512:(k + 1) * 512]), reads=[Z], writes=[ST])
            S.op('dve', lambda g: g.bn_aggr(MV[:], ST[:].rearrange("p a b -> p (a b)")), reads=[ST], writes=[MV])
            ts('dve', RS_[:], MV[:, 1:2], LN_EPS, None, ALU.add, None, [MV], [RS_])
            act(RS_[:], RS_[:], AF.Sqrt, [RS_], [RS_])
            S.op('dve', lambda g: g.reciprocal(RS_[:], RS_[:]), reads=[RS_], writes=[RS_])
            ts('dve', OUT[:], Z[:], MV[:, 0:1], RS_[:, 0:1], ALU.subtract, ALU.mult, [Z, MV, RS_], [OUT])
            tt('dve', OUT[:], OUT[:], G[:], ALU.mult, [OUT, G], [OUT])
            tt('dve', OUT[:], OUT[:], Bt[:], ALU.add, [OUT, Bt], [OUT])

        def ln_tiles(ph):
            return {'ST': ph.sbuf("lnST", [128, 2, 6], F32), 'MV': ph.sbuf("lnMV", [128, 2], F32),
                    'RS': ph.sbuf("lnRS", [128, 1], F32)}

        def oproj_phase(Xsrc, L, kind, wo_d):
            with Phase(S) as ph:
                Wo = ph.sbuf("Wo", [128, 8, 1024], BF16)
                S.dma('pool', Wo[:], wo_d.rearrange("(c p) n -> p c n", p=128), writes=[Wo], st=Wo)
                G = ph.sbuf("G", [128, 1024], F32)
                Bt = ph.sbuf("Bt", [128, 1024], F32)
                S.dma('sp', G[:], lnp[L, 0:1, :].to_broadcast([128, 1024]), writes=[G], st=G)
                S.dma('sp', Bt[:], lnp[L, 1:2, :].to_broadcast([128, 1024]), writes=[Bt], st=Bt)
                Wr = ph.sbuf("Wr", [128, 8, 32], F32)
                S.dma('sp', Wr[:], rw[L].rearrange("(c p) n -> p c n", p=128), writes=[Wr], st=Wr)
                rbt = ph.sbuf("rbt", [1, 32], F32)
                S.dma('sp', rbt[:], rb[L:L + 1, :], writes=[rbt], st=rbt)
                lt_2 = [ln_tiles(ph), ln_tiles(ph)]
                ym = [ph.sbuf("ym%d" % i, [128, 1024], BF16) for i in range(2)]
                yT_2 = [ph.sbuf("yT%d" % i_, [128, 8, 128], BF16) for i_ in range(2)]
                xt_ = [ph.sbuf("xt%d" % i, [128, 1024], F32) for i in range(2)]
                Z_2 = [ph.sbuf("Z%d" % i_, [128, 1024], F32) for i_ in range(2)]
                x1 = [ph.sbuf("x1_%d" % i, [128, 1024], F32) for i in range(2)]
                x1T_2 = [ph.sbuf("x1T%d" % i_, [128, 8, 128], F32) for i_ in range(2)]
                LG_2 = [ph.sbuf("LG%d" % i_, [128, 32], F32) for i_ in range(2)]
                MX_2 = [ph.sbuf("MX%d" % i_, [128, 8], F32) for i_ in range(2)]
                MI_2 = [ph.sbuf("MI%d" % i_, [128, 8], U32) for i_ in range(2)]
                MIF_2 = [ph.sbuf("MIF%d" % i_, [128, 4], F32) for i_ in range(2)]
                NM_2 = [ph.sbuf("NM%d" % i_, [128, 1], F32) for i_ in range(2)]
                EX_2 = [ph.sbuf("EX%d" % i_, [128, 4], F32) for i_ in range(2)]
                SM_2 = [ph.sbuf("SM%d" % i_, [128, 1], F32) for i_ in range(2)]
                MK_2 = [ph.sbuf("MK%d" % i_, [128, 32], BF16) for i_ in range(2)]
                SLOT_2 = [ph.sbuf("SLOT%d" % i_, [128, 32], F32) for i_ in range(2)]
                CNT = ph.sbuf("CNT", [128, 32], F32)
                TMP_2 = [ph.sbuf("TMP%d" % i_, [128, 32], F32) for i_ in range(2)]
                SK_2 = [ph.sbuf("SK%d" % i_, [128, 4], F32) for i_ in range(2)]
                GSF_2 = [ph.sbuf("GSF%d" % i_, [128, 4], F32) for i_ in range(2)]
                S.op('dve', lambda g: g.memset(CNT[:], 0.0), writes=[CNT])
                if kind == 'o':
                    Uu = [[ph.sbuf("Uu%d_%d" % (p, i), [128, 8, 65], F32) for i in range(2)] for p in range(3)]
                    RD = ph.sbuf("RD", [128, 8], F32)
                iota32 = cst[:, C_IOTA:C_IOTA + 32]
                tri = cb[:, C_TRI:C_TRI + 128]
                ones = cb[:, C_ONE:C_ONE + 128]
                scat = [ph.sbuf("scat%d" % k, [128, 1], I32) for k in range(4)]
                def bind(i):
                    return [t2_[i % 2] for t2_ in (yT_2, Z_2, x1T_2, LG_2, MX_2, MI_2, MIF_2, NM_2, EX_2, SM_2, MK_2, SLOT_2,
                                                   TMP_2, SK_2, GSF_2)]

                def stageA(i):
                    rows = slice(128 * i, 128 * (i + 1))
                    (yT, Z, x1T, LG, MX, MI, MIF, NM, EX, SM, MK, SLOT, TMP, SK, GSF) = bind(i)
                    PL = P[5] if i % 2 == 0 else P[7]
                    lt = lt_2[i % 2]
                    y = ym[i % 2]
                    if kind == 'e':
                        S.dma('sp', y[:], YMIX[rows, :], writes=[y], st=y)
                    else:
                        S.dma('sp', y[:, 0:512], YMIX[rows, 0:512], writes=[y], st=y)
                        us = [Uu[p][i % 2] for p in range(3)]
                        for p in range(3):
                            S.dma('sp', us[p][:].rearrange("p a b -> p (a b)"), UD[p][rows, :], writes=[us[p]], st=us[p])
                        tt('dve', us[0][:], us[0][:], us[1][:], ALU.add, [us[0], us[1]], [us[0]])
                        tt('dve', us[0][:], us[0][:], us[2][:], ALU.add, [us[0], us[2]], [us[0]])
                        S.op('dve', lambda g: g.reciprocal(RD[:], us[0][:, :, 64]), reads=[us[0]], writes=[RD])
                        tt('dve', y[:, 512:1024].rearrange("p (a b) -> p a b", a=8), us[0][:, :, 0:64],
                           RD[:].unsqueeze(2).to_broadcast([128, 8, 64]), ALU.mult, [us[0], RD], [y])
                    pt = P[0].tile[:].bitcast(BF16)
                    for c in range(8):
                        tr(P[0], pt[:, c * 128:(c + 1) * 128], y[:, c * 128:(c + 1) * 128], identb, [y] + CK)
                    S.op('act', lambda g: g.copy(yT[:].rearrange("p a b -> p (a b)"), pt), reads=[P[0].tile], writes=[yT])
                    for bk in P[1:8]:
                        bk.started = False
                    for k in range(2):
                        for c in range(8):
                            mm(P[1 + k], P[1 + k].tile[:, :], yT[:, c, :], Wo[:, c, k * 512:(k + 1) * 512], [yT, Wo])
                    xx = xt_[i % 2]
                    S.dma('sp', xx[:], Xsrc[rows, :], writes=[xx], st=xx)
                    for k in range(2):
                        S.op('dve', lambda g, k=k: g.scalar_tensor_tensor(
                            Z[:, k * 512:(k + 1) * 512], xx[:, k * 512:(k + 1) * 512], DN_ALPHA, P[1 + k].tile[:, :],
                            ALU.mult, ALU.add), reads=[xx, P[1 + k].tile], writes=[Z])
                    xo = x1[i % 2]
                    layer_norm(lt, Z, xo, G, Bt)
                    S.dma('sp', X1D[rows, :], xo[:], reads=[xo], st=xo)
                    S.dma('pool', X1B[rows, :], xo[:], reads=[xo], st=xo)
                    for c in range(8):
                        bk = P[3 + c // 4]
                        tr(bk, bk.tile[:, (c % 4) * 128:(c % 4 + 1) * 128], xo[:, c * 128:(c + 1) * 128], identf, [xo] + CK)
                    for k in range(2):
                        S.op('act', lambda g, k=k: g.copy(x1T[:, 4 * k:4 * k + 4, :].rearrange("p a b -> p (a b)"),
                                                          P[3 + k].tile[:, :]), reads=[P[3 + k].tile], writes=[x1T])
                    PL.started = False
                    for c in range(8):
                        mm(PL, PL.tile[:, 0:32], x1T[:, c, :], Wr[:, c, :], [x1T, Wr])
                    mm(PL, PL.tile[:, 0:32], onesf[0:1, :], rbt[0:1, :], [onesf, rbt])

                def stageB(i):
                    (yT, Z, x1T, LG, MX, MI, MIF, NM, EX, SM, MK, SLOT, TMP, SK, GSF) = bind(i)
                    PL = P[5] if i % 2 == 0 else P[7]
                    P[6].started = False
                    S.op('dve', lambda g: g.tensor_copy(LG[:], PL.tile[:, 0:32]), reads=[PL.tile], writes=[LG])
                    S.op('dve', lambda g: g.max(MX[:], LG[:]), reads=[LG], writes=[MX])
                    S.op('dve', lambda g: g.max_index(MI[:], MX[:], LG[:]), reads=[LG, MX], writes=[MI])
                    ts('dve', NM[:], MX[:, 0:1], -1.0, None, ALU.mult, None, [MX], [NM])
                    act(EX[:], MX[:, 0:4], AF.Exp, [MX, NM], [EX, SM], bias=NM[:, 0:1], accum_out=SM[:, 0:1])
                    S.op('dve', lambda g: g.reciprocal(SM[:], SM[:]), reads=[SM], writes=[SM])
                    ts('dve', GT[:, i, :], EX[:], SM[:, 0:1], None, ALU.mult, None, [EX, SM], [('GT', i)])
                    ts('dve', MK[:], LG[:], MX[:, 3:4], None, ALU.is_ge, None, [LG, MX], [MK])
                    mm(P[6], P[6].tile[:, 0:32], tri, MK[:], [MK] + CK)
                    mm(P[6], P[6].tile[:, 32:64], ones, MK[:], [MK] + CK)
                    tt('dve', SLOT[:], P[6].tile[:, 0:32], CNT[:], ALU.add, [P[6].tile, CNT], [SLOT])
                    tt('dve', CNT[:], CNT[:], P[6].tile[:, 32:64], ALU.add, [P[6].tile, CNT], [CNT])
                    S.op('dve', lambda g: g.tensor_copy(MIF[:], MI[:, 0:4]), reads=[MI], writes=[MIF])
                    for k in range(4):
                        S.op('dve', lambda g, k=k: g.scalar_tensor_tensor(
                            TMP[:], iota32, MIF[:, k:k + 1], SLOT[:], ALU.is_equal, ALU.mult, accum_out=SK[:, k:k + 1]),
                            reads=[MIF, SLOT] + CK, writes=[TMP, SK])
                    ts('dve', SK[:], SK[:], float(CAP - 1), None, ALU.min, None, [SK], [SK])
                    S.op('dve', lambda g: g.scalar_tensor_tensor(GSF[:], MIF[:], float(CAP), SK[:], ALU.mult, ALU.add),
                         reads=[MIF, SK], writes=[GSF])
                    S.op('dve', lambda g: g.tensor_copy(GS[:, i, :], GSF[:]), reads=[GSF], writes=[('GS', i)])
                    for k in range(4):
                        S.dma('pool', None, None, reads=[('GS', i)], st=scat[k],
                              fn=lambda q, k=k: q.indirect_dma_start(
                                  out=TOK, out_offset=bass.IndirectOffsetOnAxis(ap=GS[:, i, k:k + 1], axis=0),
                                  in_=tokid[:, i, :], in_offset=None))

                stageA(0)
                for i in range(1, NT):
                    stageA(i)
                    stageB(i - 1)
                stageB(NT - 1)

        def expert_phase(L):
            NS = CAP // 128
            groups = [(g0, min(512, CAP - g0)) for g0 in range(0, CAP, 512)]
            with Phase(S) as ph:
                WG = [ph.sbuf("WG%d" % i, [128, 8, 2048], BF16) for i in range(2)]
                WD = [ph.sbuf("WD%d" % i, [128, 8, 1024], BF16) for i in range(2)]
                BDN = [ph.sbuf("BDN%d" % i, [128, 1024], F32) for i in range(2)]
                BGU = ph.sbuf("BGU", [128, 512], F32)
                BGT = [ph.sbuf("BGT%d" % i, [128, 128], F32) for i in range(4)]
                bsrc = ebgu[L].rearrange("e (c p) -> (e c) p", p=128)
                P[7].started = False
                for q4 in range(4):
                    S.dma('sp', BGT[q4][:], bsrc[q4 * 128:(q4 + 1) * 128, :], writes=[BGT[q4]], st=BGT[q4])
                    tr(P[7], P[7].tile[:, q4 * 128:(q4 + 1) * 128], BGT[q4][:], identf, [BGT[q4]] + CK)
                S.op('dve', lambda g: g.tensor_copy(BGU[:], P[7].tile[:, :]), reads=[P[7].tile], writes=[BGU])
                bv = BGU[:].rearrange("p (e c) -> p e c", c=16)
                ts('dve', bv[:, :, 8:16], bv[:, :, 8:16], 1.0, None, ALU.add, None, [BGU], [BGU])
                IDX = [ph.sbuf("IDX%d" % i, [128, 16], I32) for i in range(NS)]
                XG = [ph.sbuf("XG%d" % i, [128, 1024], BF16) for i in range(NS)]
                XGT = ph.sbuf("XGT", [128, 8, CAP], BF16)
                AT = [ph.sbuf("AT%d" % i, [128, 8, 512], BF16) for i in range(2)]
                GC = ph.sbuf("GC", [128, 512], F32)
                SGt = ph.sbuf("SGt", [128, 512], F32)
                UC = ph.sbuf("UC", [128, 512], F32)
                YO = [ph.sbuf("YO%d" % i, [128, 1024], F32) for i in range(2)]
                cnt_ = [0, 0]

                def e_gather(e):
                    wg, wdn, bdn = WG[e % 2], WD[e % 2], BDN[e % 2]
                    S.dma('pool', wg[:], ewgu[L, e].rearrange("(c p) n -> p c n", p=128), writes=[wg], st=wg)
                    S.dma('pool', wdn[:], ewdn[L, e].rearrange("(c p) n -> p c n", p=128), writes=[wdn], st=wdn)
                    S.dma('sp', bdn[:], ebdn[L, e:e + 1, :].to_broadcast([128, 1024]), writes=[bdn], st=bdn)
                    for j in range(NS):
                        ix, xg = IDX[j], XG[j]
                        r0 = e * CAP + j * 128
                        S.dma('sp', ix[:], TOK[r0:r0 + 128, :], writes=[ix], st=ix)
                        S.dma('pool', None, None, reads=[ix], writes=[xg], st=xg,
                              fn=lambda q, ix=ix, xg=xg: q.indirect_dma_start(
                                  out=xg[:], out_offset=None, in_=X1B,
                                  in_offset=bass.IndirectOffsetOnAxis(ap=ix[:, 0:1], axis=0)))

                def e_transp(e):
                    for j in range(NS):
                        xg = XG[j]
                        pt = P[0].tile[:].bitcast(BF16)
                        for c in range(8):
                            tr(P[0], pt[:, c * 128:(c + 1) * 128], xg[:, c * 128:(c + 1) * 128], identb, [xg] + CK)
                        S.op('act', lambda g, j=j: g.copy(XGT[:, :, j * 128:(j + 1) * 128],
                                                          pt.rearrange("p (a b) -> p a b", a=8)),
                             reads=[P[0].tile], writes=[XGT])

                def e_compute(e):
                    wg, wdn, bdn = WG[e % 2], WD[e % 2], BDN[e % 2]
                    n = cnt_[0]
                    gi = cnt_[1]
                    for (g0, gn) in groups:
                        at = AT[gi % 2]
                        gi += 1
                        for fc in range(8):
                            bg, bu = P[1 + (fc % 2) * 2], P[2 + (fc % 2) * 2]
                            bg.started = False
                            bu.started = False
                            for (bk, f0) in ((bg, fc * 128), (bu, 1024 + fc * 128)):
                                for c in range(8):
                                    mm(bk, bk.tile[:, 0:gn], wg[:, c, f0:f0 + 128], XGT[:, c, g0:g0 + gn], [wg, XGT])
                            ts('dve', GC[:, 0:gn], bg.tile[:, 0:gn], BGU[:, e * 16 + fc:e * 16 + fc + 1], 7.0, ALU.add, ALU.min,
                               [bg.tile, BGU], [GC])
                            act(SGt[:, 0:gn], GC[:, 0:gn], AF.Sigmoid, [GC], [SGt], scale=1.702)
                            tt('pool', GC[:, 0:gn], GC[:, 0:gn], SGt[:, 0:gn], ALU.mult, [GC, SGt], [GC])
                            ts('dve', UC[:, 0:gn], bu.tile[:, 0:gn], BGU[:, e * 16 + 8 + fc:e * 16 + 8 + fc + 1], -6.0,
                               ALU.add, ALU.max, [bu.tile, BGU], [UC])
                            S.op('dve', lambda g, fc=fc, at=at: g.scalar_tensor_tensor(
                                at[:, fc, 0:gn], UC[:, 0:gn], 8.0, GC[:, 0:gn], ALU.min, ALU.mult),
                                reads=[UC, GC], writes=[at])
                        for s0 in range(0, gn, 128):
                            yo = YO[n % 2]
                            n += 1
                            for k in range(2):
                                bk = P[5 + k]
                                bk.started = False
                                for c in range(8):
                                    mm(bk, bk.tile[:, :], at[:, c, s0:s0 + 128], wdn[:, c, k * 512:(k + 1) * 512], [at, wdn])
                                tt('dve', yo[:, k * 512:(k + 1) * 512], bk.tile[:, :], bdn[:, k * 512:(k + 1) * 512], ALU.add,
                                   [bk.tile, bdn], [yo])
                            r0 = e * CAP + g0 + s0
                            S.dma('sp', YE[r0:r0 + 128, :], yo[:], reads=[yo], st=yo)
                    cnt_[0] = n
                    cnt_[1] = gi

                e_gather(0)
                e_transp(0)
                for e in range(NEXP):
                    if e + 1 < NEXP:
                        e_gather(e + 1)
                    e_compute(e)
                    if e + 1 < NEXP:
                        e_transp(e + 1)

        def combine_phase(L, Xdst):
            with Phase(S) as ph:
                G = ph.sbuf("G", [128, 1024], F32)
                Bt = ph.sbuf("Bt", [128, 1024], F32)
                S.dma('sp', G[:], lnp[L, 2:3, :].to_broadcast([128, 1024]), writes=[G], st=G)
                S.dma('sp', Bt[:], lnp[L, 3:4, :].to_broadcast([128, 1024]), writes=[Bt], st=Bt)
                lt = ln_tiles(ph)
                YK = [[ph.sbuf("YK%d_%d" % (k, i), [128, 1024], F32) for i in range(2)] for k in range(4)]
                ACC = ph.sbuf("ACC", [128, 1024], F32)
                xt_ = [ph.sbuf("xt%d" % i, [128, 1024], F32) for i in range(2)]
                XN = [ph.sbuf("XN%d" % i, [128, 1024], F32) for i in range(2)]
                for i in range(NT):
                    rows = slice(128 * i, 128 * (i + 1))
                    yk = [YK[k][i % 2] for k in range(4)]
                    for k in range(4):
                        S.dma('pool', None, None, reads=[('GS', i)], writes=[yk[k]], st=yk[k],
                              fn=lambda q, k=k: q.indirect_dma_start(
                                  out=yk[k][:], out_offset=None, in_=YE,
                                  in_offset=bass.IndirectOffsetOnAxis(ap=GS[:, i, k:k + 1], axis=0)))
                    xx = xt_[i % 2]
                    S.dma('sp', xx[:], X1D[rows, :], writes=[xx], st=xx)
                    ts('dve', ACC[:], yk[0][:], GT[:, i, 0:1], None, ALU.mult, None, [yk[0], ('GT', i)], [ACC])
                    for k in range(1, 4):
                        S.op('dve', lambda g, k=k: g.scalar_tensor_tensor(
                            ACC[:], yk[k][:], GT[:, i, k:k + 1], ACC[:], ALU.mult, ALU.add),
                            reads=[yk[k], ('GT', i), ACC], writes=[ACC])
                    S.op('dve', lambda g: g.scalar_tensor_tensor(ACC[:], xx[:], DN_ALPHA, ACC[:], ALU.mult, ALU.add),
                         reads=[xx, ACC], writes=[ACC])
                    xn = XN[i % 2]
                    layer_norm(lt, ACC, xn, G, Bt)
                    S.dma('sp', Xdst[rows, :], xn[:], reads=[xn], st=xn)

        def zero_tok():
            tk = TOK.rearrange("(p a) b -> p (a b)", p=128)
            ncols = NEXP * CAP * 16 // 128
            for c0 in range(0, ncols, 1024):
                cn = min(1024, ncols - c0)
                S.dma('sp', tk[:, c0:c0 + cn], zer[:, 0:cn], reads=[zer], st=zer)

        je = jo = 0
        Xcur = x_in
        for L, kind in enumerate(layer_kinds):
            zero_tok()
            if kind == 'e':
                attn_pass(Xcur, wA[je], 1, C_MPA, ropeA[1], sinks[je], None)
                hgrn_pass(Xcur, wB[je], je, hnorm[je])
                oproj_phase(Xcur, L, 'e', woe[je])
                je += 1
            else:
                ret_pass(Xcur, wC[jo], rnorm[jo])
                for p, d in enumerate(DPAT):
                    attn_pass(Xcur, wDp[jo][p], d, C_MPD, ropeA[d], None, p)
                oproj_phase(Xcur, L, 'o', woo[jo])
                jo += 1
            expert_phase(L)
            Xdst = out_d if L == NL - 1 else XS
            combine_phase(L, Xdst)
            Xcur = XS
        S.barrier()
        print("bass program: ninst=%d" % S.ninst)
    return nc


def _rot_perm(nheads, hd, half):
    idx = np.arange(nheads * hd).reshape(nheads, hd).copy()
    for h in range(nheads):
        base = h * hd
        idx[h, :half] = base + np.arange(half, 2 * half)
        idx[h, half:2 * half] = base + np.arange(0, half)
    return idx.reshape(-1)


def _consts(T):
    NT = T // 128
    cst = np.zeros((128, NCST), np.float32)
    i = np.arange(128)
    cst[:, C_ID:C_ID + 128] = np.eye(128)
    kj, qi = i[:, None], i[None, :]
    cst[:, C_MCUR:C_MCUR + 128] = (qi >= kj)
    cst[:, C_MPA:C_MPA + 128] = (kj >= qi + 1)
    cst[:, C_MPD:C_MPD + 128] = (kj >= qi)
    cst[:, C_MBD:C_MBD + 128] = ((kj // 32) == (qi // 32)) & (kj <= qi)
    cst[:, C_TRI:C_TRI + 128] = (kj < qi)
    cst[:, C_ONE:C_ONE + 128] = 1.0
    cst[:, C_IOTA:C_IOTA + 32] = np.arange(32)[None, :]
    rs = np.ones(512)
    rs[::32] = 0.0
    cst[:, C_RS:C_RS + 512] = rs[None, :]
    lg = np.log1p(-np.exp2(-5.0 - np.arange(4, dtype=np.float64)))
    for h in range(4):
        rel = (qi - kj).astype(np.float64)
        cst[:, C_DT + h * 128:C_DT + (h + 1) * 128] = np.where(rel >= 0, np.exp(lg[h] * np.maximum(rel, 0)), 0.0)
        cst[:64, C_QD + h * 128:C_QD + (h + 1) * 128] = np.exp(lg[h] * (i + 1.0))[None, :]
        cst[:64, C_KD + h * 128:C_KD + (h + 1) * 128] = np.exp(lg[h] * (127.0 - i))[None, :]
    for jc in range(4):
        cst[:, C_RM + jc] = (i // 32 == jc)
    tokid = np.zeros((128, NT, 16), np.int32)
    tokid[:] = (np.arange(NT)[None, :, None] * 128 + np.arange(128)[:, None, None])
    pos = np.arange(T, dtype=np.float32)
    inv = (1.0 / (np.float32(500000.0) ** (np.arange(0, 16, 2, dtype=np.float32) / np.float32(16)))).astype(np.float32)
    ang = (pos[:, None] * inv[None, :]).astype(np.float32).astype(np.float64)
    Cn = np.ones((64, T))
    Sn = np.zeros((64, T))
    Cn[0:8] = np.cos(ang).T
    Cn[8:16] = np.cos(ang).T
    Sn[0:8] = -np.sin(ang).T
    Sn[8:16] = np.sin(ang).T
    rope = {}
    for d in DPAT:
        nb = T // (128 * d)
        perm = np.concatenate([r + d * (128 * b + np.arange(128)) for r in range(d) for b in range(nb)])
        rope[d] = (np.ascontiguousarray(Cn[:, perm], dtype=np.float32), np.ascontiguousarray(Sn[:, perm], dtype=np.float32))
    rinv = (1.0 / (np.float32(10000.0) ** np.linspace(0.0, 1.0, 32, dtype=np.float32))).astype(np.float32)
    rang = (pos[:, None] * rinv[None, :]).astype(np.float32).astype(np.float64)
    RC = np.concatenate([np.cos(rang).T, np.cos(rang).T], 0)
    RSn = np.concatenate([-np.sin(rang).T, np.sin(rang).T], 0)
    ret = [RC, RSn, RC * 0.125, RSn * 0.125]
    ret = [np.ascontiguousarray(a, dtype=np.float32) for a in ret]
    return cst, tokid.reshape(128, NT * 16), rope, ret


def _prep_shared(inp, layer_kinds, T):
    f = lambda a: np.ascontiguousarray(np.asarray(a), dtype=np.float32)
    cst, tokid, rope, ret = _consts(T)
    m = {"cst": cst, "tokid": tokid}
    for d in DPAT:
        m["ropeC%d" % d], m["ropeS%d" % d] = rope[d]
    for i in range(4):
        m["ret%d" % i] = ret[i]
    m["lbl"] = f(inp["hgrn_lb_logits"])[:2]
    pq = _rot_perm(8, 64, 8)
    pk = _rot_perm(2, 64, 8)
    pc = _rot_perm(4, 64, 32)
    NL = len(layer_kinds)
    je = jo = 0
    for L, k in enumerate(layer_kinds):
        if k == 'e':
            w = np.concatenate([f(inp["w_in_even"])[je], f(inp["b_in_even"])[je][None, :]], 0)
            q, kk, v = w[:, 0:512], w[:, 512:640], w[:, 640:768]
            m["wA%d" % je] = np.ascontiguousarray(np.concatenate([q, q[:, pq], kk, kk[:, pk], v], 1))
            m["wB%d" % je] = np.ascontiguousarray(w[:, 768:2816])
            m["sink%d" % je] = f(inp["attn_sinks"])[je][None, :]
            m["hnorm%d" % je] = f(inp["hgrn_norm"])[je][None, :]
            m["woe%d" % je] = f(inp["w_out_even"])[je]
            je += 1
        else:
            w = np.concatenate([f(inp["w_in_odd"])[jo], f(inp["b_in_odd"])[jo][None, :]], 0)
            cq, ck, cv, cg = w[:, 0:256], w[:, 256:512], w[:, 512:1024], w[:, 1024:1536]
            m["wC%d" % jo] = np.ascontiguousarray(np.concatenate([cq, cq[:, pc], ck, ck[:, pc], cv, cg], 1))
            for p in range(3):
                o = 1536 + p * 768
                q, kk, v = w[:, o:o + 512], w[:, o + 512:o + 640], w[:, o + 640:o + 768]
                m["wD%d_%d" % (jo, p)] = np.ascontiguousarray(np.concatenate([q, q[:, pq], kk, kk[:, pk], v], 1))
            m["rnorm%d" % jo] = f(inp["ret_norm"])[jo][None, :]
            m["woo%d" % jo] = f(inp["w_out_odd"])[jo]
            jo += 1
    m["lnp"] = np.ascontiguousarray(np.stack([f(inp["ln1_g"])[:NL], f(inp["ln1_b"])[:NL], f(inp["ln2_g"])[:NL],
                                              f(inp["ln2_b"])[:NL]], 1))
    m["rw"] = f(inp["router_w"])[:NL]
    m["rb"] = f(inp["router_b"])[:NL]
    m["ewgu"] = f(inp["expert_w_gu"])[:NL]
    m["ebgu"] = f(inp["expert_b_gu"])[:NL]
    m["ewdn"] = f(inp["expert_w_dn"])[:NL]
    m["ebdn"] = f(inp["expert_b_dn"])[:NL]
    return m


def run(inp, layer_kinds, CAP, debug=False):
    x = np.ascontiguousarray(np.asarray(inp["x"]), dtype=np.float32)
    B, T, _ = x.shape
    nc = build(T, layer_kinds, CAP, debug=debug)
    shared = _prep_shared(inp, layer_kinds, T)
    in_maps = []
    for b in range(B):
        mm_ = dict(shared)
        mm_["x"] = x[b]
        in_maps.append(mm_)
    res = run_bass_kernel_spmd(nc, in_maps, core_ids=list(range(B)))
    return res


def kernel(**inputs):
    res = run(inputs, ['e', 'o', 'e', 'o'], 1280)
    return np.stack([np.asarray(r["out"]) for r in res.results], 0).astype(np.float32)
```
